# Optimizing a Trainium2 kernel written in Bass

```python
import math
import jax, jax.numpy as jnp
from jax import lax
import numpy as np


D_MODEL = 2048
BATCH = 4
SEQ = 4096
DEPTH = 2

HEAD_DIM = 64
N_HEADS = D_MODEL // HEAD_DIM
H_A = 12
H_B = 10
H_C = N_HEADS - H_A - H_B
W_A = H_A * HEAD_DIM
W_B = H_B * HEAD_DIM
W_C = H_C * HEAD_DIM
IN_COLS = 3 * (W_A + W_B + W_C) + H_C
DIL_PATTERNS = ((128, 1), (512, 4), (2048, 16))
MOBA_BLOCK = 256
MOBA_TOPK = 3
MOBA_QBLOCK = 32
FOX_QBLOCK = 128
N_BUCKETS = 32
REL_MAX_DIST = 2048
D_FF = 5632
CONV_WIDTH = 3
DEEPNORM_ALPHA = (2 * DEPTH) ** 0.25
DEEPNORM_BETA = (8 * DEPTH) ** -0.25
LN_EPS = 1e-5
NEG = -1e30
ATTN_SCALE = HEAD_DIM ** -0.5

kernel_name = 'hybrid_dilated_moba_fox_block'


def layer_norm(x, g, b):
    xf = x.astype(jnp.float32)
    mu = jnp.mean(xf, axis=-1, keepdims=True)
    var = jnp.mean(jnp.square(xf - mu), axis=-1, keepdims=True)
    return ((xf - mu) * lax.rsqrt(var + LN_EPS) * g + b).astype(x.dtype)


def rms_norm(x, g):
    xf = x.astype(jnp.float32)
    return (xf * lax.rsqrt(jnp.mean(xf * xf, axis=-1, keepdims=True) + LN_EPS) * g).astype(x.dtype)


def t5_bucket(dist):
    n = jnp.maximum(dist, 0)
    max_exact = N_BUCKETS // 2
    nf = jnp.maximum(n, 1).astype(jnp.float32)
    large = max_exact + (jnp.log(nf / max_exact) / math.log(REL_MAX_DIST / max_exact)
                         * (N_BUCKETS - max_exact)).astype(jnp.int32)
    large = jnp.minimum(large, N_BUCKETS - 1)
    return jnp.where(n < max_exact, n, large)


def _chunks(t, nc, qb):
    b, h = t.shape[0], t.shape[1]
    return jnp.moveaxis(t.reshape(b, h, nc, qb, *t.shape[3:]), 2, 0)


def dilated_window_attention(q, k, v, rel_tab, window, dilation):
    B, H, S, hd = q.shape
    W = window // dilation
    L = S // dilation
    nb = -(-L // W)
    Lp = nb * W

    def to_blocks(t):
        t = t.reshape(B, H, L, dilation, hd).transpose(0, 1, 3, 2, 4)
        t = jnp.pad(t, ((0, 0), (0, 0), (0, 0), (0, Lp - L), (0, 0)))
        return t.reshape(B, H, dilation, nb, W, hd)

    def with_prev(t):
        prev = jnp.pad(t, ((0, 0), (0, 0), (0, 0), (1, 0), (0, 0), (0, 0)))[:, :, :, :-1]
        return jnp.concatenate([prev, t], axis=4)

    qb = to_blocks(q)
    kk = with_prev(to_blocks(k))
    vv = with_prev(to_blocks(v))
    a = jnp.arange(W)[:, None]
    j = jnp.arange(2 * W)[None, :]
    dist = a + W - j
    band = (dist >= 0) & (dist <= W)
    first = (jnp.arange(nb)[:, None, None] == 0) & (j[None] < W)
    mask = band[None] & ~first
    bias = jnp.moveaxis(rel_tab[t5_bucket(dist * dilation)], -1, 0)
    s = (jnp.einsum('bhrnqd,bhrnkd->bhrnqk', qb, kk).astype(jnp.float32) * ATTN_SCALE
         + bias[:, None, None].astype(jnp.float32))
    s = jnp.where(mask, s, NEG)
    m = jnp.max(s, axis=-1, keepdims=True)
    p = jnp.exp(s - m)
    l = jnp.sum(p, axis=-1, keepdims=True)
    o = jnp.einsum('bhrnqk,bhrnkd->bhrnqd', p.astype(v.dtype), vv).astype(jnp.float32) / l
    lse = (m + jnp.log(l))[..., 0]

    def from_blocks(t):
        t = t.reshape(B, H, dilation, Lp, *t.shape[5:])[:, :, :, :L]
        t = jnp.swapaxes(t, 2, 3)
        return t.reshape(B, H, S, *t.shape[4:])

    return from_blocks(o), from_blocks(lse)


def dilated_mixture(q, k, v, rel_tab):
    res = [dilated_window_attention(q, k, v, rel_tab, w, d) for (w, d) in DIL_PATTERNS]
    outs = jnp.stack([r[0] for r in res])
    lses = jnp.stack([r[1] for r in res])
    wts = jax.nn.softmax(lses, axis=0)
    return jnp.einsum('gbhs,gbhsd->bhsd', wts, outs).astype(q.dtype)


def moba_attention(q, k, v, rel_tab):
    B, H, S, hd = q.shape
    blk = MOBA_BLOCK
    nblk = -(-S // blk)
    Sp = nblk * blk
    pad = ((0, 0), (0, 0), (0, Sp - S), (0, 0))
    kb = jnp.pad(k, pad).reshape(B, H, nblk, blk, hd)
    vb = jnp.pad(v, pad).reshape(B, H, nblk, blk, hd)
    kmean = jnp.mean(kb.astype(jnp.float32), axis=3)
    gate = jnp.einsum('bhsd,bhnd->bhsn', q.astype(jnp.float32), kmean)
    qblk = jnp.arange(S) // blk
    past = jnp.arange(nblk)[None, :] < qblk[:, None]
    gate = jnp.where(past, gate, NEG)
    n_sel = min(MOBA_TOPK, nblk)
    _, idx = lax.top_k(gate, n_sel)
    valid = idx < qblk[:, None]
    QB = MOBA_QBLOCK
    nc = S // QB
    bi = jnp.arange(B)[:, None, None, None]
    hi = jnp.arange(H)[None, :, None, None]
    hi5 = jnp.arange(H)[None, :, None, None, None]
    tab_t = rel_tab.T

    def step(xs):
        ci, q_c, idx_c, valid_c = xs
        qpos = ci * QB + jnp.arange(QB)
        own = (ci * QB) // blk
        k_own = lax.dynamic_index_in_dim(kb, own, axis=2, keepdims=False)
        v_own = lax.dynamic_index_in_dim(vb, own, axis=2, keepdims=False)
        d_own = qpos[:, None] - (own * blk + jnp.arange(blk))[None, :]
        s_own = (jnp.einsum('bhqd,bhkd->bhqk', q_c, k_own).astype(jnp.float32) * ATTN_SCALE
                 + tab_t[:, t5_bucket(d_own)].astype(jnp.float32))
        s_own = jnp.where(d_own >= 0, s_own, NEG)
        k_sel = kb[bi, hi, idx_c]
        v_sel = vb[bi, hi, idx_c]
        d_sel = qpos[:, None, None] - (idx_c[..., None] * blk + jnp.arange(blk))
        s_sel = (jnp.einsum('bhqd,bhqnkd->bhqnk', q_c, k_sel).astype(jnp.float32) * ATTN_SCALE
                 + tab_t[hi5, t5_bucket(d_sel)].astype(jnp.float32))
        s_sel = jnp.where(valid_c[..., None], s_sel, NEG).reshape(B, H, QB, n_sel * blk)
        p = jax.nn.softmax(jnp.concatenate([s_sel, s_own], axis=-1), axis=-1).astype(v.dtype)
        p_sel = p[..., :n_sel * blk].reshape(B, H, QB, n_sel, blk)
        p_own = p[..., n_sel * blk:]
        return (jnp.einsum('bhqnk,bhqnkd->bhqd', p_sel, v_sel)
                + jnp.einsum('bhqk,bhkd->bhqd', p_own, v_own))

    out = lax.map(step, (jnp.arange(nc), _chunks(q, nc, QB), _chunks(idx, nc, QB),
                         _chunks(valid, nc, QB)))
    return jnp.moveaxis(out, 0, 2).reshape(B, H, S, hd)


def forgetting_attention(q, k, v, f_logit):
    B, H, S, hd = q.shape
    cum = lax.cumsum(jax.nn.log_sigmoid(f_logit.astype(jnp.float32)), axis=2)
    QB = FOX_QBLOCK
    nc = S // QB
    kpos = jnp.arange(S)

    def step(xs):
        ci, q_c, cum_c = xs
        qpos = ci * QB + jnp.arange(QB)
        s = (jnp.einsum('bhqd,bhkd->bhqk', q_c, k).astype(jnp.float32) * ATTN_SCALE
             + (cum_c[..., None] - cum[:, :, None, :]))
        s = jnp.where(kpos[None, :] <= qpos[:, None], s, NEG)
        p = jax.nn.softmax(s, axis=-1).astype(v.dtype)
        return jnp.einsum('bhqk,bhkd->bhqd', p, v)

    out = lax.map(step, (jnp.arange(nc), _chunks(q, nc, QB), _chunks(cum, nc, QB)))
    return jnp.moveaxis(out, 0, 2).reshape(B, H, S, hd)


def hybrid_mixer(h, w_in, b_f, g_mix_a, g_mix_b, g_mix_c, w_out, rel_bias):
    B, S, _ = h.shape
    proj = h @ w_in
    sizes = [W_A] * 3 + [W_B] * 3 + [W_C] * 3 + [H_C]
    offs = np.cumsum(sizes)[:-1].tolist()
    qa, ka, va, qb, kb, vb, qc, kc, vc, f = jnp.split(proj, offs, axis=-1)

    def heads(t, n):
        return t.reshape(B, S, n, HEAD_DIM).transpose(0, 2, 1, 3)

    def merge(o):
        return o.transpose(0, 2, 1, 3).reshape(B, S, -1)

    o_a = dilated_mixture(heads(qa, H_A), heads(ka, H_A), heads(va, H_A), rel_bias[:, :H_A])
    o_b = moba_attention(heads(qb, H_B), heads(kb, H_B), heads(vb, H_B), rel_bias[:, H_A:])
    f_logit = (f + b_f).transpose(0, 2, 1)
    o_c = forgetting_attention(heads(qc, H_C), heads(kc, H_C), heads(vc, H_C), f_logit)
    y = jnp.concatenate([rms_norm(merge(o_a), g_mix_a), rms_norm(merge(o_b), g_mix_b),
                         rms_norm(merge(o_c), g_mix_c)], axis=-1)
    return y @ w_out


def conv_ffn(h, w_up, conv_w, conv_b, w_down):
    u = h @ w_up
    u = lax.conv_general_dilated(u, conv_w[:, None, :], window_strides=(1,),
                                 padding=((CONV_WIDTH - 1, 0),),
                                 dimension_numbers=('NWC', 'WIO', 'NWC'),
                                 feature_group_count=u.shape[-1]) + conv_b
    a, b = jnp.split(u, 2, axis=-1)
    return (jax.nn.silu(a) * b) @ w_down


def setup_inputs(seed: int = 0) -> dict:
    key = jax.random.key(seed)
    ks = jax.random.split(key, 20)
    f32 = jnp.float32

    def nrm(k, shape, s):
        return jax.random.normal(k, shape, f32) * s

    return {
        'x': nrm(ks[0], (BATCH, SEQ, D_MODEL), 1.0),
        'c': nrm(ks[1], (BATCH, D_MODEL), 1.0),
        'rel_bias': nrm(ks[2], (N_BUCKETS, H_A + H_B), 0.2),
        'w_ada': nrm(ks[3], (DEPTH, D_MODEL, 6 * D_MODEL), 0.02),
        'b_ada': nrm(ks[4], (DEPTH, 6 * D_MODEL), 0.01),
        'w_in': nrm(ks[5], (DEPTH, D_MODEL, IN_COLS), D_MODEL ** -0.5),
        'b_f': 3.0 + nrm(ks[6], (DEPTH, H_C), 0.1),
        'g_mix_a': 1.0 + nrm(ks[7], (DEPTH, W_A), 0.02),
        'g_mix_b': 1.0 + nrm(ks[8], (DEPTH, W_B), 0.02),
        'g_mix_c': 1.0 + nrm(ks[9], (DEPTH, W_C), 0.02),
        'w_out': nrm(ks[10], (DEPTH, D_MODEL, D_MODEL), DEEPNORM_BETA * D_MODEL ** -0.5),
        'ln1_g': 1.0 + nrm(ks[11], (DEPTH, D_MODEL), 0.02),
        'ln1_b': nrm(ks[12], (DEPTH, D_MODEL), 0.02),
        'w_up': nrm(ks[13], (DEPTH, D_MODEL, 2 * D_FF), D_MODEL ** -0.5),
        'conv_w': nrm(ks[14], (DEPTH, CONV_WIDTH, 2 * D_FF), CONV_WIDTH ** -0.5),
        'conv_b': nrm(ks[15], (DEPTH, 2 * D_FF), 0.01),
        'w_down': nrm(ks[16], (DEPTH, D_FF, D_MODEL), DEEPNORM_BETA * D_FF ** -0.5),
        'ln2_g': 1.0 + nrm(ks[17], (DEPTH, D_MODEL), 0.02),
        'ln2_b': nrm(ks[18], (DEPTH, D_MODEL), 0.02),
    }


def reference(x, c, rel_bias, w_ada, b_ada, w_in, b_f, g_mix_a, g_mix_b, g_mix_c, w_out,
              ln1_g, ln1_b, w_up, conv_w, conv_b, w_down, ln2_g, ln2_b):
    cond = jax.nn.silu(c)
    for l in range(DEPTH):
        mod = cond @ w_ada[l] + b_ada[l]
        sh1, sc1, g1, sh2, sc2, g2 = [m[:, None, :] for m in jnp.split(mod, 6, axis=-1)]
        h = x * (1 + sc1) + sh1
        y = hybrid_mixer(h, w_in[l], b_f[l], g_mix_a[l], g_mix_b[l], g_mix_c[l], w_out[l], rel_bias)
        x = layer_norm(DEEPNORM_ALPHA * x + (1 + g1) * y, ln1_g[l], ln1_b[l])
        h = x * (1 + sc2) + sh2
        y = conv_ffn(h, w_up[l], conv_w[l], conv_b[l], w_down[l])
        x = layer_norm(DEEPNORM_ALPHA * x + (1 + g2) * y, ln2_g[l], ln2_b[l])
    return x
```

```python
from contextlib import ExitStack
import math
import numpy as np
import concourse.bass as bass
import concourse.mybir as mybir
from concourse.bass_utils import run_bass_kernel_spmd

F32 = mybir.dt.float32
BF16 = mybir.dt.bfloat16
AF = mybir.ActivationFunctionType
ALU = mybir.AluOpType
AX = mybir.AxisListType

S = 4096
D = 2048
KC = 16
DFF = 5632
NFC = 44
INC = 6154
LB = 4608
TSW = 4480
ALPHA = 4.0 ** 0.25
EPS = 1e-5
SCALE = 0.125
NCORES = 8


class R:
    __slots__ = ("name", "w", "rd")

    def __init__(self, name=""):
        self.name = name
        self.w = None
        self.rd = {}


class Prog:
    SAME = True
    NDS = 20

    def __init__(self, nc):
        self.nc = nc
        self.E = {"pe": nc.tensor, "act": nc.scalar, "dve": nc.vector,
                  "pool": nc.gpsimd, "sp": nc.sync}
        self.sem = {}
        self.cnt = {}
        for e in self.E:
            self.sem[e] = nc.alloc_semaphore("s_" + e)
            self.cnt[e] = 0
        self.waited = {e: {} for e in self.E}
        self.dsem = {}
        self.dcnt = {}
        self.dnext = {}
        for e in ("sp", "pool"):
            self.dsem[e] = [nc.alloc_semaphore("d_%s%d" % (e, i)) for i in range(self.NDS)]
            self.dcnt[e] = [0] * self.NDS
            self.dnext[e] = 0
        self.n_ins = 0

    def _wait(self, eng, toks):
        best = {}
        wd = self.waited[eng]
        for (k, h, v) in toks:
            if k == eng and (eng == "pe" or not self.SAME):
                continue
            if wd.get(k, 0) >= v:
                continue
            if k not in best or best[k][1] < v:
                best[k] = (h, v)
        for k, (h, v) in best.items():
            self.E[eng].wait_ge(h, v)
            wd[k] = v
            self.n_ins += 1

    def _deps(self, eng, reads, writes, extra=()):
        toks = list(extra)
        for r in reads:
            if r.w is not None:
                toks.append(r.w)
        for w in writes:
            if w.w is not None:
                toks.append(w.w)
            toks.extend(w.rd.values())
        self._wait(eng, toks)

    def _reg(self, tok, reads, writes):
        for r in reads:
            r.rd[tok[0]] = tok
        for w in writes:
            w.w = tok
            w.rd = {}

    def op(self, eng, fn, reads=(), writes=()):
        self._deps(eng, reads, writes)
        ins = fn()
        self.n_ins += 1
        self.cnt[eng] += 1
        ins.then_inc(self.sem[eng], 1)
        self._reg((eng, self.sem[eng], self.cnt[eng]), reads, writes)

    def mm(self, fns, reads=(), writes=()):
        self._deps("pe", reads, writes)
        ins = None
        for f in fns:
            ins = f()
            self.n_ins += 1
        self.cnt["pe"] += 1
        ins.then_inc(self.sem["pe"], 1)
        self._reg(("pe", self.sem["pe"], self.cnt["pe"]), reads, writes)

    def dma(self, eng, out, in_, reads=(), writes=()):
        i = self.dnext[eng]
        self.dnext[eng] = (i + 1) % self.NDS
        key = "d_%s%d" % (eng, i)
        extra = []
        if self.dcnt[eng][i]:
            extra.append((key, self.dsem[eng][i], self.dcnt[eng][i]))
        self._deps(eng, reads, writes, extra)
        ins = self.E[eng].dma_start(out=out, in_=in_)
        self.n_ins += 1
        self.dcnt[eng][i] += 16
        ins.then_inc(self.dsem[eng][i], 16)
        self._reg((key, self.dsem[eng][i], self.dcnt[eng][i]), reads, writes)

    def barrier(self):
        toks = []
        for e in self.E:
            if self.cnt[e]:
                toks.append((e, self.sem[e], self.cnt[e]))
        for e in self.dsem:
            for i in range(self.NDS):
                if self.dcnt[e][i]:
                    toks.append(("d_%s%d" % (e, i), self.dsem[e][i], self.dcnt[e][i]))
        same = self.SAME
        self.SAME = False
        for e in self.E:
            self._wait(e, toks)
        self.SAME = same


def t5_bucket_np(d):
    n = np.maximum(d, 0)
    nf = np.maximum(n, 1).astype(np.float32)
    large = 16 + (np.log(nf / np.float32(16)) / np.float32(math.log(2048 / 16)) * np.float32(16)).astype(np.int32)
    large = np.minimum(large, 31)
    return np.where(n < 16, n, large)


def host_constants():
    c = {}
    c["ident"] = np.eye(128, dtype=np.float32)
    i = np.arange(LB)
    d = i - 512
    bk = t5_bucket_np(d)
    oh = np.zeros((32, LB), np.float32)
    oh[bk, i] = 1.0
    oh[:, d < 0] = 0.0
    c["oh"] = oh
    cb = (d >= 0).astype(np.float32)
    ca = (((d >= 0) & (d <= 128)).astype(np.float32)
          + ((d >= 0) & (d <= 512) & (d % 4 == 0)).astype(np.float32)
          + ((d >= 0) & (d <= 2048) & (d % 16 == 0)).astype(np.float32))
    c["cmA"] = np.tile(ca[None, :], (128, 1)).astype(np.float32)
    c["cmB"] = np.tile(cb[None, :], (128, 1)).astype(np.float32)
    b1 = np.zeros((16, S), np.float32)
    for n in range(16):
        b1[n, n * 256:(n + 1) * 256] = 1.0
    c["blk1h"] = b1
    pn = np.zeros((128, 32, 16), np.float32)
    of = np.zeros((128, 32, 16), np.float32)
    for tt in range(32):
        qblk = tt // 2
        pn[:, tt, qblk:] = -1e30
        of[:, tt, qblk] = 1.0
    c["pastneg"] = pn.reshape(128, 512)
    c["ownfix"] = of.reshape(128, 512)
    cn = np.zeros((128, 4, 512), np.float32)
    p_ = np.arange(128)[:, None]
    j_ = np.arange(512)[None, :]
    for ii in range(4):
        cn[:, ii, :] = np.where(ii * 128 + p_ <= j_, 0.0, -1e9)
    c["cneg"] = cn.reshape(128, 2048)
    return c


def build(dbg=()):
    nc = bass.Bass("TRN2", target_bir_lowering=False)
    p = Prog(nc)

    def din(name, shape):
        return nc.dram_tensor(name, list(shape), F32, kind="ExternalInput").ap()

    def scratch(name, shape, dt):
        kind = "ExternalOutput" if name in dbg else "Internal"
        return nc.dram_tensor(name, list(shape), dt, kind=kind).ap()

    x_in = din("x", [S, D])
    cT = din("cT", [128, KC])
    w_ada = [din("w_ada%d" % l, [D, 6 * D]) for l in range(2)]
    bada = [din("bada%d" % l, [128, 96]) for l in range(2)]
    w_in = [din("w_in%d" % l, [D, INC]) for l in range(2)]
    bf_in = din("bf", [10, 2])
    gmix_in = din("gmix", [128, 32])
    w_out = [din("w_out%d" % l, [D, D]) for l in range(2)]
    lnp_in = din("lnp", [128, 128])
    w_up = [din("w_up%d" % l, [D, 2 * DFF]) for l in range(2)]
    convw_in = din("convw", [128, 2 * 3 * 88])
    convb_in = din("convb", [128, 2 * 88])
    w_down = [din("w_down%d" % l, [DFF, D]) for l in range(2)]
    relrep_in = din("relrep", [32, 22 * 128])
    ident_in = din("ident", [128, 128])
    oh_in = din("oh", [32, LB])
    cmA_in = din("cmA", [128, LB])
    cmB_in = din("cmB", [128, LB])
    blk1h_in = din("blk1h", [16, S])
    pastneg_in = din("pastneg", [128, 512])
    ownfix_in = din("ownfix", [128, 512])
    cneg_in = din("cneg", [128, 2048])
    out = nc.dram_tensor("out", [S, D], F32, kind="ExternalOutput").ap()

    XT = scratch("XT", [D, S], F32)
    X1T = scratch("X1T", [D, S], F32)
    PT = scratch("PT", [6144, S], BF16)
    VTM = scratch("VTM", [S, D], BF16)
    OT = scratch("OT", [D, S], F32)
    GT = scratch("GT", [DFF, S], BF16)
    FB = scratch("FB", [22, 128 * LB], F32)
    CUMQ = scratch("CUMQ", [10, S], BF16)
    CUMK = scratch("CUMK", [128, 320], F32)
    WD16 = scratch("WD16", [16, 128, NFC, 128], BF16)
    WO16 = scratch("WO16", [16, 128, KC, 128], BF16)
    MODD = scratch("MODD", [2, 128, 96], F32)

    uid = [0]

    class Phase:
        def __init__(self):
            self.st = ExitStack()

        def sb(self, name, shape, dt):
            uid[0] += 1
            return self.st.enter_context(nc.sbuf_tensor("s%d_%s" % (uid[0], name), list(shape), dt))

        def ps(self, name):
            uid[0] += 1
            return self.st.enter_context(nc.psum_tensor("p%d_%s" % (uid[0], name), [128, 512], F32))

        def close(self):
            p.barrier()
            self.st.close()

    V = nc.vector
    A = nc.scalar
    G = nc.gpsimd
    T = nc.tensor

    def evac(i, out_, in_, reads, writes):
        if i % 2 == 0:
            p.op("act", lambda: A.copy(out_, in_), reads, writes)
        else:
            p.op("dve", lambda: V.tensor_copy(out_, in_), reads, writes)

    gph = Phase()
    ident = gph.sb("ident", [128, 128], F32)
    r_c = R("consts")
    ones_bf = gph.sb("ones_bf", [128, 128], BF16)
    ones_f = gph.sb("ones_f", [128, 64], F32)
    mhalf = gph.sb("mhalf", [128, 512], F32)
    modT = gph.sb("modT", [128, 192], F32)
    gmix = gph.sb("gmix", [128, 32], F32)
    lnp = gph.sb("lnp", [128, 128], F32)
    convw = gph.sb("convw", [128, 528], F32)
    convb = gph.sb("convb", [128, 176], F32)
    bfs = gph.sb("bfs", [10, 2], F32)
    p.dma("sp", ident[:], ident_in, writes=[r_c])
    p.dma("sp", gmix[:], gmix_in, writes=[r_c])
    p.dma("sp", lnp[:], lnp_in, writes=[r_c])
    p.dma("sp", convw[:], convw_in, writes=[r_c])
    p.dma("sp", convb[:], convb_in, writes=[r_c])
    p.dma("sp", bfs[:], bf_in, writes=[r_c])
    p.op("dve", lambda: V.memset(ones_bf[:], 1.0), writes=[r_c])
    p.op("dve", lambda: V.memset(ones_f[:], 1.0), writes=[r_c])
    p.op("dve", lambda: V.memset(mhalf[:], -0.5), writes=[r_c])
    p.barrier()

    def phase_T():
        ph = Phase()
        xs = [ph.sb("xs%d" % i, [128, 4, D], F32) for i in range(2)]
        xT = [ph.sb("xTs%d" % i, [128, KC, 512], F32) for i in range(2)]
        r_xs = [R(), R()]
        r_xT = [R(), R()]
        ps = [ph.ps("psT%d" % i) for i in range(4)]
        r_ps = [R() for _ in range(4)]
        for tb in range(8):
            i = tb % 2
            p.dma("sp", xs[i][:], x_in[tb * 512:(tb + 1) * 512, :].rearrange("(t q) d -> q t d", q=128),
                  writes=[r_xs[i]])
            for kc in range(KC):
                b = kc % 4
                p.mm([(lambda t=t: T.transpose(ps[b][:, t * 128:(t + 1) * 128],
                                              xs[i][:, t, kc * 128:(kc + 1) * 128], ident[:]))
                      for t in range(4)], reads=[r_xs[i], r_c], writes=[r_ps[b]])
                evac(kc, xT[i][:, kc, :], ps[b][:], [r_ps[b]], [r_xT[i]])
            p.dma("sp", XT.rearrange("(kc q) t -> q kc t", q=128)[:, :, tb * 512:(tb + 1) * 512], xT[i][:],
                  reads=[r_xT[i]])
        ph.close()

    def phase_M():
        ph = Phase()
        cond = ph.sb("cond", [128, KC], F32)
        bad = ph.sb("bad", [128, 192], F32)
        wa = [ph.sb("wa%d" % i, [128, KC, 512], F32) for i in range(2)]
        r_wa = [R(), R()]
        r_cond = R()
        r_mod = R()
        mps = ph.ps("mps")
        r_mps = R()
        p.dma("sp", cond[:], cT, writes=[r_cond])
        p.dma("sp", bad[:, 0:96], bada[0], writes=[r_cond])
        p.dma("sp", bad[:, 96:192], bada[1], writes=[r_cond])
        p.op("act", lambda: A.activation(out=cond[:], in_=cond[:], func=AF.Silu), reads=[r_cond], writes=[r_cond])
        n = 0
        for l in range(2):
            for jb in range(24):
                i = n % 2
                n += 1
                p.dma("sp", wa[i][:], w_ada[l][:, jb * 512:(jb + 1) * 512].rearrange("(kc q) c -> q kc c", q=128),
                      writes=[r_wa[i]])
                for j4 in range(4):
                    col = jb * 4 + j4
                    p.mm([(lambda kc=kc: T.matmul(mps[:, col:col + 1], wa[i][:, kc, j4 * 128:(j4 + 1) * 128],
                                                  cond[:, kc:kc + 1], start=(kc == 0), stop=(kc == KC - 1)))
                          for kc in range(KC)], reads=[r_wa[i], r_cond], writes=[r_mps])
            o = l * 96
            p.op("dve", lambda: V.tensor_tensor(out=modT[:, o:o + 96], in0=mps[:, 0:96], in1=bad[:, o:o + 96],
                                                op=ALU.add), reads=[r_mps, r_cond], writes=[r_mod])
            for (a, sc) in ((16, 1.0), (32, 1.0 / ALPHA), (64, 1.0), (80, 1.0 / ALPHA)):
                p.op("dve", lambda: V.tensor_scalar(out=modT[:, o + a:o + a + 16], in0=modT[:, o + a:o + a + 16],
                                                    scalar1=1.0, scalar2=sc, op0=ALU.add, op1=ALU.mult),
                     reads=[r_mod], writes=[r_mod])
        if "MODD" in dbg:
            p.dma("sp", MODD.rearrange("l q c -> q l c"), modT[:].rearrange("q (l c) -> q l c", l=2), reads=[r_mod])
        ph.close()

    def mod(l, which, c):
        base = {"sh1": 0, "sc1": 16, "g1": 32, "sh2": 48, "sc2": 64, "g2": 80}[which]
        return modT[:, l * 96 + base + c: l * 96 + base + c + 1]

    def phase_B():
        ph = Phase()
        oh = ph.sb("oh", [32, LB], F32)
        cmA = ph.sb("cmA", [128, LB], F32)
        cmB = ph.sb("cmB", [128, LB], F32)
        rel = ph.sb("rel", [32, 22 * 128], F32)
        gs = [ph.sb("gs%d" % i, [128, LB], F32) for i in range(2)]
        r_g = [R(), R()]
        r_k = R()
        ps = [ph.ps("psB%d" % i) for i in range(4)]
        r_ps = [R() for _ in range(4)]
        p.dma("sp", oh[:], oh_in, writes=[r_k])
        p.dma("sp", cmA[:], cmA_in, writes=[r_k])
        p.dma("sp", cmB[:], cmB_in, writes=[r_k])
        p.dma("sp", rel[:], relrep_in, writes=[r_k])
        n = 0
        for h in range(22):
            i = h % 2
            for blk in range(9):
                b = n % 4
                n += 1
                p.mm([lambda: T.matmul(ps[b][:], rel[:, h * 128:(h + 1) * 128], oh[:, blk * 512:(blk + 1) * 512],
                                       start=True, stop=True)], reads=[r_k], writes=[r_ps[b]])
                p.op("act", lambda: A.activation(out=gs[i][:, blk * 512:(blk + 1) * 512], in_=ps[b][:], func=AF.Exp),
                     reads=[r_ps[b]], writes=[r_g[i]])
            cm = cmA if h < 12 else cmB
            p.op("dve", lambda: V.tensor_tensor(out=gs[i][:], in0=gs[i][:], in1=cm[:], op=ALU.mult),
                 reads=[r_g[i], r_k], writes=[r_g[i]])
            p.dma("sp", FB[h].rearrange("(q c) -> q c", q=128), gs[i][:], reads=[r_g[i]])
        ph.close()

    def phase_W(l):
        ph = Phase()
        wt = [ph.sb("wt%d" % i, [128, 4, D], BF16) for i in range(2)]
        r_wt = [R(), R()]
        n = 0
        for (src, dst, ng) in ((w_down[l], WD16, 11), (w_out[l], WO16, 4)):
            dv = dst.rearrange("j q kc c -> q kc j c")
            for kg in range(ng):
                i = n % 2
                n += 1
                p.dma("pool", wt[i][:], src[kg * 512:(kg + 1) * 512, :].rearrange("(kc q) n -> q kc n", q=128),
                      writes=[r_wt[i]])
                for k4 in range(4):
                    p.dma("sp", dv[:, kg * 4 + k4, :, :], wt[i][:, k4, :].rearrange("q (j c) -> q j c", c=128),
                          reads=[r_wt[i]])
        ph.close()

    def build_hT(ph, hT, r_hT, src, l, sc, sh):
        xin = [ph.sb("xin%d" % i, [128, KC, 512], F32) for i in range(2)]
        r_xin = [R(), R()]
        sv = src.rearrange("(kc q) t -> q kc t", q=128)
        for tb in range(8):
            i = tb % 2
            p.dma("sp", xin[i][:], sv[:, :, tb * 512:(tb + 1) * 512], writes=[r_xin[i]])
            for kc in range(KC):
                o_ = hT[:, kc, tb * 512:(tb + 1) * 512]
                if kc % 2 == 0:
                    p.op("act", lambda: A.activation(out=o_, in_=xin[i][:, kc, :], func=AF.Identity,
                                                     scale=mod(l, sc, kc), bias=mod(l, sh, kc)),
                         reads=[r_xin[i]], writes=[r_hT[tb]])
                else:
                    p.op("dve", lambda: V.tensor_scalar(out=o_, in0=xin[i][:, kc, :], scalar1=mod(l, sc, kc),
                                                        scalar2=mod(l, sh, kc), op0=ALU.mult, op1=ALU.add),
                         reads=[r_xin[i]], writes=[r_hT[tb]])

    VRANGES = ((1536, 2304, 0), (3584, 4224, 768), (5504, 6144, 1408))

    def vcol_of(col0):
        for (a, b, base) in VRANGES:
            if a <= col0 < b:
                return base + (col0 - a)
        return None

    def phase_QKV(l, src):
        oph = Phase()
        hT = oph.sb("hT", [128, KC, S], BF16)
        r_hT = [R() for _ in range(8)]
        ph = Phase()
        build_hT(ph, hT, r_hT, src, l, "sc1", "sh1")
        ph.close()
        ph = Phase()
        wb = [ph.sb("wb%d" % i, [128, KC, 512], BF16) for i in range(2)]
        r_wb = [R(), R()]
        fst = [ph.sb("fst%d" % i, [128, S], BF16) for i in range(2)]
        r_fst = [R(), R()]
        vst = [ph.sb("vst%d" % i, [128, 32, 128], BF16) for i in range(2)]
        r_vst = [R(), R()]
        ps = [ph.ps("psQ%d" % i) for i in range(6)]
        r_ps = [R() for _ in range(6)]
        npz = 0
        nf = 0
        nv = 0
        for g in range(12):
            i = g % 2
            p.dma("pool", wb[i][:], w_in[l][:, g * 512:(g + 1) * 512].rearrange("(kc q) c -> q kc c", q=128),
                  writes=[r_wb[i]])
            for jj in range(4):
                col0 = (g * 4 + jj) * 128
                vcol = vcol_of(col0)
                wsl = lambda kc: wb[i][:, kc, jj * 128:(jj + 1) * 128]
                if vcol is None:
                    fi = nf % 2
                    nf += 1
                    for tb in range(8):
                        b = npz % 6
                        npz += 1
                        p.mm([(lambda kc=kc: T.matmul(ps[b][:], wsl(kc), hT[:, kc, tb * 512:(tb + 1) * 512],
                                                      start=(kc == 0), stop=(kc == KC - 1))) for kc in range(KC)],
                             reads=[r_wb[i], r_hT[tb]], writes=[r_ps[b]])
                        evac(npz, fst[fi][:, tb * 512:(tb + 1) * 512], ps[b][:], [r_ps[b]], [r_fst[fi]])
                    p.dma("sp", PT[col0:col0 + 128, :], fst[fi][:], reads=[r_fst[fi]])
                else:
                    vi = nv % 2
                    nv += 1
                    for tt in range(32):
                        b = npz % 6
                        npz += 1
                        p.mm([(lambda kc=kc: T.matmul(ps[b][:, 0:128], hT[:, kc, tt * 128:(tt + 1) * 128], wsl(kc),
                                                      start=(kc == 0), stop=(kc == KC - 1))) for kc in range(KC)],
                             reads=[r_wb[i], r_hT[tt // 4]], writes=[r_ps[b]])
                        evac(npz, vst[vi][:, tt, :], ps[b][:, 0:128], [r_ps[b]], [r_vst[vi]])
                    vv = VTM.rearrange("(tt q) c -> q tt c", q=128)
                    for q4 in range(4):
                        p.dma("sp", vv[:, q4 * 8:(q4 + 1) * 8, vcol:vcol + 128], vst[vi][:, q4 * 8:(q4 + 1) * 8, :],
                              reads=[r_vst[vi]])
        ph.close()
        ph = Phase()
        wtail = ph.sb("wtail", [128, KC, 10], BF16)
        r_wtail = R()
        fT = ph.sb("fT", [32, S], F32)
        e2 = ph.sb("e2", [10, S], F32)
        cq = ph.sb("cq", [10, S], BF16)
        ck = ph.sb("ck", [128, 320], F32)
        r_f = R()
        r_e2 = R()
        r_cq = R()
        r_ck = R()
        ps = [ph.ps("psF%d" % i) for i in range(6)]
        r_ps = [R() for _ in range(6)]
        p.dma("pool", wtail[:], w_in[l][:, 6144:6154].rearrange("(kc q) c -> q kc c", q=128), writes=[r_wtail])
        p.op("pool", lambda: G.memset(fT[:], 0.0), writes=[r_f])
        for tb in range(8):
            b = npz % 6
            npz += 1
            p.mm([(lambda kc=kc: T.matmul(ps[b][0:10, :], wtail[:, kc, :], hT[:, kc, tb * 512:(tb + 1) * 512],
                                          start=(kc == 0), stop=(kc == KC - 1))) for kc in range(KC)],
                 reads=[r_wtail, r_hT[tb]], writes=[r_ps[b]])
            p.op("act", lambda: A.activation(out=fT[0:10, tb * 512:(tb + 1) * 512], in_=ps[b][0:10, :],
                                             func=AF.Identity, bias=bfs[0:10, l:l + 1]),
                 reads=[r_ps[b], r_c], writes=[r_f])
        p.op("act", lambda: A.activation(out=e2[0:10, :], in_=fT[0:10, :], func=AF.Exp, scale=-1.0),
             reads=[r_f], writes=[r_e2])
        p.op("act", lambda: A.activation(out=e2[0:10, :], in_=e2[0:10, :], func=AF.Ln, bias=1.0),
             reads=[r_e2], writes=[r_e2])
        p.op("dve", lambda: V.tensor_tensor_scan(out=fT[0:10, :], data0=e2[0:10, :], data1=e2[0:10, :], initial=0.0,
                                                 op0=ALU.add, op1=ALU.bypass), reads=[r_e2, r_f], writes=[r_f])
        p.op("act", lambda: A.activation(out=cq[0:10, :], in_=fT[0:10, :], func=AF.Identity, scale=-1.0 / SCALE),
             reads=[r_f], writes=[r_cq])
        p.dma("sp", CUMQ, cq[0:10, :], reads=[r_cq])
        b = npz % 6
        p.mm([(lambda tt=tt: T.matmul(ps[b][:, tt * 10:(tt + 1) * 10], fT[0:32, tt * 128:(tt + 1) * 128],
                                      ident[0:32, 0:10], start=True, stop=True)) for tt in range(32)],
             reads=[r_f, r_c], writes=[r_ps[b]])
        p.op("dve", lambda: V.tensor_copy(ck[:], ps[b][:, 0:320]), reads=[r_ps[b]], writes=[r_ck])
        p.dma("sp", CUMK, ck[:], reads=[r_ck])
        ph.close()
        oph.close()

    def phase_ATT(l, heads=range(32)):
        ph = Phase()
        QA = [ph.sb("QA%d" % i, [128, S], BF16) for i in range(2)]
        KA = [ph.sb("KA%d" % i, [128, S], BF16) for i in range(2)]
        VA = [ph.sb("VA%d" % i, [128, 32, 65], BF16) for i in range(2)]
        TS = [ph.sb("TS%d" % i, [128, TSW], F32) for i in range(2)]
        OST = [ph.sb("OST%d" % i, [64, S], F32) for i in range(2)]
        ET = [ph.sb("ET%d" % i, [128, 512], F32) for i in range(3)]
        PTL = [ph.sb("PTL%d" % i, [128, 512], BF16) for i in range(4)]
        cneg = ph.sb("cneg", [128, 2048], F32)
        pastneg = ph.sb("pastneg", [128, 512], F32)
        ownfix = ph.sb("ownfix", [128, 512], F32)
        ck = ph.sb("ck", [128, 320], F32)
        gate = ph.sb("gate", [128, 512], F32)
        top8 = ph.sb("top8", [128, 256], F32)
        sel = ph.sb("sel", [128, 512], F32)
        selw = ph.sb("selw", [128, 32 * 80], F32)
        km = ph.sb("km", [128, 16], F32)
        kmh = ph.sb("kmh", [128, 16], BF16)
        kml = ph.sb("kml", [128, 16], BF16)
        rden = ph.sb("rden", [128, 512], F32)
        osb = ph.sb("osb", [64, 512], F32)
        r_QA = [R(), R()]
        r_KA = [R(), R()]
        r_VA = [R(), R()]
        r_TS = [R(), R()]
        r_OST = [R(), R()]
        r_ET = [R() for _ in range(3)]
        r_PTL = [R() for _ in range(4)]
        r_k = R()
        r_gate = R()
        r_top8 = R()
        r_sel = R()
        r_km = R()
        r_rden = R()
        r_osb = R()
        PSS = [ph.ps("pss%d" % i) for i in range(3)]
        r_PSS = [R() for _ in range(3)]
        PSO = [ph.ps("pso%d" % i) for i in range(2)]
        r_PSO = [R(), R()]
        PSB = ph.ps("psb")
        r_PSB = R()
        PSG = ph.ps("psg")
        r_PSG = R()
        PSX = ph.ps("psx")
        r_PSX = R()
        p.dma("sp", cneg[:], cneg_in, writes=[r_k])
        p.dma("sp", pastneg[:], pastneg_in, writes=[r_k])
        p.dma("sp", ownfix[:], ownfix_in, writes=[r_k])
        p.dma("sp", ck[:], CUMK, writes=[r_k])
        bsel = ph.sb("bsel", [128, 64], F32)
        p.op("pool", lambda: G.memset(bsel[:], 0.0), writes=[r_k])
        p.op("pool", lambda: G.memset(bsel[64:65, :], 1.0), writes=[r_k])
        p.op("pool", lambda: G.memset(rden[:], 0.0), writes=[r_rden])
        p.op("pool", lambda: G.memset(selw[:], 0.0), writes=[r_sel])
        for i in range(2):
            p.op("pool", lambda: G.memset(VA[i][:, :, 64:65], 1.0), writes=[r_VA[i]])
            p.op("pool", lambda: G.memset(QA[i][:], 0.0), writes=[r_QA[i]])
            p.op("pool", lambda: G.memset(KA[i][:], 0.0), writes=[r_KA[i]])

        def hinfo(hg):
            if hg < 12:
                return ("A", hg * 64, 768 + hg * 64, hg * 64, hg)
            if hg < 22:
                hb = hg - 12
                return ("B", 2304 + hb * 64, 2944 + hb * 64, 768 + hb * 64, hg)
            hc = hg - 22
            return ("C", 4224 + hc * 64, 4864 + hc * 64, 1408 + hc * 64, hc)

        def load(hg, i):
            typ, q0, k0, v0, ridx = hinfo(hg)
            if True:
                p.dma("sp", QA[i][0:64, :], PT[q0:q0 + 64, :], writes=[r_QA[i]])
                p.dma("sp", KA[i][0:64, :], PT[k0:k0 + 64, :], writes=[r_KA[i]])
                if typ == "B":
                    p.op("pool", lambda: G.memset(KA[i][64:96, :], 0.0), writes=[r_KA[i]])
                    p.dma("pool", KA[i][64:80, :], blk1h_in, writes=[r_KA[i]])
                if typ == "C":
                    p.dma("sp", QA[i][64:65, :], CUMQ[ridx:ridx + 1, :], writes=[r_QA[i]])
                    p.op("pool", lambda: G.memset(KA[i][64:96, :], 0.0), writes=[r_KA[i]])
                    p.op("pool", lambda: G.memset(KA[i][64:65, :], 1.0), writes=[r_KA[i]])
            vv = VTM.rearrange("(tt q) c -> q tt c", q=128)
            for q4 in range(4):
                p.dma("sp", VA[i][:, q4 * 8:(q4 + 1) * 8, 0:64], vv[:, q4 * 8:(q4 + 1) * 8, v0:v0 + 64],
                      writes=[r_VA[i]])
            if typ != "C":
                src = bass.AP(tensor=FB.tensor, offset=ridx * 128 * LB + 128, ap=[[LB - 1, 128], [1, TSW]])
                p.dma("sp", TS[i][:], src, writes=[r_TS[i]])

        hl = list(heads)
        load(hl[0], 0)
        cnt = 0
        nqb = 0
        for n_h, hg in enumerate(hl):
            i = n_h % 2
            typ, q0, k0, v0, ridx = hinfo(hg)
            if n_h + 1 < len(hl):
                load(hl[n_h + 1], (n_h + 1) % 2)
            K = {"A": 64, "B": 96, "C": 96}[typ]
            if typ == "B":
                p.op("dve", lambda: V.tensor_reduce(out=km[0:64, :],
                                                    in_=KA[i][0:64, :].rearrange("q (n s) -> q n s", s=256),
                                                    axis=AX.X, op=ALU.add), reads=[r_KA[i]], writes=[r_km])
                p.op("dve", lambda: V.tensor_copy(kmh[0:64, :], km[0:64, :]), reads=[r_km], writes=[r_km])
                p.op("dve", lambda: V.tensor_tensor(out=kml[0:64, :], in0=km[0:64, :], in1=kmh[0:64, :],
                                                    op=ALU.subtract), reads=[r_km], writes=[r_km])
                fns = []
                for tt in range(32):
                    fns.append(lambda tt=tt: T.matmul(PSG[:, tt * 16:(tt + 1) * 16],
                                                      QA[i][0:64, tt * 128:(tt + 1) * 128], kmh[0:64, :],
                                                      start=True, stop=False))
                    fns.append(lambda tt=tt: T.matmul(PSG[:, tt * 16:(tt + 1) * 16],
                                                      QA[i][0:64, tt * 128:(tt + 1) * 128], kml[0:64, :],
                                                      start=False, stop=True))
                p.mm(fns, reads=[r_QA[i], r_km], writes=[r_PSG])
                p.op("dve", lambda: V.tensor_tensor(out=gate[:], in0=PSG[:], in1=pastneg[:], op=ALU.add),
                     reads=[r_PSG, r_k], writes=[r_gate])
                for tt in range(32):
                    p.op("dve", lambda: V.max(out=top8[:, tt * 8:(tt + 1) * 8], in_=gate[:, tt * 16:(tt + 1) * 16]),
                         reads=[r_gate], writes=[r_top8])
                thr = top8[:].rearrange("q (t e) -> q t e", e=8)[:, :, 2:3].to_broadcast([128, 32, 16])
                g3 = gate[:].rearrange("q (t e) -> q t e", e=16)
                s3 = sel[:].rearrange("q (t e) -> q t e", e=16)
                p.op("dve", lambda: V.tensor_tensor(out=s3, in0=g3, in1=thr, op=ALU.is_ge),
                     reads=[r_gate, r_top8], writes=[r_sel])
                p.op("dve", lambda: V.tensor_tensor(out=sel[:], in0=sel[:], in1=ownfix[:], op=ALU.max),
                     reads=[r_sel, r_k], writes=[r_sel])
                p.op("dve", lambda: V.tensor_scalar(out=selw[:].rearrange("q (t e) -> q t e", e=80)[:, :, 64:80],
                                                    in0=s3, scalar1=-1.0, scalar2=30000.0,
                                                    op0=ALU.add, op1=ALU.mult), reads=[r_sel], writes=[r_sel])
                for g8 in range(8):
                    p.mm([(lambda t=t: T.transpose(PSX[0:80, t * 128:(t + 1) * 128],
                                                   selw[:, (g8 * 4 + t) * 80:(g8 * 4 + t + 1) * 80], ident[:]))
                          for t in range(4)], reads=[r_sel, r_c], writes=[r_PSX])
                    p.op("act", lambda: A.copy(QA[i][64:80, g8 * 512:(g8 + 1) * 512], PSX[64:80, :]),
                         reads=[r_PSX], writes=[r_QA[i]])
            oi = n_h % 2
            for qb in range(8):
                kt_lo = max(0, 4 * qb - 16) if typ == "A" else 0
                kts = list(range(kt_lo, 4 * qb + 4))
                po = nqb % 2
                nqb += 1
                for n, kt in enumerate(kts):
                    sb_ = cnt % 3
                    pb = cnt % 4
                    cnt += 1
                    p.mm([lambda: T.matmul(PSS[sb_][:], KA[i][0:K, kt * 128:(kt + 1) * 128],
                                           QA[i][0:K, qb * 512:(qb + 1) * 512], start=True, stop=True)],
                         reads=[r_KA[i], r_QA[i]], writes=[r_PSS[sb_]])
                    if typ != "C":
                        off = qb * 512 - kt * 128 + 384
                        p.op("act", lambda: A.activation(out=ET[sb_][:], in_=PSS[sb_][:], func=AF.Exp, scale=SCALE),
                             reads=[r_PSS[sb_]], writes=[r_ET[sb_]])
                        p.op("dve", lambda: V.tensor_tensor(out=PTL[pb][:], in0=ET[sb_][:],
                                                            in1=TS[i][:, off:off + 512], op=ALU.mult),
                             reads=[r_ET[sb_], r_TS[i]], writes=[r_PTL[pb]])
                    else:
                        bias = ck[:, kt * 10 + ridx:kt * 10 + ridx + 1]
                        if kt >= 4 * qb:
                            dg = kt - 4 * qb
                            p.op("dve", lambda: V.tensor_tensor(out=ET[sb_][:], in0=PSS[sb_][:],
                                                                in1=cneg[:, dg * 512:(dg + 1) * 512], op=ALU.add),
                                 reads=[r_PSS[sb_], r_k], writes=[r_ET[sb_]])
                            p.op("act", lambda: A.activation(out=PTL[pb][:], in_=ET[sb_][:], func=AF.Exp,
                                                             scale=SCALE, bias=bias),
                                 reads=[r_ET[sb_], r_k], writes=[r_PTL[pb]])
                        else:
                            p.op("act", lambda: A.activation(out=PTL[pb][:], in_=PSS[sb_][:], func=AF.Exp,
                                                             scale=SCALE, bias=bias),
                                 reads=[r_PSS[sb_], r_k], writes=[r_PTL[pb]])
                    p.mm([lambda: T.matmul(PSO[po][0:65, :], VA[i][:, kt, :], PTL[pb][:],
                                           start=(n == 0), stop=(n == len(kts) - 1))],
                         reads=[r_VA[i], r_PTL[pb]], writes=[r_PSO[po]])
                p.op("dve", lambda: V.reciprocal(rden[64:65, :], PSO[po][64:65, :]), reads=[r_PSO[po]], writes=[r_rden])
                p.mm([lambda: T.matmul(PSB[0:64, :], bsel[64:96, 0:64], rden[64:96, :], start=True, stop=True)],
                     reads=[r_rden, r_k], writes=[r_PSB])
                p.op("act", lambda: A.copy(osb[0:64, :], PSO[po][0:64, :]), reads=[r_PSO[po]], writes=[r_osb])
                p.op("dve", lambda: V.tensor_tensor(out=OST[oi][0:64, qb * 512:(qb + 1) * 512], in0=osb[0:64, :],
                                                    in1=PSB[0:64, :], op=ALU.mult),
                     reads=[r_osb, r_PSB], writes=[r_OST[oi]])
            p.dma("sp", OT[hg * 64:(hg + 1) * 64, :], OST[oi][0:64, :], reads=[r_OST[oi]])
        ph.close()

    def ln_block(ph_bufs, tb, l, which_g, lng_off, lnb_off, psm_get, resid_src, dst, final):
        (rT, xc, rbf, sqb, mean, msq, var, rstd, t1, xo, PS1, PS2, PST, osbig,
         r_rT, r_xc, r_rbf, r_sqb, r_st, r_t1, r_xo, r_PS1, r_PS2, r_PST, r_osbig) = ph_bufs
        for j in range(KC):
            jj = j % 2
            p.dma("sp", xc[jj][:], resid_src[j * 128:(j + 1) * 128, tb * 512:(tb + 1) * 512], writes=[r_xc[jj]])
            psm, r_psm = psm_get(j)
            p.op("dve", lambda: V.scalar_tensor_tensor(out=rT[:, j, :], in0=psm[:], scalar=mod(l, which_g, j),
                                                       in1=xc[jj][:], op0=ALU.mult, op1=ALU.add),
                 reads=[r_psm, r_xc[jj]], writes=[r_rT[j]])
            p.op("pool", lambda: G.tensor_copy(rbf[jj][:], rT[:, j, :]), reads=[r_rT[j]], writes=[r_rbf[jj]])
            p.op("act", lambda: A.activation(out=sqb[jj][:], in_=rT[:, j, :], func=AF.Square),
                 reads=[r_rT[j]], writes=[r_sqb[jj]])
            p.mm([lambda: T.matmul(PS1[:], ones_bf[:], rbf[jj][:], start=(j == 0), stop=(j == KC - 1))],
                 reads=[r_rbf[jj], r_c], writes=[r_PS1])
            p.mm([lambda: T.matmul(PS2[:], ones_bf[:], sqb[jj][:], start=(j == 0), stop=(j == KC - 1))],
                 reads=[r_sqb[jj], r_c], writes=[r_PS2])
        p.op("act", lambda: A.activation(out=mean[:], in_=PS1[:], func=AF.Identity, scale=1.0 / D),
             reads=[r_PS1], writes=[r_st])
        p.op("dve", lambda: V.tensor_tensor(out=msq[:], in0=mean[:], in1=mean[:], op=ALU.mult),
             reads=[r_st], writes=[r_st])
        p.op("dve", lambda: V.scalar_tensor_tensor(out=var[:], in0=PS2[:], scalar=1.0 / D, in1=msq[:],
                                                   op0=ALU.mult, op1=ALU.subtract), reads=[r_PS2, r_st], writes=[r_st])
        p.op("dve", lambda: V.tensor_scalar(out=var[:], in0=var[:], scalar1=EPS / (ALPHA * ALPHA), scalar2=None,
                                            op0=ALU.add), reads=[r_st], writes=[r_st])
        p.op("pool", lambda: G.tensor_tensor(out=rstd[:], in0=var[:], in1=mhalf[:], op=ALU.pow),
             reads=[r_st, r_c], writes=[r_st])
        for j in range(KC):
            jj = j % 2
            p.op("dve", lambda: V.tensor_tensor(out=t1[jj][:], in0=rT[:, j, :], in1=mean[:], op=ALU.subtract),
                 reads=[r_rT[j], r_st], writes=[r_t1[jj]])
            p.op("pool", lambda: G.tensor_tensor(out=t1[jj][:], in0=t1[jj][:], in1=rstd[:], op=ALU.mult),
                 reads=[r_t1[jj], r_st], writes=[r_t1[jj]])
            p.op("act", lambda: A.activation(out=xo[jj][:], in_=t1[jj][:], func=AF.Identity,
                                             scale=lnp[:, lng_off + j:lng_off + j + 1],
                                             bias=lnp[:, lnb_off + j:lnb_off + j + 1]),
                 reads=[r_t1[jj], r_c], writes=[r_xo[jj]])
            if not final:
                p.dma("sp", dst[j * 128:(j + 1) * 128, tb * 512:(tb + 1) * 512], xo[jj][:], reads=[r_xo[jj]])
            else:
                pt_ = j % 2
                p.mm([(lambda t=t: T.transpose(PST[pt_][:, t * 128:(t + 1) * 128], xo[jj][:, t * 128:(t + 1) * 128],
                                               ident[:])) for t in range(4)],
                     reads=[r_xo[jj], r_c], writes=[r_PST[pt_]])
                p.op("act", lambda: A.copy(osbig[:, :, j * 128:(j + 1) * 128],
                                           PST[pt_][:].rearrange("q (t c) -> q t c", c=128)),
                     reads=[r_PST[pt_]], writes=[r_osbig])
        if final:
            p.dma("sp", out[tb * 512:(tb + 1) * 512, :].rearrange("(t q) d -> q t d", q=128), osbig[:],
                  reads=[r_osbig])

    def ln_bufs(ph, final):
        rT = ph.sb("rT", [128, KC, 512], F32)
        xc = [ph.sb("xc%d" % i, [128, 512], F32) for i in range(2)]
        rbf = [ph.sb("rbf%d" % i, [128, 512], BF16) for i in range(2)]
        sqb = [ph.sb("sqb%d" % i, [128, 512], BF16) for i in range(2)]
        mean = ph.sb("mean", [128, 512], F32)
        msq = ph.sb("msq", [128, 512], F32)
        var = ph.sb("var", [128, 512], F32)
        rstd = ph.sb("rstd", [128, 512], F32)
        t1 = [ph.sb("t1%d" % i, [128, 512], F32) for i in range(2)]
        xo = [ph.sb("xo%d" % i, [128, 512], F32) for i in range(2)]
        PS1 = ph.ps("ps1")
        PS2 = ph.ps("ps2")
        PST = [ph.ps("pst%d" % i) for i in range(2)] if final else None
        osbig = ph.sb("osbig", [128, 4, D], F32) if final else None
        return (rT, xc, rbf, sqb, mean, msq, var, rstd, t1, xo, PS1, PS2, PST, osbig,
                [R() for _ in range(KC)], [R(), R()], [R(), R()], [R(), R()], R(), [R(), R()], [R(), R()],
                R(), R(), [R(), R()], R())

    def phase_POST(l, resid_src, dst):
        ph = Phase()
        bufs = ln_bufs(ph, False)
        ob = ph.sb("ob", [128, KC, 512], F32)
        sq = [ph.sb("sq%d" % i, [128, 512], BF16) for i in range(2)]
        ysb = ph.sb("ysb", [128, KC, 512], BF16)
        rs = [ph.sb("rs%d" % i, [128, 512], F32) for i in range(3)]
        wo = [ph.sb("wo%d" % i, [128, KC, 128], BF16) for i in range(2)]
        r_ob = R()
        r_sq = [R(), R()]
        r_ysb = R()
        r_rs = [R(), R(), R()]
        r_wo = [R(), R()]
        PG = [ph.ps("pg%d" % i) for i in range(3)]
        r_PG = [R(), R(), R()]
        PM = [ph.ps("pm%d" % i) for i in range(2)]
        r_PM = [R(), R()]
        grp_of = lambda c: 0 if c < 6 else (1 if c < 11 else 2)
        gfirst = (0, 6, 11)
        glast = (5, 10, 15)
        gn = (768.0, 640.0, 640.0)
        ov = OT.rearrange("(c q) t -> q c t", q=128)
        nw = 0
        for tb in range(8):
            p.dma("sp", ob[:], ov[:, :, tb * 512:(tb + 1) * 512], writes=[r_ob])
            for c in range(KC):
                g = grp_of(c)
                p.op("act", lambda: A.activation(out=sq[c % 2][:], in_=ob[:, c, :], func=AF.Square),
                     reads=[r_ob], writes=[r_sq[c % 2]])
                p.mm([lambda: T.matmul(PG[g][:], ones_bf[:], sq[c % 2][:], start=(c == gfirst[g]),
                                       stop=(c == glast[g]))], reads=[r_sq[c % 2], r_c], writes=[r_PG[g]])
            for g in range(3):
                p.op("dve", lambda: V.tensor_scalar(out=rs[g][:], in0=PG[g][:], scalar1=1.0 / gn[g], scalar2=EPS,
                                                    op0=ALU.mult, op1=ALU.add), reads=[r_PG[g]], writes=[r_rs[g]])
                p.op("pool", lambda: G.tensor_tensor(out=rs[g][:], in0=rs[g][:], in1=mhalf[:], op=ALU.pow),
                     reads=[r_rs[g], r_c], writes=[r_rs[g]])
            for c in range(KC):
                g = grp_of(c)
                p.op("dve", lambda: V.scalar_tensor_tensor(out=ysb[:, c, :], in0=ob[:, c, :],
                                                           scalar=gmix[:, l * 16 + c:l * 16 + c + 1], in1=rs[g][:],
                                                           op0=ALU.mult, op1=ALU.mult),
                     reads=[r_ob, r_rs[g], r_c], writes=[r_ysb])

            def psm_get(j):
                nonlocal nw
                wi = nw % 2
                nw += 1
                p.dma("sp", wo[wi][:], WO16[j], writes=[r_wo[wi]])
                p.mm([(lambda kc=kc: T.matmul(PM[wi][:], wo[wi][:, kc, :], ysb[:, kc, :], start=(kc == 0),
                                              stop=(kc == KC - 1))) for kc in range(KC)],
                     reads=[r_wo[wi], r_ysb], writes=[r_PM[wi]])
                return PM[wi], r_PM[wi]

            ln_block(bufs, tb, l, "g1", l * 16, 32 + l * 16, psm_get, resid_src, dst, False)
        ph.close()

    def phase_UP(l, src):
        oph = Phase()
        hT = oph.sb("hT2", [128, KC, S], BF16)
        r_hT = [R() for _ in range(8)]
        ph = Phase()
        build_hT(ph, hT, r_hT, src, l, "sc2", "sh2")
        ph.close()
        ph = Phase()
        wa = [ph.sb("wua%d" % i, [128, KC, 256], BF16) for i in range(2)]
        wb = [ph.sb("wub%d" % i, [128, KC, 256], BF16) for i in range(2)]
        r_w = [R(), R()]
        ua = [ph.sb("ua%d" % i, [128, 514], F32) for i in range(2)]
        ub = [ph.sb("ub%d" % i, [128, 514], F32) for i in range(2)]
        r_ua = [R(), R()]
        r_ub = [R(), R()]
        ta = [ph.sb("ta%d" % i, [128, 512], F32) for i in range(2)]
        tb_ = [ph.sb("tb%d" % i, [128, 512], F32) for i in range(2)]
        r_ta = [R(), R()]
        r_tb = [R(), R()]
        gt = [ph.sb("gt%d" % i, [128, S], BF16) for i in range(2)]
        r_gt = [R(), R()]
        PA = [ph.ps("pa%d" % i) for i in range(3)]
        PB = [ph.ps("pb%d" % i) for i in range(3)]
        r_PA = [R() for _ in range(3)]
        r_PB = [R() for _ in range(3)]
        cw = lambda tap, ci: convw[:, l * 264 + tap * 88 + ci:l * 264 + tap * 88 + ci + 1]
        cbias = lambda ci: convb[:, l * 88 + ci:l * 88 + ci + 1]
        n = 0
        for g2 in range(22):
            wi = g2 % 2
            p.dma("pool", wa[wi][:], w_up[l][:, g2 * 256:(g2 + 1) * 256].rearrange("(kc q) c -> q kc c", q=128),
                  writes=[r_w[wi]])
            p.dma("pool", wb[wi][:],
                  w_up[l][:, DFF + g2 * 256:DFF + (g2 + 1) * 256].rearrange("(kc q) c -> q kc c", q=128),
                  writes=[r_w[wi]])
            for jj in range(2):
                j = g2 * 2 + jj
                gi = j % 2
                for tb in range(8):
                    b = n % 3
                    u = n % 2
                    n += 1
                    p.mm([(lambda kc=kc: T.matmul(PA[b][:], wa[wi][:, kc, jj * 128:(jj + 1) * 128],
                                                  hT[:, kc, tb * 512:(tb + 1) * 512], start=(kc == 0),
                                                  stop=(kc == KC - 1))) for kc in range(KC)],
                         reads=[r_w[wi], r_hT[tb]], writes=[r_PA[b]])
                    p.mm([(lambda kc=kc: T.matmul(PB[b][:], wb[wi][:, kc, jj * 128:(jj + 1) * 128],
                                                  hT[:, kc, tb * 512:(tb + 1) * 512], start=(kc == 0),
                                                  stop=(kc == KC - 1))) for kc in range(KC)],
                         reads=[r_w[wi], r_hT[tb]], writes=[r_PB[b]])
                    for (uu, r_uu, PP, r_PP, tt_, r_tt, ci, e1) in (
                            (ua, r_ua, PA, r_PA, ta, r_ta, j, "act"), (ub, r_ub, PB, r_PB, tb_, r_tb, NFC + j, "dve")):
                        if e1 == "act":
                            p.op("act", lambda: A.copy(uu[u][:, 2:514], PP[b][:]), reads=[r_PP[b]], writes=[r_uu[u]])
                        else:
                            p.op("dve", lambda: V.tensor_copy(uu[u][:, 2:514], PP[b][:]), reads=[r_PP[b]],
                                 writes=[r_uu[u]])
                        if tb == 0:
                            p.op("pool", lambda: G.memset(uu[u][:, 0:2], 0.0), writes=[r_uu[u]])
                        else:
                            p.op("pool", lambda: G.tensor_copy(uu[u][:, 0:2], uu[1 - u][:, 512:514]),
                                 reads=[r_uu[1 - u]], writes=[r_uu[u]])
                        p.op("act", lambda: A.activation(out=tt_[u][:], in_=uu[u][:, 2:514], func=AF.Identity,
                                                         scale=cw(2, ci), bias=cbias(ci)),
                             reads=[r_uu[u], r_c], writes=[r_tt[u]])
                        p.op("dve", lambda: V.scalar_tensor_tensor(out=tt_[u][:], in0=uu[u][:, 1:513], scalar=cw(1, ci),
                                                                   in1=tt_[u][:], op0=ALU.mult, op1=ALU.add),
                             reads=[r_uu[u], r_tt[u], r_c], writes=[r_tt[u]])
                        p.op("dve", lambda: V.scalar_tensor_tensor(out=tt_[u][:], in0=uu[u][:, 0:512], scalar=cw(0, ci),
                                                                   in1=tt_[u][:], op0=ALU.mult, op1=ALU.add),
                             reads=[r_uu[u], r_tt[u], r_c], writes=[r_tt[u]])
                    p.op("act", lambda: A.activation(out=ta[u][:], in_=ta[u][:], func=AF.Silu),
                         reads=[r_ta[u]], writes=[r_ta[u]])
                    p.op("dve", lambda: V.tensor_tensor(out=gt[gi][:, tb * 512:(tb + 1) * 512], in0=ta[u][:],
                                                        in1=tb_[u][:], op=ALU.mult),
                         reads=[r_ta[u], r_tb[u]], writes=[r_gt[gi]])
                p.dma("sp", GT[j * 128:(j + 1) * 128, :], gt[gi][:], reads=[r_gt[gi]])
        ph.close()
        oph.close()

    def phase_DOWN(l, resid_src, dst, final):
        ph = Phase()
        bufs = ln_bufs(ph, final)
        gin = ph.sb("gin", [128, NFC, 512], BF16)
        r_gin = R()
        wd = [ph.sb("wd%d" % i, [128, NFC, 128], BF16) for i in range(2)]
        r_wd = [R(), R()]
        PM = [ph.ps("pmd%d" % i) for i in range(2)]
        r_PM = [R(), R()]
        gv = GT.rearrange("(kc q) t -> q kc t", q=128)
        nw = 0
        for tb in range(8):
            for k4 in range(4):
                p.dma("sp", gin[:, k4 * 11:(k4 + 1) * 11, :], gv[:, k4 * 11:(k4 + 1) * 11, tb * 512:(tb + 1) * 512],
                      writes=[r_gin])

            def psm_get(j):
                nonlocal nw
                wi = nw % 2
                nw += 1
                p.dma("sp", wd[wi][:], WD16[j], writes=[r_wd[wi]])
                p.mm([(lambda kc=kc: T.matmul(PM[wi][:], wd[wi][:, kc, :], gin[:, kc, :], start=(kc == 0),
                                              stop=(kc == NFC - 1))) for kc in range(NFC)],
                     reads=[r_wd[wi], r_gin], writes=[r_PM[wi]])
                return PM[wi], r_PM[wi]

            ln_block(bufs, tb, l, "g2", 64 + l * 16, 96 + l * 16, psm_get, resid_src, dst, final)
        ph.close()

    stop = None
    for d_ in dbg:
        if d_.startswith("stop:"):
            stop = d_[5:]
    def done(tag):
        return stop == tag

    def run():
        if "skip:M" not in dbg:
            phase_M()
        if done("M"): return
        if "skip:T" not in dbg:
            phase_T()
        if done("T"): return
        if "skip:B" not in dbg:
            phase_B()
        if done("B"): return
        cur, oth = XT, X1T
        for l in range(2):
            phase_W(l)
            if done("W%d" % l): return
            phase_QKV(l, XT)
            if done("QKV%d" % l): return
            hs = range(32)
            for d_ in dbg:
                if d_.startswith("heads:"):
                    hs = [int(v) for v in d_[6:].split(".")]
            phase_ATT(l, hs)
            if done("ATT%d" % l): return
            phase_POST(l, XT, X1T)
            if done("POST%d" % l): return
            phase_UP(l, X1T)
            if done("UP%d" % l): return
            phase_DOWN(l, X1T, XT, final=(l == 1))
            if done("DOWN%d" % l): return
    run()
    p.barrier()
    return nc, p


def make_inputs(inputs, b, consts):
    f = lambda a: np.ascontiguousarray(a, dtype=np.float32)
    pl = lambda v: f(np.asarray(v).reshape(-1, 128).T)
    m = {}
    m["x"] = f(inputs["x"][b])
    m["cT"] = pl(inputs["c"][b])
    for l in range(2):
        m["w_ada%d" % l] = f(inputs["w_ada"][l])
        m["bada%d" % l] = pl(inputs["b_ada"][l])
        m["w_in%d" % l] = f(inputs["w_in"][l])
        m["w_out%d" % l] = f(inputs["w_out"][l])
        m["w_up%d" % l] = f(inputs["w_up"][l])
        m["w_down%d" % l] = f(inputs["w_down"][l])
    m["bf"] = f(np.asarray(inputs["b_f"]).T)
    gm = [np.concatenate([inputs["g_mix_a"][l], inputs["g_mix_b"][l], inputs["g_mix_c"][l]]) for l in range(2)]
    m["gmix"] = f(np.concatenate([pl(g) for g in gm], axis=1))
    m["lnp"] = f(np.concatenate([pl(inputs[k][l]) for k in ("ln1_g", "ln1_b", "ln2_g", "ln2_b") for l in range(2)],
                                axis=1))
    cw = []
    for l in range(2):
        for tap in range(3):
            cw.append(pl(inputs["conv_w"][l][tap]))
    m["convw"] = f(np.concatenate(cw, axis=1))
    m["convb"] = f(np.concatenate([pl(inputs["conv_b"][l]) for l in range(2)], axis=1))
    rb = np.asarray(inputs["rel_bias"], dtype=np.float32)
    m["relrep"] = f(np.repeat(rb[:, :, None], 128, axis=2).reshape(32, 22 * 128))
    m.update(consts)
    return m


def kernel(**inputs):
    inputs = {k: np.asarray(v) for k, v in inputs.items()}
    consts = host_constants()
    nc, _ = build()
    per_b = [make_inputs(inputs, b, consts) for b in range(4)]
    in_maps = [per_b[c % 4] for c in range(NCORES)]
    res = run_bass_kernel_spmd(nc, in_maps, core_ids=list(range(NCORES)))
    outs = [np.asarray(res.results[b]["out"], dtype=np.float32) for b in range(4)]
    return np.stack(outs, axis=0)
```

```python
from contextlib import ExitStack
import math
import numpy as np
import concourse.bass as bass
import concourse.mybir as mybir
from concourse.bass_utils import run_bass_kernel_spmd

F32 = mybir.dt.float32
BF16 = mybir.dt.bfloat16
AF = mybir.ActivationFunctionType
ALU = mybir.AluOpType
AX = mybir.AxisListType

S = 4096
D = 2048
KC = 16
DFF = 5632
NFC = 44
INC = 6154
LB = 4608
TSW = 4480
ALPHA = 4.0 ** 0.25
EPS = 1e-5
SCALE = 0.125
NCORES = 8


class R:
    __slots__ = ("name", "w", "rd")

    def __init__(self, name=""):
        self.name = name
        self.w = None
        self.rd = {}


class Prog:
    SAME = True
    NDS = 20

    def __init__(self, nc):
        self.nc = nc
        self.E = {"pe": nc.tensor, "act": nc.scalar, "dve": nc.vector,
                  "pool": nc.gpsimd, "sp": nc.sync}
        self.sem = {}
        self.cnt = {}
        for e in self.E:
            self.sem[e] = nc.alloc_semaphore("s_" + e)
            self.cnt[e] = 0
        self.waited = {e: {} for e in self.E}
        self.dsem = {}
        self.dcnt = {}
        self.dnext = {}
        for e in ("sp", "pool"):
            self.dsem[e] = [nc.alloc_semaphore("d_%s%d" % (e, i)) for i in range(self.NDS)]
            self.dcnt[e] = [0] * self.NDS
            self.dnext[e] = 0
        self.n_ins = 0

    def _wait(self, eng, toks):
        best = {}
        wd = self.waited[eng]
        for (k, h, v) in toks:
            if k == eng and (eng == "pe" or not self.SAME):
                continue
            if wd.get(k, 0) >= v:
                continue
            if k not in best or best[k][1] < v:
                best[k] = (h, v)
        for k, (h, v) in best.items():
            self.E[eng].wait_ge(h, v)
            wd[k] = v
            self.n_ins += 1

    def _deps(self, eng, reads, writes, extra=()):
        toks = list(extra)
        for r in reads:
            if r.w is not None:
                toks.append(r.w)
        for w in writes:
            if w.w is not None:
                toks.append(w.w)
            toks.extend(w.rd.values())
        self._wait(eng, toks)

    def _reg(self, tok, reads, writes):
        for r in reads:
            r.rd[tok[0]] = tok
        for w in writes:
            w.w = tok
            w.rd = {}

    def op(self, eng, fn, reads=(), writes=()):
        self._deps(eng, reads, writes)
        ins = fn()
        self.n_ins += 1
        self.cnt[eng] += 1
        ins.then_inc(self.sem[eng], 1)
        self._reg((eng, self.sem[eng], self.cnt[eng]), reads, writes)

    def mm(self, fns, reads=(), writes=()):
        self._deps("pe", reads, writes)
        ins = None
        for f in fns:
            ins = f()
            self.n_ins += 1
        self.cnt["pe"] += 1
        ins.then_inc(self.sem["pe"], 1)
        self._reg(("pe", self.sem["pe"], self.cnt["pe"]), reads, writes)

    def dma(self, eng, out, in_, reads=(), writes=()):
        i = self.dnext[eng]
        self.dnext[eng] = (i + 1) % self.NDS
        key = "d_%s%d" % (eng, i)
        extra = []
        if self.dcnt[eng][i]:
            extra.append((key, self.dsem[eng][i], self.dcnt[eng][i]))
        self._deps(eng, reads, writes, extra)
        ins = self.E[eng].dma_start(out=out, in_=in_)
        self.n_ins += 1
        self.dcnt[eng][i] += 16
        ins.then_inc(self.dsem[eng][i], 16)
        self._reg((key, self.dsem[eng][i], self.dcnt[eng][i]), reads, writes)

    def barrier(self):
        toks = []
        for e in self.E:
            if self.cnt[e]:
                toks.append((e, self.sem[e], self.cnt[e]))
        for e in self.dsem:
            for i in range(self.NDS):
                if self.dcnt[e][i]:
                    toks.append(("d_%s%d" % (e, i), self.dsem[e][i], self.dcnt[e][i]))
        same = self.SAME
        self.SAME = False
        for e in self.E:
            self._wait(e, toks)
        self.SAME = same


def t5_bucket_np(d):
    n = np.maximum(d, 0)
    nf = np.maximum(n, 1).astype(np.float32)
    large = 16 + (np.log(nf / np.float32(16)) / np.float32(math.log(2048 / 16)) * np.float32(16)).astype(np.int32)
    large = np.minimum(large, 31)
    return np.where(n < 16, n, large)


def host_constants():
    c = {}
    c["ident"] = np.eye(128, dtype=np.float32)
    i = np.arange(LB)
    d = i - 512
    bk = t5_bucket_np(d)
    oh = np.zeros((32, LB), np.float32)
    oh[bk, i] = 1.0
    oh[:, d < 0] = 0.0
    c["oh"] = oh
    cb = (d >= 0).astype(np.float32)
    ca = (((d >= 0) & (d <= 128)).astype(np.float32)
          + ((d >= 0) & (d <= 512) & (d % 4 == 0)).astype(np.float32)
          + ((d >= 0) & (d <= 2048) & (d % 16 == 0)).astype(np.float32))
    c["cmA"] = np.tile(ca[None, :], (128, 1)).astype(np.float32)
    c["cmB"] = np.tile(cb[None, :], (128, 1)).astype(np.float32)
    b1 = np.zeros((16, S), np.float32)
    for n in range(16):
        b1[n, n * 256:(n + 1) * 256] = 1.0
    c["blk1h"] = b1
    pn = np.zeros((128, 32, 16), np.float32)
    of = np.zeros((128, 32, 16), np.float32)
    for tt in range(32):
        qblk = tt // 2
        pn[:, tt, qblk:] = -1e30
        of[:, tt, qblk] = 1.0
    c["pastneg"] = pn.reshape(128, 512)
    c["ownfix"] = of.reshape(128, 512)
    cn = np.zeros((128, 4, 512), np.float32)
    p_ = np.arange(128)[:, None]
    j_ = np.arange(512)[None, :]
    for ii in range(4):
        cn[:, ii, :] = np.where(ii * 128 + p_ <= j_, 0.0, -1e9)
    c["cneg"] = cn.reshape(128, 2048)
    return c


def build(dbg=()):
    nc = bass.Bass("TRN2", target_bir_lowering=False)
    p = Prog(nc)

    def din(name, shape):
        return nc.dram_tensor(name, list(shape), F32, kind="ExternalInput").ap()

    def scratch(name, shape, dt):
        kind = "ExternalOutput" if name in dbg else "Internal"
        return nc.dram_tensor(name, list(shape), dt, kind=kind).ap()

    x_in = din("x", [S, D])
    cT = din("cT", [128, KC])
    w_ada = [din("w_ada%d" % l, [D, 6 * D]) for l in range(2)]
    bada = [din("bada%d" % l, [128, 96]) for l in range(2)]
    w_in = [din("w_in%d" % l, [D, INC]) for l in range(2)]
    bf_in = din("bf", [10, 2])
    gmix_in = din("gmix", [128, 32])
    w_out = [din("w_out%d" % l, [D, D]) for l in range(2)]
    lnp_in = din("lnp", [128, 128])
    w_up = [din("w_up%d" % l, [D, 2 * DFF]) for l in range(2)]
    convw_in = din("convw", [128, 2 * 3 * 88])
    convb_in = din("convb", [128, 2 * 88])
    w_down = [din("w_down%d" % l, [DFF, D]) for l in range(2)]
    relrep_in = din("relrep", [32, 22 * 128])
    ident_in = din("ident", [128, 128])
    oh_in = din("oh", [32, LB])
    cmA_in = din("cmA", [128, LB])
    cmB_in = din("cmB", [128, LB])
    blk1h_in = din("blk1h", [16, S])
    pastneg_in = din("pastneg", [128, 512])
    ownfix_in = din("ownfix", [128, 512])
    cneg_in = din("cneg", [128, 2048])
    out = nc.dram_tensor("out", [S, D], F32, kind="ExternalOutput").ap()

    XT = scratch("XT", [D, S], F32)
    X1T = scratch("X1T", [D, S], F32)
    PT = scratch("PT", [6144, S], BF16)
    VTM = scratch("VTM", [S, D], BF16)
    OT = scratch("OT", [D, S], F32)
    GT = scratch("GT", [DFF, S], BF16)
    FB = scratch("FB", [22, 128 * LB], F32)
    CUMQ = scratch("CUMQ", [10, S], BF16)
    CUMK = scratch("CUMK", [128, 320], F32)
    WD16 = scratch("WD16", [16, 128, NFC, 128], BF16)
    WO16 = scratch("WO16", [16, 128, KC, 128], BF16)
    MODD = scratch("MODD", [2, 128, 96], F32)

    uid = [0]

    class Phase:
        def __init__(self):
            self.st = ExitStack()

        def sb(self, name, shape, dt):
            uid[0] += 1
            return self.st.enter_context(nc.sbuf_tensor("s%d_%s" % (uid[0], name), list(shape), dt))

        def ps(self, name):
            uid[0] += 1
            return self.st.enter_context(nc.psum_tensor("p%d_%s" % (uid[0], name), [128, 512], F32))

        def close(self):
            p.barrier()
            self.st.close()

    V = nc.vector
    A = nc.scalar
    G = nc.gpsimd
    T = nc.tensor

    def evac(i, out_, in_, reads, writes):
        if i % 2 == 0:
            p.op("act", lambda: A.copy(out_, in_), reads, writes)
        else:
            p.op("dve", lambda: V.tensor_copy(out_, in_), reads, writes)

    gph = Phase()
    ident = gph.sb("ident", [128, 128], F32)
    r_c = R("consts")
    ones_bf = gph.sb("ones_bf", [128, 128], BF16)
    ones_f = gph.sb("ones_f", [128, 64], F32)
    mhalf = gph.sb("mhalf", [128, 512], F32)
    modT = gph.sb("modT", [128, 192], F32)
    gmix = gph.sb("gmix", [128, 32], F32)
    lnp = gph.sb("lnp", [128, 128], F32)
    convw = gph.sb("convw", [128, 528], F32)
    convb = gph.sb("convb", [128, 176], F32)
    bfs = gph.sb("bfs", [10, 2], F32)
    p.dma("sp", ident[:], ident_in, writes=[r_c])
    p.dma("sp", gmix[:], gmix_in, writes=[r_c])
    p.dma("sp", lnp[:], lnp_in, writes=[r_c])
    p.dma("sp", convw[:], convw_in, writes=[r_c])
    p.dma("sp", convb[:], convb_in, writes=[r_c])
    p.dma("sp", bfs[:], bf_in, writes=[r_c])
    p.op("dve", lambda: V.memset(ones_bf[:], 1.0), writes=[r_c])
    p.op("dve", lambda: V.memset(ones_f[:], 1.0), writes=[r_c])
    p.op("dve", lambda: V.memset(mhalf[:], -0.5), writes=[r_c])
    p.barrier()

    def phase_T():
        ph = Phase()
        xs = [ph.sb("xs%d" % i, [128, 4, D], F32) for i in range(2)]
        xT = [ph.sb("xTs%d" % i, [128, KC, 512], F32) for i in range(2)]
        r_xs = [R(), R()]
        r_xT = [R(), R()]
        ps = [ph.ps("psT%d" % i) for i in range(4)]
        r_ps = [R() for _ in range(4)]
        for tb in range(8):
            i = tb % 2
            p.dma("sp", xs[i][:], x_in[tb * 512:(tb + 1) * 512, :].rearrange("(t q) d -> q t d", q=128),
                  writes=[r_xs[i]])
            for kc in range(KC):
                b = kc % 4
                p.mm([(lambda t=t: T.transpose(ps[b][:, t * 128:(t + 1) * 128],
                                              xs[i][:, t, kc * 128:(kc + 1) * 128], ident[:]))
                      for t in range(4)], reads=[r_xs[i], r_c], writes=[r_ps[b]])
                evac(kc, xT[i][:, kc, :], ps[b][:], [r_ps[b]], [r_xT[i]])
            p.dma("sp", XT.rearrange("(kc q) t -> q kc t", q=128)[:, :, tb * 512:(tb + 1) * 512], xT[i][:],
                  reads=[r_xT[i]])
        ph.close()

    def phase_M():
        ph = Phase()
        cond = ph.sb("cond", [128, KC], F32)
        bad = ph.sb("bad", [128, 192], F32)
        wa = [ph.sb("wa%d" % i, [128, KC, 512], F32) for i in range(2)]
        r_wa = [R(), R()]
        r_cond = R()
        r_mod = R()
        mps = ph.ps("mps")
        r_mps = R()
        p.dma("sp", cond[:], cT, writes=[r_cond])
        p.dma("sp", bad[:, 0:96], bada[0], writes=[r_cond])
        p.dma("sp", bad[:, 96:192], bada[1], writes=[r_cond])
        p.op("act", lambda: A.activation(out=cond[:], in_=cond[:], func=AF.Silu), reads=[r_cond], writes=[r_cond])
        n = 0
        for l in range(2):
            for jb in range(24):
                i = n % 2
                n += 1
                p.dma("sp", wa[i][:], w_ada[l][:, jb * 512:(jb + 1) * 512].rearrange("(kc q) c -> q kc c", q=128),
                      writes=[r_wa[i]])
                for j4 in range(4):
                    col = jb * 4 + j4
                    p.mm([(lambda kc=kc: T.matmul(mps[:, col:col + 1], wa[i][:, kc, j4 * 128:(j4 + 1) * 128],
                                                  cond[:, kc:kc + 1], start=(kc == 0), stop=(kc == KC - 1)))
                          for kc in range(KC)], reads=[r_wa[i], r_cond], writes=[r_mps])
            o = l * 96
            p.op("dve", lambda: V.tensor_tensor(out=modT[:, o:o + 96], in0=mps[:, 0:96], in1=bad[:, o:o + 96],
                                                op=ALU.add), reads=[r_mps, r_cond], writes=[r_mod])
            for (a, sc) in ((16, 1.0), (32, 1.0 / ALPHA), (64, 1.0), (80, 1.0 / ALPHA)):
                p.op("dve", lambda: V.tensor_scalar(out=modT[:, o + a:o + a + 16], in0=modT[:, o + a:o + a + 16],
                                                    scalar1=1.0, scalar2=sc, op0=ALU.add, op1=ALU.mult),
                     reads=[r_mod], writes=[r_mod])
        if "MODD" in dbg:
            p.dma("sp", MODD.rearrange("l q c -> q l c"), modT[:].rearrange("q (l c) -> q l c", l=2), reads=[r_mod])
        ph.close()

    def mod(l, which, c):
        base = {"sh1": 0, "sc1": 16, "g1": 32, "sh2": 48, "sc2": 64, "g2": 80}[which]
        return modT[:, l * 96 + base + c: l * 96 + base + c + 1]

    def phase_B():
        ph = Phase()
        oh = ph.sb("oh", [32, LB], F32)
        cmA = ph.sb("cmA", [128, LB], F32)
        cmB = ph.sb("cmB", [128, LB], F32)
        rel = ph.sb("rel", [32, 22 * 128], F32)
        gs = [ph.sb("gs%d" % i, [128, LB], F32) for i in range(2)]
        r_g = [R(), R()]
        r_k = R()
        ps = [ph.ps("psB%d" % i) for i in range(4)]
        r_ps = [R() for _ in range(4)]
        p.dma("sp", oh[:], oh_in, writes=[r_k])
        p.dma("sp", cmA[:], cmA_in, writes=[r_k])
        p.dma("sp", cmB[:], cmB_in, writes=[r_k])
        p.dma("sp", rel[:], relrep_in, writes=[r_k])
        n = 0
        for h in range(22):
            i = h % 2
            for blk in range(9):
                b = n % 4
                n += 1
                p.mm([lambda: T.matmul(ps[b][:], rel[:, h * 128:(h + 1) * 128], oh[:, blk * 512:(blk + 1) * 512],
                                       start=True, stop=True)], reads=[r_k], writes=[r_ps[b]])
                p.op("act", lambda: A.activation(out=gs[i][:, blk * 512:(blk + 1) * 512], in_=ps[b][:], func=AF.Exp),
                     reads=[r_ps[b]], writes=[r_g[i]])
            cm = cmA if h < 12 else cmB
            p.op("dve", lambda: V.tensor_tensor(out=gs[i][:], in0=gs[i][:], in1=cm[:], op=ALU.mult),
                 reads=[r_g[i], r_k], writes=[r_g[i]])
            p.dma("sp", FB[h].rearrange("(q c) -> q c", q=128), gs[i][:], reads=[r_g[i]])
        ph.close()

    def phase_W(l):
        ph = Phase()
        wt = [ph.sb("wt%d" % i, [128, 4, D], BF16) for i in range(2)]
        r_wt = [R(), R()]
        n = 0
        for (src, dst, ng) in ((w_down[l], WD16, 11), (w_out[l], WO16, 4)):
            dv = dst.rearrange("j q kc c -> q kc j c")
            for kg in range(ng):
                i = n % 2
                n += 1
                p.dma("pool", wt[i][:], src[kg * 512:(kg + 1) * 512, :].rearrange("(kc q) n -> q kc n", q=128),
                      writes=[r_wt[i]])
                for k4 in range(4):
                    p.dma("sp", dv[:, kg * 4 + k4, :, :], wt[i][:, k4, :].rearrange("q (j c) -> q j c", c=128),
                          reads=[r_wt[i]])
        ph.close()

    def build_hT(ph, hT, r_hT, src, l, sc, sh):
        xin = [ph.sb("xin%d" % i, [128, KC, 512], F32) for i in range(2)]
        r_xin = [R(), R()]
        sv = src.rearrange("(kc q) t -> q kc t", q=128)
        for tb in range(8):
            i = tb % 2
            p.dma("sp", xin[i][:], sv[:, :, tb * 512:(tb + 1) * 512], writes=[r_xin[i]])
            for kc in range(KC):
                o_ = hT[:, kc, tb * 512:(tb + 1) * 512]
                if kc % 2 == 0:
                    p.op("act", lambda: A.activation(out=o_, in_=xin[i][:, kc, :], func=AF.Identity,
                                                     scale=mod(l, sc, kc), bias=mod(l, sh, kc)),
                         reads=[r_xin[i]], writes=[r_hT[tb]])
                else:
                    p.op("dve", lambda: V.tensor_scalar(out=o_, in0=xin[i][:, kc, :], scalar1=mod(l, sc, kc),
                                                        scalar2=mod(l, sh, kc), op0=ALU.mult, op1=ALU.add),
                         reads=[r_xin[i]], writes=[r_hT[tb]])

    VRANGES = ((1536, 2304, 0), (3584, 4224, 768), (5504, 6144, 1408))

    def vcol_of(col0):
        for (a, b, base) in VRANGES:
            if a <= col0 < b:
                return base + (col0 - a)
        return None

    def phase_QKV(l, src):
        oph = Phase()
        hT = oph.sb("hT", [128, KC, S], BF16)
        r_hT = [R() for _ in range(8)]
        ph = Phase()
        build_hT(ph, hT, r_hT, src, l, "sc1", "sh1")
        ph.close()
        ph = Phase()
        wb = [ph.sb("wb%d" % i, [128, KC, 512], BF16) for i in range(2)]
        r_wb = [R(), R()]
        fst = [ph.sb("fst%d" % i, [128, S], BF16) for i in range(2)]
        r_fst = [R(), R()]
        vst = [ph.sb("vst%d" % i, [128, 32, 128], BF16) for i in range(2)]
        r_vst = [R(), R()]
        ps = [ph.ps("psQ%d" % i) for i in range(6)]
        r_ps = [R() for _ in range(6)]
        npz = 0
        nf = 0
        nv = 0
        for g in range(12):
            i = g % 2
            p.dma("pool", wb[i][:], w_in[l][:, g * 512:(g + 1) * 512].rearrange("(kc q) c -> q kc c", q=128),
                  writes=[r_wb[i]])
            for jj in range(4):
                col0 = (g * 4 + jj) * 128
                vcol = vcol_of(col0)
                wsl = lambda kc: wb[i][:, kc, jj * 128:(jj + 1) * 128]
                if vcol is None:
                    fi = nf % 2
                    nf += 1
                    for tb in range(8):
                        b = npz % 6
                        npz += 1
                        p.mm([(lambda kc=kc: T.matmul(ps[b][:], wsl(kc), hT[:, kc, tb * 512:(tb + 1) * 512],
                                                      start=(kc == 0), stop=(kc == KC - 1))) for kc in range(KC)],
                             reads=[r_wb[i], r_hT[tb]], writes=[r_ps[b]])
                        evac(npz, fst[fi][:, tb * 512:(tb + 1) * 512], ps[b][:], [r_ps[b]], [r_fst[fi]])
                    p.dma("sp", PT[col0:col0 + 128, :], fst[fi][:], reads=[r_fst[fi]])
                else:
                    vi = nv % 2
                    nv += 1
                    for tt in range(32):
                        b = npz % 6
                        npz += 1
                        p.mm([(lambda kc=kc: T.matmul(ps[b][:, 0:128], hT[:, kc, tt * 128:(tt + 1) * 128], wsl(kc),
                                                      start=(kc == 0), stop=(kc == KC - 1))) for kc in range(KC)],
                             reads=[r_wb[i], r_hT[tt // 4]], writes=[r_ps[b]])
                        evac(npz, vst[vi][:, tt, :], ps[b][:, 0:128], [r_ps[b]], [r_vst[vi]])
                    vv = VTM.rearrange("(tt q) c -> q tt c", q=128)
                    for q4 in range(4):
                        p.dma("sp", vv[:, q4 * 8:(q4 + 1) * 8, vcol:vcol + 128], vst[vi][:, q4 * 8:(q4 + 1) * 8, :],
                              reads=[r_vst[vi]])
        ph.close()
        ph = Phase()
        wtail = ph.sb("wtail", [128, KC, 10], BF16)
        r_wtail = R()
        fT = ph.sb("fT", [32, S], F32)
        e2 = ph.sb("e2", [10, S], F32)
        cq = ph.sb("cq", [10, S], BF16)
        ck = ph.sb("ck", [128, 320], F32)
        r_f = R()
        r_e2 = R()
        r_cq = R()
        r_ck = R()
        ps = [ph.ps("psF%d" % i) for i in range(6)]
        r_ps = [R() for _ in range(6)]
        p.dma("pool", wtail[:], w_in[l][:, 6144:6154].rearrange("(kc q) c -> q kc c", q=128), writes=[r_wtail])
        p.op("pool", lambda: G.memset(fT[:], 0.0), writes=[r_f])
        for tb in range(8):
            b = npz % 6
            npz += 1
            p.mm([(lambda kc=kc: T.matmul(ps[b][0:10, :], wtail[:, kc, :], hT[:, kc, tb * 512:(tb + 1) * 512],
                                          start=(kc == 0), stop=(kc == KC - 1))) for kc in range(KC)],
                 reads=[r_wtail, r_hT[tb]], writes=[r_ps[b]])
            p.op("act", lambda: A.activation(out=fT[0:10, tb * 512:(tb + 1) * 512], in_=ps[b][0:10, :],
                                             func=AF.Identity, bias=bfs[0:10, l:l + 1]),
                 reads=[r_ps[b], r_c], writes=[r_f])
        p.op("act", lambda: A.activation(out=e2[0:10, :], in_=fT[0:10, :], func=AF.Exp, scale=-1.0),
             reads=[r_f], writes=[r_e2])
        p.op("act", lambda: A.activation(out=e2[0:10, :], in_=e2[0:10, :], func=AF.Ln, bias=1.0),
             reads=[r_e2], writes=[r_e2])
        p.op("dve", lambda: V.tensor_tensor_scan(out=fT[0:10, :], data0=e2[0:10, :], data1=e2[0:10, :], initial=0.0,
                                                 op0=ALU.add, op1=ALU.bypass), reads=[r_e2, r_f], writes=[r_f])
        p.op("act", lambda: A.activation(out=cq[0:10, :], in_=fT[0:10, :], func=AF.Identity, scale=-1.0 / SCALE),
             reads=[r_f], writes=[r_cq])
        p.dma("sp", CUMQ, cq[0:10, :], reads=[r_cq])
        b = npz % 6
        p.mm([(lambda tt=tt: T.matmul(ps[b][:, tt * 10:(tt + 1) * 10], fT[0:32, tt * 128:(tt + 1) * 128],
                                      ident[0:32, 0:10], start=True, stop=True)) for tt in range(32)],
             reads=[r_f, r_c], writes=[r_ps[b]])
        p.op("dve", lambda: V.tensor_copy(ck[:], ps[b][:, 0:320]), reads=[r_ps[b]], writes=[r_ck])
        p.dma("sp", CUMK, ck[:], reads=[r_ck])
        ph.close()
        oph.close()

    def phase_ATT(l, heads=range(32)):
        ph = Phase()
        QA = [ph.sb("QA%d" % i, [128, S], BF16) for i in range(2)]
        KA = [ph.sb("KA%d" % i, [128, S], BF16) for i in range(2)]
        VA = [ph.sb("VA%d" % i, [128, 32, 65], BF16) for i in range(2)]
        TS = [ph.sb("TS%d" % i, [128, TSW], F32) for i in range(2)]
        OST = [ph.sb("OST%d" % i, [64, S], F32) for i in range(2)]
        LA = 4
        NPS = 5
        NPT = 7
        ET = [ph.sb("ET%d" % i, [128, 512], F32) for i in range(NPS)]
        PTL = [ph.sb("PTL%d" % i, [128, 512], BF16) for i in range(NPT)]
        cneg = ph.sb("cneg", [128, 2048], F32)
        pastneg = ph.sb("pastneg", [128, 512], F32)
        ownfix = ph.sb("ownfix", [128, 512], F32)
        ck = ph.sb("ck", [128, 320], F32)
        gate = ph.sb("gate", [128, 512], F32)
        top8 = ph.sb("top8", [128, 256], F32)
        sel = ph.sb("sel", [128, 512], F32)
        selw = ph.sb("selw", [128, 32 * 80], F32)
        km = ph.sb("km", [128, 16], F32)
        kmh = ph.sb("kmh", [128, 16], BF16)
        kml = ph.sb("kml", [128, 16], BF16)
        rden = ph.sb("rden", [128, 512], F32)
        osb = ph.sb("osb", [64, 512], F32)
        r_QA = [R(), R()]
        r_KA = [R(), R()]
        r_VA = [R(), R()]
        r_TS = [R(), R()]
        r_OST = [R(), R()]
        r_ET = [R() for _ in range(NPS)]
        r_PTL = [R() for _ in range(NPT)]
        r_k = R()
        r_gate = R()
        r_top8 = R()
        r_sel = R()
        r_km = R()
        r_rden = R()
        r_osb = R()
        PSS = [ph.ps("pss%d" % i) for i in range(NPS)]
        r_PSS = [R() for _ in range(NPS)]
        PSO = [ph.ps("pso%d" % i) for i in range(2)]
        r_PSO = [R(), R()]
        PSB = ph.ps("psb")
        r_PSB = R()
        PSG = PSB
        r_PSG = r_PSB
        PSX = PSB
        r_PSX = r_PSB
        p.dma("sp", cneg[:], cneg_in, writes=[r_k])
        p.dma("sp", pastneg[:], pastneg_in, writes=[r_k])
        p.dma("sp", ownfix[:], ownfix_in, writes=[r_k])
        p.dma("sp", ck[:], CUMK, writes=[r_k])
        bsel = ph.sb("bsel", [128, 64], F32)
        p.op("pool", lambda: G.memset(bsel[:], 0.0), writes=[r_k])
        p.op("pool", lambda: G.memset(bsel[64:65, :], 1.0), writes=[r_k])
        p.op("pool", lambda: G.memset(rden[:], 0.0), writes=[r_rden])
        p.op("pool", lambda: G.memset(selw[:], 0.0), writes=[r_sel])
        for i in range(2):
            p.op("pool", lambda: G.memset(VA[i][:, :, 64:65], 1.0), writes=[r_VA[i]])
            p.op("pool", lambda: G.memset(QA[i][:], 0.0), writes=[r_QA[i]])
            p.op("pool", lambda: G.memset(KA[i][:], 0.0), writes=[r_KA[i]])

        def hinfo(hg):
            if hg < 12:
                return ("A", hg * 64, 768 + hg * 64, hg * 64, hg)
            if hg < 22:
                hb = hg - 12
                return ("B", 2304 + hb * 64, 2944 + hb * 64, 768 + hb * 64, hg)
            hc = hg - 22
            return ("C", 4224 + hc * 64, 4864 + hc * 64, 1408 + hc * 64, hc)

        def load(hg, i):
            typ, q0, k0, v0, ridx = hinfo(hg)
            if True:
                p.dma("sp", QA[i][0:64, :], PT[q0:q0 + 64, :], writes=[r_QA[i]])
                p.dma("sp", KA[i][0:64, :], PT[k0:k0 + 64, :], writes=[r_KA[i]])
                if typ == "B":
                    p.op("pool", lambda: G.memset(KA[i][64:96, :], 0.0), writes=[r_KA[i]])
                    p.dma("pool", KA[i][64:80, :], blk1h_in, writes=[r_KA[i]])
                if typ == "C":
                    p.dma("sp", QA[i][64:65, :], CUMQ[ridx:ridx + 1, :], writes=[r_QA[i]])
                    p.op("pool", lambda: G.memset(KA[i][64:96, :], 0.0), writes=[r_KA[i]])
                    p.op("pool", lambda: G.memset(KA[i][64:65, :], 1.0), writes=[r_KA[i]])
            vv = VTM.rearrange("(tt q) c -> q tt c", q=128)
            for q4 in range(4):
                p.dma("sp", VA[i][:, q4 * 8:(q4 + 1) * 8, 0:64], vv[:, q4 * 8:(q4 + 1) * 8, v0:v0 + 64],
                      writes=[r_VA[i]])
            if typ != "C":
                src = bass.AP(tensor=FB.tensor, offset=ridx * 128 * LB + 128, ap=[[LB - 1, 128], [1, TSW]])
                p.dma("sp", TS[i][:], src, writes=[r_TS[i]])

        hl = list(heads)
        load(hl[0], 0)
        cnt = 0
        nqb = 0
        pending = []
        for n_h, hg in enumerate(hl):
            i = n_h % 2
            typ, q0, k0, v0, ridx = hinfo(hg)
            while pending:
                pending.pop(0)()
            if n_h + 1 < len(hl):
                load(hl[n_h + 1], (n_h + 1) % 2)
            K = {"A": 64, "B": 96, "C": 96}[typ]
            if typ == "B":
                p.op("dve", lambda: V.tensor_reduce(out=km[0:64, :],
                                                    in_=KA[i][0:64, :].rearrange("q (n s) -> q n s", s=256),
                                                    axis=AX.X, op=ALU.add), reads=[r_KA[i]], writes=[r_km])
                p.op("dve", lambda: V.tensor_copy(kmh[0:64, :], km[0:64, :]), reads=[r_km], writes=[r_km])
                p.op("dve", lambda: V.tensor_tensor(out=kml[0:64, :], in0=km[0:64, :], in1=kmh[0:64, :],
                                                    op=ALU.subtract), reads=[r_km], writes=[r_km])
                fns = []
                for tt in range(32):
                    fns.append(lambda tt=tt: T.matmul(PSG[:, tt * 16:(tt + 1) * 16],
                                                      QA[i][0:64, tt * 128:(tt + 1) * 128], kmh[0:64, :],
                                                      start=True, stop=False))
                    fns.append(lambda tt=tt: T.matmul(PSG[:, tt * 16:(tt + 1) * 16],
                                                      QA[i][0:64, tt * 128:(tt + 1) * 128], kml[0:64, :],
                                                      start=False, stop=True))
                p.mm(fns, reads=[r_QA[i], r_km], writes=[r_PSG])
                p.op("dve", lambda: V.tensor_tensor(out=gate[:], in0=PSG[:], in1=pastneg[:], op=ALU.add),
                     reads=[r_PSG, r_k], writes=[r_gate])
                for tt in range(32):
                    p.op("dve", lambda: V.max(out=top8[:, tt * 8:(tt + 1) * 8], in_=gate[:, tt * 16:(tt + 1) * 16]),
                         reads=[r_gate], writes=[r_top8])
                thr = top8[:].rearrange("q (t e) -> q t e", e=8)[:, :, 2:3].to_broadcast([128, 32, 16])
                g3 = gate[:].rearrange("q (t e) -> q t e", e=16)
                s3 = sel[:].rearrange("q (t e) -> q t e", e=16)
                p.op("dve", lambda: V.tensor_tensor(out=s3, in0=g3, in1=thr, op=ALU.is_ge),
                     reads=[r_gate, r_top8], writes=[r_sel])
                p.op("dve", lambda: V.tensor_tensor(out=sel[:], in0=sel[:], in1=ownfix[:], op=ALU.max),
                     reads=[r_sel, r_k], writes=[r_sel])
                p.op("dve", lambda: V.tensor_scalar(out=selw[:].rearrange("q (t e) -> q t e", e=80)[:, :, 64:80],
                                                    in0=s3, scalar1=-1.0, scalar2=30000.0,
                                                    op0=ALU.add, op1=ALU.mult), reads=[r_sel], writes=[r_sel])
                for g8 in range(8):
                    p.mm([(lambda t=t: T.transpose(PSX[0:80, t * 128:(t + 1) * 128],
                                                   selw[:, (g8 * 4 + t) * 80:(g8 * 4 + t + 1) * 80], ident[:]))
                          for t in range(4)], reads=[r_sel, r_c], writes=[r_PSX])
                    p.op("act", lambda: A.copy(QA[i][64:80, g8 * 512:(g8 + 1) * 512], PSX[64:80, :]),
                         reads=[r_PSX], writes=[r_QA[i]])
            oi = n_h % 2
            for qb in range(8):
                kt_lo = max(0, 4 * qb - 16) if typ == "A" else 0
                kts = list(range(kt_lo, 4 * qb + 4))
                po = nqb % 2
                nqb += 1
                for n, kt in enumerate(kts):
                    sb_ = cnt % NPS
                    pb = cnt % NPT
                    cnt += 1
                    p.mm([lambda: T.matmul(PSS[sb_][:], KA[i][0:K, kt * 128:(kt + 1) * 128],
                                           QA[i][0:K, qb * 512:(qb + 1) * 512], start=True, stop=True)],
                         reads=[r_KA[i], r_QA[i]], writes=[r_PSS[sb_]])
                    if typ != "C":
                        off = qb * 512 - kt * 128 + 384
                        p.op("act", lambda: A.activation(out=ET[sb_][:], in_=PSS[sb_][:], func=AF.Exp, scale=SCALE),
                             reads=[r_PSS[sb_]], writes=[r_ET[sb_]])
                        p.op("dve", lambda: V.tensor_tensor(out=PTL[pb][:], in0=ET[sb_][:],
                                                            in1=TS[i][:, off:off + 512], op=ALU.mult),
                             reads=[r_ET[sb_], r_TS[i]], writes=[r_PTL[pb]])
                    else:
                        bias = ck[:, kt * 10 + ridx:kt * 10 + ridx + 1]
                        if kt >= 4 * qb:
                            dg = kt - 4 * qb
                            p.op("dve", lambda: V.tensor_tensor(out=ET[sb_][:], in0=PSS[sb_][:],
                                                                in1=cneg[:, dg * 512:(dg + 1) * 512], op=ALU.add),
                                 reads=[r_PSS[sb_], r_k], writes=[r_ET[sb_]])
                            p.op("act", lambda: A.activation(out=PTL[pb][:], in_=ET[sb_][:], func=AF.Exp,
                                                             scale=SCALE, bias=bias),
                                 reads=[r_ET[sb_], r_k], writes=[r_PTL[pb]])
                        else:
                            p.op("act", lambda: A.activation(out=PTL[pb][:], in_=PSS[sb_][:], func=AF.Exp,
                                                             scale=SCALE, bias=bias),
                                 reads=[r_PSS[sb_], r_k], writes=[r_PTL[pb]])

                    def tail(i=i, kt=kt, pb=pb, po=po, n=n, nk=len(kts), qb=qb, oi=oi, hg=hg):
                        p.mm([lambda: T.matmul(PSO[po][0:65, :], VA[i][:, kt, :], PTL[pb][:],
                                               start=(n == 0), stop=(n == nk - 1))],
                             reads=[r_VA[i], r_PTL[pb]], writes=[r_PSO[po]])
                        if n != nk - 1:
                            return
                        p.op("dve", lambda: V.reciprocal(rden[64:65, :], PSO[po][64:65, :]),
                             reads=[r_PSO[po]], writes=[r_rden])
                        p.mm([lambda: T.matmul(PSB[0:64, :], bsel[64:96, 0:64], rden[64:96, :], start=True, stop=True)],
                             reads=[r_rden, r_k], writes=[r_PSB])
                        p.op("act", lambda: A.copy(osb[0:64, :], PSO[po][0:64, :]), reads=[r_PSO[po]], writes=[r_osb])
                        p.op("dve", lambda: V.tensor_tensor(out=OST[oi][0:64, qb * 512:(qb + 1) * 512],
                                                            in0=osb[0:64, :], in1=PSB[0:64, :], op=ALU.mult),
                             reads=[r_osb, r_PSB], writes=[r_OST[oi]])
                        if qb == 7:
                            p.dma("sp", OT[hg * 64:(hg + 1) * 64, :], OST[oi][0:64, :], reads=[r_OST[oi]])

                    pending.append(tail)
                    if len(pending) > LA:
                        pending.pop(0)()
        while pending:
            pending.pop(0)()
        ph.close()

    def ln_block(ph_bufs, tb, l, which_g, lng_off, lnb_off, psm_get, resid_src, dst, final):
        (rT, xc, rbf, sqb, mean, msq, var, rstd, t1, xo, PS1, PS2, PST, osbig,
         r_rT, r_xc, r_rbf, r_sqb, r_st, r_t1, r_xo, r_PS1, r_PS2, r_PST, r_osbig) = ph_bufs
        for j in range(KC):
            jj = j % 2
            p.dma("sp", xc[jj][:], resid_src[j * 128:(j + 1) * 128, tb * 512:(tb + 1) * 512], writes=[r_xc[jj]])
            psm, r_psm = psm_get(j)
            p.op("dve", lambda: V.scalar_tensor_tensor(out=rT[:, j, :], in0=psm[:], scalar=mod(l, which_g, j),
                                                       in1=xc[jj][:], op0=ALU.mult, op1=ALU.add),
                 reads=[r_psm, r_xc[jj]], writes=[r_rT[j]])
            p.op("pool", lambda: G.tensor_copy(rbf[jj][:], rT[:, j, :]), reads=[r_rT[j]], writes=[r_rbf[jj]])
            p.op("act", lambda: A.activation(out=sqb[jj][:], in_=rT[:, j, :], func=AF.Square),
                 reads=[r_rT[j]], writes=[r_sqb[jj]])
            p.mm([lambda: T.matmul(PS1[:], ones_bf[:], rbf[jj][:], start=(j == 0), stop=(j == KC - 1))],
                 reads=[r_rbf[jj], r_c], writes=[r_PS1])
            p.mm([lambda: T.matmul(PS2[:], ones_bf[:], sqb[jj][:], start=(j == 0), stop=(j == KC - 1))],
                 reads=[r_sqb[jj], r_c], writes=[r_PS2])
        p.op("act", lambda: A.activation(out=mean[:], in_=PS1[:], func=AF.Identity, scale=1.0 / D),
             reads=[r_PS1], writes=[r_st])
        p.op("dve", lambda: V.tensor_tensor(out=msq[:], in0=mean[:], in1=mean[:], op=ALU.mult),
             reads=[r_st], writes=[r_st])
        p.op("dve", lambda: V.scalar_tensor_tensor(out=var[:], in0=PS2[:], scalar=1.0 / D, in1=msq[:],
                                                   op0=ALU.mult, op1=ALU.subtract), reads=[r_PS2, r_st], writes=[r_st])
        p.op("dve", lambda: V.tensor_scalar(out=var[:], in0=var[:], scalar1=EPS / (ALPHA * ALPHA), scalar2=None,
                                            op0=ALU.add), reads=[r_st], writes=[r_st])
        p.op("pool", lambda: G.tensor_tensor(out=rstd[:], in0=var[:], in1=mhalf[:], op=ALU.pow),
             reads=[r_st, r_c], writes=[r_st])
        for j in range(KC):
            jj = j % 2
            p.op("dve", lambda: V.tensor_tensor(out=t1[jj][:], in0=rT[:, j, :], in1=mean[:], op=ALU.subtract),
                 reads=[r_rT[j], r_st], writes=[r_t1[jj]])
            p.op("pool", lambda: G.tensor_tensor(out=t1[jj][:], in0=t1[jj][:], in1=rstd[:], op=ALU.mult),
                 reads=[r_t1[jj], r_st], writes=[r_t1[jj]])
            p.op("act", lambda: A.activation(out=xo[jj][:], in_=t1[jj][:], func=AF.Identity,
                                             scale=lnp[:, lng_off + j:lng_off + j + 1],
                                             bias=lnp[:, lnb_off + j:lnb_off + j + 1]),
                 reads=[r_t1[jj], r_c], writes=[r_xo[jj]])
            if not final:
                p.dma("sp", dst[j * 128:(j + 1) * 128, tb * 512:(tb + 1) * 512], xo[jj][:], reads=[r_xo[jj]])
            else:
                pt_ = j % 2
                p.mm([(lambda t=t: T.transpose(PST[pt_][:, t * 128:(t + 1) * 128], xo[jj][:, t * 128:(t + 1) * 128],
                                               ident[:])) for t in range(4)],
                     reads=[r_xo[jj], r_c], writes=[r_PST[pt_]])
                p.op("act", lambda: A.copy(osbig[:, :, j * 128:(j + 1) * 128],
                                           PST[pt_][:].rearrange("q (t c) -> q t c", c=128)),
                     reads=[r_PST[pt_]], writes=[r_osbig])
        if final:
            p.dma("sp", out[tb * 512:(tb + 1) * 512, :].rearrange("(t q) d -> q t d", q=128), osbig[:],
                  reads=[r_osbig])

    def ln_bufs(ph, final):
        rT = ph.sb("rT", [128, KC, 512], F32)
        xc = [ph.sb("xc%d" % i, [128, 512], F32) for i in range(2)]
        rbf = [ph.sb("rbf%d" % i, [128, 512], BF16) for i in range(2)]
        sqb = [ph.sb("sqb%d" % i, [128, 512], BF16) for i in range(2)]
        mean = ph.sb("mean", [128, 512], F32)
        msq = ph.sb("msq", [128, 512], F32)
        var = ph.sb("var", [128, 512], F32)
        rstd = ph.sb("rstd", [128, 512], F32)
        t1 = [ph.sb("t1%d" % i, [128, 512], F32) for i in range(2)]
        xo = [ph.sb("xo%d" % i, [128, 512], F32) for i in range(2)]
        PS1 = ph.ps("ps1")
        PS2 = ph.ps("ps2")
        PST = [ph.ps("pst%d" % i) for i in range(2)] if final else None
        osbig = ph.sb("osbig", [128, 4, D], F32) if final else None
        return (rT, xc, rbf, sqb, mean, msq, var, rstd, t1, xo, PS1, PS2, PST, osbig,
                [R() for _ in range(KC)], [R(), R()], [R(), R()], [R(), R()], R(), [R(), R()], [R(), R()],
                R(), R(), [R(), R()], R())

    def phase_POST(l, resid_src, dst):
        ph = Phase()
        bufs = ln_bufs(ph, False)
        ob = ph.sb("ob", [128, KC, 512], F32)
        sq = [ph.sb("sq%d" % i, [128, 512], BF16) for i in range(2)]
        ysb = ph.sb("ysb", [128, KC, 512], BF16)
        rs = [ph.sb("rs%d" % i, [128, 512], F32) for i in range(3)]
        wo = [ph.sb("wo%d" % i, [128, KC, 128], BF16) for i in range(2)]
        r_ob = R()
        r_sq = [R(), R()]
        r_ysb = R()
        r_rs = [R(), R(), R()]
        r_wo = [R(), R()]
        PG = [ph.ps("pg%d" % i) for i in range(3)]
        r_PG = [R(), R(), R()]
        PM = [ph.ps("pm%d" % i) for i in range(2)]
        r_PM = [R(), R()]
        grp_of = lambda c: 0 if c < 6 else (1 if c < 11 else 2)
        gfirst = (0, 6, 11)
        glast = (5, 10, 15)
        gn = (768.0, 640.0, 640.0)
        ov = OT.rearrange("(c q) t -> q c t", q=128)
        nw = 0
        for tb in range(8):
            p.dma("sp", ob[:], ov[:, :, tb * 512:(tb + 1) * 512], writes=[r_ob])
            for c in range(KC):
                g = grp_of(c)
                p.op("act", lambda: A.activation(out=sq[c % 2][:], in_=ob[:, c, :], func=AF.Square),
                     reads=[r_ob], writes=[r_sq[c % 2]])
                p.mm([lambda: T.matmul(PG[g][:], ones_bf[:], sq[c % 2][:], start=(c == gfirst[g]),
                                       stop=(c == glast[g]))], reads=[r_sq[c % 2], r_c], writes=[r_PG[g]])
            for g in range(3):
                p.op("dve", lambda: V.tensor_scalar(out=rs[g][:], in0=PG[g][:], scalar1=1.0 / gn[g], scalar2=EPS,
                                                    op0=ALU.mult, op1=ALU.add), reads=[r_PG[g]], writes=[r_rs[g]])
                p.op("pool", lambda: G.tensor_tensor(out=rs[g][:], in0=rs[g][:], in1=mhalf[:], op=ALU.pow),
                     reads=[r_rs[g], r_c], writes=[r_rs[g]])
            for c in range(KC):
                g = grp_of(c)
                p.op("dve", lambda: V.scalar_tensor_tensor(out=ysb[:, c, :], in0=ob[:, c, :],
                                                           scalar=gmix[:, l * 16 + c:l * 16 + c + 1], in1=rs[g][:],
                                                           op0=ALU.mult, op1=ALU.mult),
                     reads=[r_ob, r_rs[g], r_c], writes=[r_ysb])

            def psm_get(j):
                nonlocal nw
                wi = nw % 2
                nw += 1
                p.dma("sp", wo[wi][:], WO16[j], writes=[r_wo[wi]])
                p.mm([(lambda kc=kc: T.matmul(PM[wi][:], wo[wi][:, kc, :], ysb[:, kc, :], start=(kc == 0),
                                              stop=(kc == KC - 1))) for kc in range(KC)],
                     reads=[r_wo[wi], r_ysb], writes=[r_PM[wi]])
                return PM[wi], r_PM[wi]

            ln_block(bufs, tb, l, "g1", l * 16, 32 + l * 16, psm_get, resid_src, dst, False)
        ph.close()

    def phase_UP(l, src):
        oph = Phase()
        hT = oph.sb("hT2", [128, KC, S], BF16)
        r_hT = [R() for _ in range(8)]
        ph = Phase()
        build_hT(ph, hT, r_hT, src, l, "sc2", "sh2")
        ph.close()
        ph = Phase()
        wa = [ph.sb("wua%d" % i, [128, KC, 256], BF16) for i in range(2)]
        wb = [ph.sb("wub%d" % i, [128, KC, 256], BF16) for i in range(2)]
        r_w = [R(), R()]
        ua = [ph.sb("ua%d" % i, [128, 514], F32) for i in range(2)]
        ub = [ph.sb("ub%d" % i, [128, 514], F32) for i in range(2)]
        r_ua = [R(), R()]
        r_ub = [R(), R()]
        ta = [ph.sb("ta%d" % i, [128, 512], F32) for i in range(2)]
        tb_ = [ph.sb("tb%d" % i, [128, 512], F32) for i in range(2)]
        r_ta = [R(), R()]
        r_tb = [R(), R()]
        gt = [ph.sb("gt%d" % i, [128, S], BF16) for i in range(2)]
        r_gt = [R(), R()]
        PA = [ph.ps("pa%d" % i) for i in range(3)]
        PB = [ph.ps("pb%d" % i) for i in range(3)]
        r_PA = [R() for _ in range(3)]
        r_PB = [R() for _ in range(3)]
        cw = lambda tap, ci: convw[:, l * 264 + tap * 88 + ci:l * 264 + tap * 88 + ci + 1]
        cbias = lambda ci: convb[:, l * 88 + ci:l * 88 + ci + 1]
        n = 0
        for g2 in range(22):
            wi = g2 % 2
            p.dma("pool", wa[wi][:], w_up[l][:, g2 * 256:(g2 + 1) * 256].rearrange("(kc q) c -> q kc c", q=128),
                  writes=[r_w[wi]])
            p.dma("pool", wb[wi][:],
                  w_up[l][:, DFF + g2 * 256:DFF + (g2 + 1) * 256].rearrange("(kc q) c -> q kc c", q=128),
                  writes=[r_w[wi]])
            for jj in range(2):
                j = g2 * 2 + jj
                gi = j % 2
                for tb in range(8):
                    b = n % 3
                    u = n % 2
                    n += 1
                    p.mm([(lambda kc=kc: T.matmul(PA[b][:], wa[wi][:, kc, jj * 128:(jj + 1) * 128],
                                                  hT[:, kc, tb * 512:(tb + 1) * 512], start=(kc == 0),
                                                  stop=(kc == KC - 1))) for kc in range(KC)],
                         reads=[r_w[wi], r_hT[tb]], writes=[r_PA[b]])
                    p.mm([(lambda kc=kc: T.matmul(PB[b][:], wb[wi][:, kc, jj * 128:(jj + 1) * 128],
                                                  hT[:, kc, tb * 512:(tb + 1) * 512], start=(kc == 0),
                                                  stop=(kc == KC - 1))) for kc in range(KC)],
                         reads=[r_w[wi], r_hT[tb]], writes=[r_PB[b]])
                    for (uu, r_uu, PP, r_PP, tt_, r_tt, ci, e1) in (
                            (ua, r_ua, PA, r_PA, ta, r_ta, j, "act"), (ub, r_ub, PB, r_PB, tb_, r_tb, NFC + j, "dve")):
                        if e1 == "act":
                            p.op("act", lambda: A.copy(uu[u][:, 2:514], PP[b][:]), reads=[r_PP[b]], writes=[r_uu[u]])
                        else:
                            p.op("dve", lambda: V.tensor_copy(uu[u][:, 2:514], PP[b][:]), reads=[r_PP[b]],
                                 writes=[r_uu[u]])
                        if tb == 0:
                            p.op("pool", lambda: G.memset(uu[u][:, 0:2], 0.0), writes=[r_uu[u]])
                        else:
                            p.op("pool", lambda: G.tensor_copy(uu[u][:, 0:2], uu[1 - u][:, 512:514]),
                                 reads=[r_uu[1 - u]], writes=[r_uu[u]])
                        p.op("act", lambda: A.activation(out=tt_[u][:], in_=uu[u][:, 2:514], func=AF.Identity,
                                                         scale=cw(2, ci), bias=cbias(ci)),
                             reads=[r_uu[u], r_c], writes=[r_tt[u]])
                        p.op("dve", lambda: V.scalar_tensor_tensor(out=tt_[u][:], in0=uu[u][:, 1:513], scalar=cw(1, ci),
                                                                   in1=tt_[u][:], op0=ALU.mult, op1=ALU.add),
                             reads=[r_uu[u], r_tt[u], r_c], writes=[r_tt[u]])
                        p.op("dve", lambda: V.scalar_tensor_tensor(out=tt_[u][:], in0=uu[u][:, 0:512], scalar=cw(0, ci),
                                                                   in1=tt_[u][:], op0=ALU.mult, op1=ALU.add),
                             reads=[r_uu[u], r_tt[u], r_c], writes=[r_tt[u]])
                    p.op("act", lambda: A.activation(out=ta[u][:], in_=ta[u][:], func=AF.Silu),
                         reads=[r_ta[u]], writes=[r_ta[u]])
                    p.op("dve", lambda: V.tensor_tensor(out=gt[gi][:, tb * 512:(tb + 1) * 512], in0=ta[u][:],
                                                        in1=tb_[u][:], op=ALU.mult),
                         reads=[r_ta[u], r_tb[u]], writes=[r_gt[gi]])
                p.dma("sp", GT[j * 128:(j + 1) * 128, :], gt[gi][:], reads=[r_gt[gi]])
        ph.close()
        oph.close()

    def phase_DOWN(l, resid_src, dst, final):
        ph = Phase()
        bufs = ln_bufs(ph, final)
        gin = ph.sb("gin", [128, NFC, 512], BF16)
        r_gin = R()
        wd = [ph.sb("wd%d" % i, [128, NFC, 128], BF16) for i in range(2)]
        r_wd = [R(), R()]
        PM = [ph.ps("pmd%d" % i) for i in range(2)]
        r_PM = [R(), R()]
        gv = GT.rearrange("(kc q) t -> q kc t", q=128)
        nw = 0
        for tb in range(8):
            for k4 in range(4):
                p.dma("sp", gin[:, k4 * 11:(k4 + 1) * 11, :], gv[:, k4 * 11:(k4 + 1) * 11, tb * 512:(tb + 1) * 512],
                      writes=[r_gin])

            def psm_get(j):
                nonlocal nw
                wi = nw % 2
                nw += 1
                p.dma("sp", wd[wi][:], WD16[j], writes=[r_wd[wi]])
                p.mm([(lambda kc=kc: T.matmul(PM[wi][:], wd[wi][:, kc, :], gin[:, kc, :], start=(kc == 0),
                                              stop=(kc == NFC - 1))) for kc in range(NFC)],
                     reads=[r_wd[wi], r_gin], writes=[r_PM[wi]])
                return PM[wi], r_PM[wi]

            ln_block(bufs, tb, l, "g2", 64 + l * 16, 96 + l * 16, psm_get, resid_src, dst, final)
        ph.close()

    stop = None
    for d_ in dbg:
        if d_.startswith("stop:"):
            stop = d_[5:]
    def done(tag):
        return stop == tag

    def run():
        if "skip:M" not in dbg:
            phase_M()
        if done("M"): return
        if "skip:T" not in dbg:
            phase_T()
        if done("T"): return
        if "skip:B" not in dbg:
            phase_B()
        if done("B"): return
        cur, oth = XT, X1T
        for l in range(2):
            phase_W(l)
            if done("W%d" % l): return
            phase_QKV(l, XT)
            if done("QKV%d" % l): return
            hs = range(32)
            for d_ in dbg:
                if d_.startswith("heads:"):
                    hs = [int(v) for v in d_[6:].split(".")]
            phase_ATT(l, hs)
            if done("ATT%d" % l): return
            phase_POST(l, XT, X1T)
            if done("POST%d" % l): return
            phase_UP(l, X1T)
            if done("UP%d" % l): return
            phase_DOWN(l, X1T, XT, final=(l == 1))
            if done("DOWN%d" % l): return
    run()
    p.barrier()
    return nc, p


def make_inputs(inputs, b, consts):
    f = lambda a: np.ascontiguousarray(a, dtype=np.float32)
    pl = lambda v: f(np.asarray(v).reshape(-1, 128).T)
    m = {}
    m["x"] = f(inputs["x"][b])
    m["cT"] = pl(inputs["c"][b])
    for l in range(2):
        m["w_ada%d" % l] = f(inputs["w_ada"][l])
        m["bada%d" % l] = pl(inputs["b_ada"][l])
        m["w_in%d" % l] = f(inputs["w_in"][l])
        m["w_out%d" % l] = f(inputs["w_out"][l])
        m["w_up%d" % l] = f(inputs["w_up"][l])
        m["w_down%d" % l] = f(inputs["w_down"][l])
    m["bf"] = f(np.asarray(inputs["b_f"]).T)
    gm = [np.concatenate([inputs["g_mix_a"][l], inputs["g_mix_b"][l], inputs["g_mix_c"][l]]) for l in range(2)]
    m["gmix"] = f(np.concatenate([pl(g) for g in gm], axis=1))
    m["lnp"] = f(np.concatenate([pl(inputs[k][l]) for k in ("ln1_g", "ln1_b", "ln2_g", "ln2_b") for l in range(2)],
                                axis=1))
    cw = []
    for l in range(2):
        for tap in range(3):
            cw.append(pl(inputs["conv_w"][l][tap]))
    m["convw"] = f(np.concatenate(cw, axis=1))
    m["convb"] = f(np.concatenate([pl(inputs["conv_b"][l]) for l in range(2)], axis=1))
    rb = np.asarray(inputs["rel_bias"], dtype=np.float32)
    m["relrep"] = f(np.repeat(rb[:, :, None], 128, axis=2).reshape(32, 22 * 128))
    m.update(consts)
    return m


def kernel(**inputs):
    inputs = {k: np.asarray(v) for k, v in inputs.items()}
    consts = host_constants()
    nc, _ = build()
    per_b = [make_inputs(inputs, b, consts) for b in range(4)]
    in_maps = [per_b[c % 4] for c in range(NCORES)]
    res = run_bass_kernel_spmd(nc, in_maps, core_ids=list(range(NCORES)))
    outs = [np.asarray(res.results[b]["out"], dtype=np.float32) for b in range(4)]
    return np.stack(outs, axis=0)
```

```python
from contextlib import ExitStack
import math
import numpy as np
import concourse.bass as bass
import concourse.mybir as mybir
from concourse.bass_utils import run_bass_kernel_spmd

F32 = mybir.dt.float32
BF16 = mybir.dt.bfloat16
AF = mybir.ActivationFunctionType
ALU = mybir.AluOpType
AX = mybir.AxisListType

S = 4096
D = 2048
KC = 16
DFF = 5632
NFC = 44
INC = 6154
LB = 4608
TSW = 4480
ALPHA = 4.0 ** 0.25
EPS = 1e-5
SCALE = 0.125
NCORES = 8


class R:
    __slots__ = ("name", "w", "rd")

    def __init__(self, name=""):
        self.name = name
        self.w = None
        self.rd = {}


class Prog:
    SAME = True
    NDS = 20

    def __init__(self, nc):
        self.nc = nc
        self.E = {"pe": nc.tensor, "act": nc.scalar, "dve": nc.vector,
                  "pool": nc.gpsimd, "sp": nc.sync}
        self.sem = {}
        self.cnt = {}
        for e in self.E:
            self.sem[e] = nc.alloc_semaphore("s_" + e)
            self.cnt[e] = 0
        self.waited = {e: {} for e in self.E}
        self.dsem = {}
        self.dcnt = {}
        self.dnext = {}
        for e in ("sp", "pool"):
            self.dsem[e] = [nc.alloc_semaphore("d_%s%d" % (e, i)) for i in range(self.NDS)]
            self.dcnt[e] = [0] * self.NDS
            self.dnext[e] = 0
        self.n_ins = 0

    def _wait(self, eng, toks):
        best = {}
        wd = self.waited[eng]
        for (k, h, v) in toks:
            if k == eng and (eng == "pe" or not self.SAME):
                continue
            if wd.get(k, 0) >= v:
                continue
            if k not in best or best[k][1] < v:
                best[k] = (h, v)
        for k, (h, v) in best.items():
            self.E[eng].wait_ge(h, v)
            wd[k] = v
            self.n_ins += 1

    def _deps(self, eng, reads, writes, extra=()):
        toks = list(extra)
        for r in reads:
            if r.w is not None:
                toks.append(r.w)
        for w in writes:
            if w.w is not None:
                toks.append(w.w)
            toks.extend(w.rd.values())
        self._wait(eng, toks)

    def _reg(self, tok, reads, writes):
        for r in reads:
            r.rd[tok[0]] = tok
        for w in writes:
            w.w = tok
            w.rd = {}

    def op(self, eng, fn, reads=(), writes=()):
        self._deps(eng, reads, writes)
        ins = fn()
        self.n_ins += 1
        self.cnt[eng] += 1
        ins.then_inc(self.sem[eng], 1)
        self._reg((eng, self.sem[eng], self.cnt[eng]), reads, writes)

    def mm(self, fns, reads=(), writes=()):
        self._deps("pe", reads, writes)
        ins = None
        for f in fns:
            ins = f()
            self.n_ins += 1
        self.cnt["pe"] += 1
        ins.then_inc(self.sem["pe"], 1)
        self._reg(("pe", self.sem["pe"], self.cnt["pe"]), reads, writes)

    def dma(self, eng, out, in_, reads=(), writes=()):
        i = self.dnext[eng]
        self.dnext[eng] = (i + 1) % self.NDS
        key = "d_%s%d" % (eng, i)
        extra = []
        if self.dcnt[eng][i]:
            extra.append((key, self.dsem[eng][i], self.dcnt[eng][i]))
        self._deps(eng, reads, writes, extra)
        ins = self.E[eng].dma_start(out=out, in_=in_)
        self.n_ins += 1
        self.dcnt[eng][i] += 16
        ins.then_inc(self.dsem[eng][i], 16)
        self._reg((key, self.dsem[eng][i], self.dcnt[eng][i]), reads, writes)

    def barrier(self):
        toks = []
        for e in self.E:
            if self.cnt[e]:
                toks.append((e, self.sem[e], self.cnt[e]))
        for e in self.dsem:
            for i in range(self.NDS):
                if self.dcnt[e][i]:
                    toks.append(("d_%s%d" % (e, i), self.dsem[e][i], self.dcnt[e][i]))
        same = self.SAME
        self.SAME = False
        for e in self.E:
            self._wait(e, toks)
        self.SAME = same


def t5_bucket_np(d):
    n = np.maximum(d, 0)
    nf = np.maximum(n, 1).astype(np.float32)
    large = 16 + (np.log(nf / np.float32(16)) / np.float32(math.log(2048 / 16)) * np.float32(16)).astype(np.int32)
    large = np.minimum(large, 31)
    return np.where(n < 16, n, large)


def host_constants():
    c = {}
    c["ident"] = np.eye(128, dtype=np.float32)
    i = np.arange(LB)
    d = i - 512
    bk = t5_bucket_np(d)
    oh = np.zeros((32, LB), np.float32)
    oh[bk, i] = 1.0
    oh[:, d < 0] = 0.0
    c["oh"] = oh
    cb = (d >= 0).astype(np.float32)
    ca = (((d >= 0) & (d <= 128)).astype(np.float32)
          + ((d >= 0) & (d <= 512) & (d % 4 == 0)).astype(np.float32)
          + ((d >= 0) & (d <= 2048) & (d % 16 == 0)).astype(np.float32))
    c["cmA"] = np.tile(ca[None, :], (128, 1)).astype(np.float32)
    c["cmB"] = np.tile(cb[None, :], (128, 1)).astype(np.float32)
    b1 = np.zeros((16, S), np.float32)
    for n in range(16):
        b1[n, n * 256:(n + 1) * 256] = 1.0
    c["blk1h"] = b1
    pn = np.zeros((128, 32, 16), np.float32)
    of = np.zeros((128, 32, 16), np.float32)
    for tt in range(32):
        qblk = tt // 2
        pn[:, tt, qblk:] = -1e30
        of[:, tt, qblk] = 1.0
    c["pastneg"] = pn.reshape(128, 512)
    c["ownfix"] = of.reshape(128, 512)
    cn = np.zeros((128, 4, 512), np.float32)
    p_ = np.arange(128)[:, None]
    j_ = np.arange(512)[None, :]
    for ii in range(4):
        cn[:, ii, :] = np.where(ii * 128 + p_ <= j_, 0.0, -1e9)
    c["cneg"] = cn.reshape(128, 2048)
    return c


def build(dbg=()):
    nc = bass.Bass("TRN2", target_bir_lowering=False)
    p = Prog(nc)

    def din(name, shape):
        return nc.dram_tensor(name, list(shape), F32, kind="ExternalInput").ap()

    def scratch(name, shape, dt):
        kind = "ExternalOutput" if name in dbg else "Internal"
        return nc.dram_tensor(name, list(shape), dt, kind=kind).ap()

    x_in = din("x", [S, D])
    cT = din("cT", [128, KC])
    w_ada = [din("w_ada%d" % l, [D, 6 * D]) for l in range(2)]
    bada = [din("bada%d" % l, [128, 96]) for l in range(2)]
    w_in = [din("w_in%d" % l, [D, INC]) for l in range(2)]
    bf_in = din("bf", [10, 2])
    gmix_in = din("gmix", [128, 32])
    w_out = [din("w_out%d" % l, [D, D]) for l in range(2)]
    lnp_in = din("lnp", [128, 128])
    w_up = [din("w_up%d" % l, [D, 2 * DFF]) for l in range(2)]
    convw_in = din("convw", [128, 2 * 3 * 88])
    convb_in = din("convb", [128, 2 * 88])
    w_down = [din("w_down%d" % l, [DFF, D]) for l in range(2)]
    relrep_in = din("relrep", [32, 22 * 128])
    ident_in = din("ident", [128, 128])
    oh_in = din("oh", [32, LB])
    cmA_in = din("cmA", [128, LB])
    cmB_in = din("cmB", [128, LB])
    blk1h_in = din("blk1h", [16, S])
    pastneg_in = din("pastneg", [128, 512])
    ownfix_in = din("ownfix", [128, 512])
    cneg_in = din("cneg", [128, 2048])
    out = nc.dram_tensor("out", [S, D], F32, kind="ExternalOutput").ap()

    XT = scratch("XT", [D, S], F32)
    X1T = scratch("X1T", [D, S], F32)
    PT = scratch("PT", [6144, S], BF16)
    VTM = scratch("VTM", [S, D], BF16)
    OT = scratch("OT", [D, S], F32)
    GT = scratch("GT", [DFF, S], BF16)
    FB = scratch("FB", [22, 128 * LB], F32)
    CUMQ = scratch("CUMQ", [10, S], BF16)
    CUMK = scratch("CUMK", [128, 320], F32)
    WD16 = scratch("WD16", [16, 128, NFC, 128], BF16)
    WO16 = scratch("WO16", [16, 128, KC, 128], BF16)
    MODD = scratch("MODD", [2, 128, 96], F32)

    uid = [0]

    class Phase:
        def __init__(self):
            self.st = ExitStack()

        def sb(self, name, shape, dt):
            uid[0] += 1
            return self.st.enter_context(nc.sbuf_tensor("s%d_%s" % (uid[0], name), list(shape), dt))

        def ps(self, name):
            uid[0] += 1
            return self.st.enter_context(nc.psum_tensor("p%d_%s" % (uid[0], name), [128, 512], F32))

        def close(self):
            p.barrier()
            self.st.close()

    V = nc.vector
    A = nc.scalar
    G = nc.gpsimd
    T = nc.tensor

    def evac(i, out_, in_, reads, writes):
        if i % 2 == 0:
            p.op("act", lambda: A.copy(out_, in_), reads, writes)
        else:
            p.op("dve", lambda: V.tensor_copy(out_, in_), reads, writes)

    gph = Phase()
    ident = gph.sb("ident", [128, 128], F32)
    r_c = R("consts")
    ones_bf = gph.sb("ones_bf", [128, 128], BF16)
    ones_f = gph.sb("ones_f", [128, 64], F32)
    mhalf = gph.sb("mhalf", [128, 512], F32)
    modT = gph.sb("modT", [128, 192], F32)
    gmix = gph.sb("gmix", [128, 32], F32)
    lnp = gph.sb("lnp", [128, 128], F32)
    convw = gph.sb("convw", [128, 528], F32)
    convb = gph.sb("convb", [128, 176], F32)
    bfs = gph.sb("bfs", [10, 2], F32)
    p.dma("sp", ident[:], ident_in, writes=[r_c])
    p.dma("sp", gmix[:], gmix_in, writes=[r_c])
    p.dma("sp", lnp[:], lnp_in, writes=[r_c])
    p.dma("sp", convw[:], convw_in, writes=[r_c])
    p.dma("sp", convb[:], convb_in, writes=[r_c])
    p.dma("sp", bfs[:], bf_in, writes=[r_c])
    p.op("dve", lambda: V.memset(ones_bf[:], 1.0), writes=[r_c])
    p.op("dve", lambda: V.memset(ones_f[:], 1.0), writes=[r_c])
    p.op("dve", lambda: V.memset(mhalf[:], -0.5), writes=[r_c])
    p.barrier()

    def phase_T():
        ph = Phase()
        xs = [ph.sb("xs%d" % i, [128, 4, D], F32) for i in range(2)]
        xT = [ph.sb("xTs%d" % i, [128, KC, 512], F32) for i in range(2)]
        r_xs = [R(), R()]
        r_xT = [R(), R()]
        ps = [ph.ps("psT%d" % i) for i in range(4)]
        r_ps = [R() for _ in range(4)]
        for tb in range(8):
            i = tb % 2
            p.dma("sp", xs[i][:], x_in[tb * 512:(tb + 1) * 512, :].rearrange("(t q) d -> q t d", q=128),
                  writes=[r_xs[i]])
            for kc in range(KC):
                b = kc % 4
                p.mm([(lambda t=t: T.transpose(ps[b][:, t * 128:(t + 1) * 128],
                                              xs[i][:, t, kc * 128:(kc + 1) * 128], ident[:]))
                      for t in range(4)], reads=[r_xs[i], r_c], writes=[r_ps[b]])
                evac(kc, xT[i][:, kc, :], ps[b][:], [r_ps[b]], [r_xT[i]])
            p.dma("sp", XT.rearrange("(kc q) t -> q kc t", q=128)[:, :, tb * 512:(tb + 1) * 512], xT[i][:],
                  reads=[r_xT[i]])
        ph.close()

    def phase_M():
        ph = Phase()
        cond = ph.sb("cond", [128, KC], F32)
        bad = ph.sb("bad", [128, 192], F32)
        wa = [ph.sb("wa%d" % i, [128, KC, 512], F32) for i in range(2)]
        r_wa = [R(), R()]
        r_cond = R()
        r_mod = R()
        mps = ph.ps("mps")
        r_mps = R()
        p.dma("sp", cond[:], cT, writes=[r_cond])
        p.dma("sp", bad[:, 0:96], bada[0], writes=[r_cond])
        p.dma("sp", bad[:, 96:192], bada[1], writes=[r_cond])
        p.op("act", lambda: A.activation(out=cond[:], in_=cond[:], func=AF.Silu), reads=[r_cond], writes=[r_cond])
        n = 0
        for l in range(2):
            for jb in range(24):
                i = n % 2
                n += 1
                p.dma("sp", wa[i][:], w_ada[l][:, jb * 512:(jb + 1) * 512].rearrange("(kc q) c -> q kc c", q=128),
                      writes=[r_wa[i]])
                for j4 in range(4):
                    col = jb * 4 + j4
                    p.mm([(lambda kc=kc: T.matmul(mps[:, col:col + 1], wa[i][:, kc, j4 * 128:(j4 + 1) * 128],
                                                  cond[:, kc:kc + 1], start=(kc == 0), stop=(kc == KC - 1)))
                          for kc in range(KC)], reads=[r_wa[i], r_cond], writes=[r_mps])
            o = l * 96
            p.op("dve", lambda: V.tensor_tensor(out=modT[:, o:o + 96], in0=mps[:, 0:96], in1=bad[:, o:o + 96],
                                                op=ALU.add), reads=[r_mps, r_cond], writes=[r_mod])
            for (a, sc) in ((16, 1.0), (32, 1.0 / ALPHA), (64, 1.0), (80, 1.0 / ALPHA)):
                p.op("dve", lambda: V.tensor_scalar(out=modT[:, o + a:o + a + 16], in0=modT[:, o + a:o + a + 16],
                                                    scalar1=1.0, scalar2=sc, op0=ALU.add, op1=ALU.mult),
                     reads=[r_mod], writes=[r_mod])
        if "MODD" in dbg:
            p.dma("sp", MODD.rearrange("l q c -> q l c"), modT[:].rearrange("q (l c) -> q l c", l=2), reads=[r_mod])
        ph.close()

    def mod(l, which, c):
        base = {"sh1": 0, "sc1": 16, "g1": 32, "sh2": 48, "sc2": 64, "g2": 80}[which]
        return modT[:, l * 96 + base + c: l * 96 + base + c + 1]

    def phase_B():
        ph = Phase()
        oh = ph.sb("oh", [32, LB], F32)
        cmA = ph.sb("cmA", [128, LB], F32)
        cmB = ph.sb("cmB", [128, LB], F32)
        rel = ph.sb("rel", [32, 22 * 128], F32)
        gs = [ph.sb("gs%d" % i, [128, LB], F32) for i in range(2)]
        r_g = [R(), R()]
        r_k = R()
        ps = [ph.ps("psB%d" % i) for i in range(4)]
        r_ps = [R() for _ in range(4)]
        p.dma("sp", oh[:], oh_in, writes=[r_k])
        p.dma("sp", cmA[:], cmA_in, writes=[r_k])
        p.dma("sp", cmB[:], cmB_in, writes=[r_k])
        p.dma("sp", rel[:], relrep_in, writes=[r_k])
        n = 0
        for h in range(22):
            i = h % 2
            for blk in range(9):
                b = n % 4
                n += 1
                p.mm([lambda: T.matmul(ps[b][:], rel[:, h * 128:(h + 1) * 128], oh[:, blk * 512:(blk + 1) * 512],
                                       start=True, stop=True)], reads=[r_k], writes=[r_ps[b]])
                p.op("act", lambda: A.activation(out=gs[i][:, blk * 512:(blk + 1) * 512], in_=ps[b][:], func=AF.Exp),
                     reads=[r_ps[b]], writes=[r_g[i]])
            cm = cmA if h < 12 else cmB
            p.op("dve", lambda: V.tensor_tensor(out=gs[i][:], in0=gs[i][:], in1=cm[:], op=ALU.mult),
                 reads=[r_g[i], r_k], writes=[r_g[i]])
            p.dma("sp", FB[h].rearrange("(q c) -> q c", q=128), gs[i][:], reads=[r_g[i]])
        ph.close()

    def gen_W(l, ph):
        wt = [ph.sb("wt%d" % i, [128, 4, D], BF16) for i in range(2)]
        r_wt = [R(), R()]
        n = 0
        for (src, dst, ng) in ((w_out[l], WO16, 4), (w_down[l], WD16, 11)):
            dv = dst.rearrange("j q kc c -> q kc j c")
            for kg in range(ng):
                i = n % 2
                n += 1
                p.dma("pool", wt[i][:], src[kg * 512:(kg + 1) * 512, :].rearrange("(kc q) n -> q kc n", q=128),
                      writes=[r_wt[i]])
                for k4 in range(4):
                    p.dma("sp", dv[:, kg * 4 + k4, :, :], wt[i][:, k4, :].rearrange("q (j c) -> q j c", c=128),
                          reads=[r_wt[i]])
                yield

    def phase_W(l):
        ph = Phase()
        for _ in gen_W(l, ph):
            pass
        ph.close()

    def build_hT(ph, hT, r_hT, src, l, sc, sh):
        xin = [ph.sb("xin%d" % i, [128, KC, 512], F32) for i in range(2)]
        r_xin = [R(), R()]
        sv = src.rearrange("(kc q) t -> q kc t", q=128)
        for tb in range(8):
            i = tb % 2
            p.dma("sp", xin[i][:], sv[:, :, tb * 512:(tb + 1) * 512], writes=[r_xin[i]])
            for kc in range(KC):
                o_ = hT[:, kc, tb * 512:(tb + 1) * 512]
                if kc % 2 == 0:
                    p.op("act", lambda: A.activation(out=o_, in_=xin[i][:, kc, :], func=AF.Identity,
                                                     scale=mod(l, sc, kc), bias=mod(l, sh, kc)),
                         reads=[r_xin[i]], writes=[r_hT[tb]])
                else:
                    p.op("dve", lambda: V.tensor_scalar(out=o_, in0=xin[i][:, kc, :], scalar1=mod(l, sc, kc),
                                                        scalar2=mod(l, sh, kc), op0=ALU.mult, op1=ALU.add),
                         reads=[r_xin[i]], writes=[r_hT[tb]])

    VRANGES = ((1536, 2304, 0), (3584, 4224, 768), (5504, 6144, 1408))

    def vcol_of(col0):
        for (a, b, base) in VRANGES:
            if a <= col0 < b:
                return base + (col0 - a)
        return None

    def phase_QKV(l, src):
        oph = Phase()
        hT = oph.sb("hT", [128, KC, S], BF16)
        r_hT = [R() for _ in range(8)]
        ph = Phase()
        build_hT(ph, hT, r_hT, src, l, "sc1", "sh1")
        ph.close()
        ph = Phase()
        wb = [ph.sb("wb%d" % i, [128, KC, 512], BF16) for i in range(2)]
        r_wb = [R(), R()]
        fst = [ph.sb("fst%d" % i, [128, S], BF16) for i in range(2)]
        r_fst = [R(), R()]
        vst = [ph.sb("vst%d" % i, [128, 32, 128], BF16) for i in range(2)]
        r_vst = [R(), R()]
        ps = [ph.ps("psQ%d" % i) for i in range(6)]
        r_ps = [R() for _ in range(6)]
        npz = 0
        nf = 0
        nv = 0
        for g in range(12):
            i = g % 2
            p.dma("pool", wb[i][:], w_in[l][:, g * 512:(g + 1) * 512].rearrange("(kc q) c -> q kc c", q=128),
                  writes=[r_wb[i]])
            for jj in range(4):
                col0 = (g * 4 + jj) * 128
                vcol = vcol_of(col0)
                wsl = lambda kc: wb[i][:, kc, jj * 128:(jj + 1) * 128]
                if vcol is None:
                    fi = nf % 2
                    nf += 1
                    for tb in range(8):
                        b = npz % 6
                        npz += 1
                        p.mm([(lambda kc=kc: T.matmul(ps[b][:], wsl(kc), hT[:, kc, tb * 512:(tb + 1) * 512],
                                                      start=(kc == 0), stop=(kc == KC - 1))) for kc in range(KC)],
                             reads=[r_wb[i], r_hT[tb]], writes=[r_ps[b]])
                        evac(npz, fst[fi][:, tb * 512:(tb + 1) * 512], ps[b][:], [r_ps[b]], [r_fst[fi]])
                    p.dma("sp", PT[col0:col0 + 128, :], fst[fi][:], reads=[r_fst[fi]])
                else:
                    vi = nv % 2
                    nv += 1
                    for tt in range(32):
                        b = npz % 6
                        npz += 1
                        p.mm([(lambda kc=kc: T.matmul(ps[b][:, 0:128], hT[:, kc, tt * 128:(tt + 1) * 128], wsl(kc),
                                                      start=(kc == 0), stop=(kc == KC - 1))) for kc in range(KC)],
                             reads=[r_wb[i], r_hT[tt // 4]], writes=[r_ps[b]])
                        evac(npz, vst[vi][:, tt, :], ps[b][:, 0:128], [r_ps[b]], [r_vst[vi]])
                    vv = VTM.rearrange("(tt q) c -> q tt c", q=128)
                    for q4 in range(4):
                        p.dma("sp", vv[:, q4 * 8:(q4 + 1) * 8, vcol:vcol + 128], vst[vi][:, q4 * 8:(q4 + 1) * 8, :],
                              reads=[r_vst[vi]])
        ph.close()
        ph = Phase()
        wtail = ph.sb("wtail", [128, KC, 10], BF16)
        r_wtail = R()
        fT = ph.sb("fT", [32, S], F32)
        e2 = ph.sb("e2", [10, S], F32)
        cq = ph.sb("cq", [10, S], BF16)
        ck = ph.sb("ck", [128, 320], F32)
        r_f = R()
        r_e2 = R()
        r_cq = R()
        r_ck = R()
        ps = [ph.ps("psF%d" % i) for i in range(6)]
        r_ps = [R() for _ in range(6)]
        p.dma("pool", wtail[:], w_in[l][:, 6144:6154].rearrange("(kc q) c -> q kc c", q=128), writes=[r_wtail])
        p.op("pool", lambda: G.memset(fT[:], 0.0), writes=[r_f])
        for tb in range(8):
            b = npz % 6
            npz += 1
            p.mm([(lambda kc=kc: T.matmul(ps[b][0:10, :], wtail[:, kc, :], hT[:, kc, tb * 512:(tb + 1) * 512],
                                          start=(kc == 0), stop=(kc == KC - 1))) for kc in range(KC)],
                 reads=[r_wtail, r_hT[tb]], writes=[r_ps[b]])
            p.op("act", lambda: A.activation(out=fT[0:10, tb * 512:(tb + 1) * 512], in_=ps[b][0:10, :],
                                             func=AF.Identity, bias=bfs[0:10, l:l + 1]),
                 reads=[r_ps[b], r_c], writes=[r_f])
        p.op("act", lambda: A.activation(out=e2[0:10, :], in_=fT[0:10, :], func=AF.Exp, scale=-1.0),
             reads=[r_f], writes=[r_e2])
        p.op("act", lambda: A.activation(out=e2[0:10, :], in_=e2[0:10, :], func=AF.Ln, bias=1.0),
             reads=[r_e2], writes=[r_e2])
        p.op("dve", lambda: V.tensor_tensor_scan(out=fT[0:10, :], data0=e2[0:10, :], data1=e2[0:10, :], initial=0.0,
                                                 op0=ALU.add, op1=ALU.bypass), reads=[r_e2, r_f], writes=[r_f])
        p.op("act", lambda: A.activation(out=cq[0:10, :], in_=fT[0:10, :], func=AF.Identity, scale=-1.0 / SCALE),
             reads=[r_f], writes=[r_cq])
        p.dma("sp", CUMQ, cq[0:10, :], reads=[r_cq])
        b = npz % 6
        p.mm([(lambda tt=tt: T.matmul(ps[b][:, tt * 10:(tt + 1) * 10], fT[0:32, tt * 128:(tt + 1) * 128],
                                      ident[0:32, 0:10], start=True, stop=True)) for tt in range(32)],
             reads=[r_f, r_c], writes=[r_ps[b]])
        p.op("dve", lambda: V.tensor_copy(ck[:], ps[b][:, 0:320]), reads=[r_ps[b]], writes=[r_ck])
        p.dma("sp", CUMK, ck[:], reads=[r_ck])
        ph.close()
        oph.close()

    def phase_ATT(l, heads=range(32)):
        ph = Phase()
        QA = [ph.sb("QA%d" % i, [128, S], BF16) for i in range(2)]
        KA = [ph.sb("KA%d" % i, [128, S], BF16) for i in range(2)]
        VA = [ph.sb("VA%d" % i, [128, 32, 65], BF16) for i in range(2)]
        TS = [ph.sb("TS%d" % i, [128, TSW], F32) for i in range(2)]
        OST = [ph.sb("OST%d" % i, [64, S], F32) for i in range(2)]
        LA = 4
        NPS = 5
        NPT = 7
        ET = [ph.sb("ET%d" % i, [128, 512], F32) for i in range(NPS)]
        PTL = [ph.sb("PTL%d" % i, [128, 512], BF16) for i in range(NPT)]
        cneg = ph.sb("cneg", [128, 2048], F32)
        pastneg = ph.sb("pastneg", [128, 512], F32)
        ownfix = ph.sb("ownfix", [128, 512], F32)
        ck = ph.sb("ck", [128, 320], F32)
        gate = ph.sb("gate", [128, 512], F32)
        top8 = ph.sb("top8", [128, 256], F32)
        sel = ph.sb("sel", [128, 512], F32)
        selw = ph.sb("selw", [128, 32 * 80], F32)
        km = ph.sb("km", [128, 16], F32)
        kmh = ph.sb("kmh", [128, 16], BF16)
        kml = ph.sb("kml", [128, 16], BF16)
        rden = ph.sb("rden", [128, 512], F32)
        osb = ph.sb("osb", [64, 512], F32)
        r_QA = [R(), R()]
        r_KA = [R(), R()]
        r_VA = [R(), R()]
        r_TS = [R(), R()]
        r_OST = [R(), R()]
        r_ET = [R() for _ in range(NPS)]
        r_PTL = [R() for _ in range(NPT)]
        r_k = R()
        r_gate = R()
        r_top8 = R()
        r_sel = R()
        r_km = R()
        r_rden = R()
        r_osb = R()
        PSS = [ph.ps("pss%d" % i) for i in range(NPS)]
        r_PSS = [R() for _ in range(NPS)]
        PSO = [ph.ps("pso%d" % i) for i in range(2)]
        r_PSO = [R(), R()]
        PSB = ph.ps("psb")
        r_PSB = R()
        PSG = PSB
        r_PSG = r_PSB
        PSX = PSB
        r_PSX = r_PSB
        p.dma("sp", cneg[:], cneg_in, writes=[r_k])
        p.dma("sp", pastneg[:], pastneg_in, writes=[r_k])
        p.dma("sp", ownfix[:], ownfix_in, writes=[r_k])
        p.dma("sp", ck[:], CUMK, writes=[r_k])
        bsel = ph.sb("bsel", [128, 64], F32)
        p.op("pool", lambda: G.memset(bsel[:], 0.0), writes=[r_k])
        p.op("pool", lambda: G.memset(bsel[64:65, :], 1.0), writes=[r_k])
        p.op("pool", lambda: G.memset(rden[:], 0.0), writes=[r_rden])
        p.op("pool", lambda: G.memset(selw[:], 0.0), writes=[r_sel])
        for i in range(2):
            p.op("pool", lambda: G.memset(VA[i][:, :, 64:65], 1.0), writes=[r_VA[i]])
            p.op("pool", lambda: G.memset(QA[i][:], 0.0), writes=[r_QA[i]])
            p.op("pool", lambda: G.memset(KA[i][:], 0.0), writes=[r_KA[i]])

        def hinfo(hg):
            if hg < 12:
                return ("A", hg * 64, 768 + hg * 64, hg * 64, hg)
            if hg < 22:
                hb = hg - 12
                return ("B", 2304 + hb * 64, 2944 + hb * 64, 768 + hb * 64, hg)
            hc = hg - 22
            return ("C", 4224 + hc * 64, 4864 + hc * 64, 1408 + hc * 64, hc)

        def load(hg, i):
            typ, q0, k0, v0, ridx = hinfo(hg)
            if True:
                p.dma("sp", QA[i][0:64, :], PT[q0:q0 + 64, :], writes=[r_QA[i]])
                p.dma("sp", KA[i][0:64, :], PT[k0:k0 + 64, :], writes=[r_KA[i]])
                if typ == "B":
                    p.op("pool", lambda: G.memset(KA[i][64:96, :], 0.0), writes=[r_KA[i]])
                    p.dma("pool", KA[i][64:80, :], blk1h_in, writes=[r_KA[i]])
                if typ == "C":
                    p.dma("sp", QA[i][64:65, :], CUMQ[ridx:ridx + 1, :], writes=[r_QA[i]])
                    p.op("pool", lambda: G.memset(KA[i][64:96, :], 0.0), writes=[r_KA[i]])
                    p.op("pool", lambda: G.memset(KA[i][64:65, :], 1.0), writes=[r_KA[i]])
            vv = VTM.rearrange("(tt q) c -> q tt c", q=128)
            for q4 in range(4):
                p.dma("sp", VA[i][:, q4 * 8:(q4 + 1) * 8, 0:64], vv[:, q4 * 8:(q4 + 1) * 8, v0:v0 + 64],
                      writes=[r_VA[i]])
            if typ != "C":
                src = bass.AP(tensor=FB.tensor, offset=ridx * 128 * LB + 128, ap=[[LB - 1, 128], [1, TSW]])
                p.dma("sp", TS[i][:], src, writes=[r_TS[i]])

        hl = list(heads)
        wgen = gen_W(l, ph) if len(hl) >= 15 else None
        load(hl[0], 0)
        cnt = 0
        nqb = 0
        pending = []
        for n_h, hg in enumerate(hl):
            i = n_h % 2
            typ, q0, k0, v0, ridx = hinfo(hg)
            while pending:
                pending.pop(0)()
            if n_h + 1 < len(hl):
                load(hl[n_h + 1], (n_h + 1) % 2)
            if wgen is not None and n_h % 2 == 0:
                next(wgen, None)
            K = {"A": 64, "B": 96, "C": 96}[typ]
            if typ == "B":
                p.op("dve", lambda: V.tensor_reduce(out=km[0:64, :],
                                                    in_=KA[i][0:64, :].rearrange("q (n s) -> q n s", s=256),
                                                    axis=AX.X, op=ALU.add), reads=[r_KA[i]], writes=[r_km])
                p.op("dve", lambda: V.tensor_copy(kmh[0:64, :], km[0:64, :]), reads=[r_km], writes=[r_km])
                p.op("dve", lambda: V.tensor_tensor(out=kml[0:64, :], in0=km[0:64, :], in1=kmh[0:64, :],
                                                    op=ALU.subtract), reads=[r_km], writes=[r_km])
                fns = []
                for tt in range(32):
                    fns.append(lambda tt=tt: T.matmul(PSG[:, tt * 16:(tt + 1) * 16],
                                                      QA[i][0:64, tt * 128:(tt + 1) * 128], kmh[0:64, :],
                                                      start=True, stop=False))
                    fns.append(lambda tt=tt: T.matmul(PSG[:, tt * 16:(tt + 1) * 16],
                                                      QA[i][0:64, tt * 128:(tt + 1) * 128], kml[0:64, :],
                                                      start=False, stop=True))
                p.mm(fns, reads=[r_QA[i], r_km], writes=[r_PSG])
                p.op("dve", lambda: V.tensor_tensor(out=gate[:], in0=PSG[:], in1=pastneg[:], op=ALU.add),
                     reads=[r_PSG, r_k], writes=[r_gate])
                for tt in range(32):
                    p.op("dve", lambda: V.max(out=top8[:, tt * 8:(tt + 1) * 8], in_=gate[:, tt * 16:(tt + 1) * 16]),
                         reads=[r_gate], writes=[r_top8])
                thr = top8[:].rearrange("q (t e) -> q t e", e=8)[:, :, 2:3].to_broadcast([128, 32, 16])
                g3 = gate[:].rearrange("q (t e) -> q t e", e=16)
                s3 = sel[:].rearrange("q (t e) -> q t e", e=16)
                p.op("dve", lambda: V.tensor_tensor(out=s3, in0=g3, in1=thr, op=ALU.is_ge),
                     reads=[r_gate, r_top8], writes=[r_sel])
                p.op("dve", lambda: V.tensor_tensor(out=sel[:], in0=sel[:], in1=ownfix[:], op=ALU.max),
                     reads=[r_sel, r_k], writes=[r_sel])
                p.op("dve", lambda: V.tensor_scalar(out=selw[:].rearrange("q (t e) -> q t e", e=80)[:, :, 64:80],
                                                    in0=s3, scalar1=-1.0, scalar2=30000.0,
                                                    op0=ALU.add, op1=ALU.mult), reads=[r_sel], writes=[r_sel])
                for g8 in range(8):
                    p.mm([(lambda t=t: T.transpose(PSX[0:80, t * 128:(t + 1) * 128],
                                                   selw[:, (g8 * 4 + t) * 80:(g8 * 4 + t + 1) * 80], ident[:]))
                          for t in range(4)], reads=[r_sel, r_c], writes=[r_PSX])
                    p.op("act", lambda: A.copy(QA[i][64:80, g8 * 512:(g8 + 1) * 512], PSX[64:80, :]),
                         reads=[r_PSX], writes=[r_QA[i]])
            oi = n_h % 2
            for qb in range(8):
                kt_lo = max(0, 4 * qb - 16) if typ == "A" else 0
                kts = list(range(kt_lo, 4 * qb + 4))
                po = nqb % 2
                nqb += 1
                for n, kt in enumerate(kts):
                    sb_ = cnt % NPS
                    pb = cnt % NPT
                    cnt += 1
                    p.mm([lambda: T.matmul(PSS[sb_][:], KA[i][0:K, kt * 128:(kt + 1) * 128],
                                           QA[i][0:K, qb * 512:(qb + 1) * 512], start=True, stop=True)],
                         reads=[r_KA[i], r_QA[i]], writes=[r_PSS[sb_]])
                    if typ != "C":
                        off = qb * 512 - kt * 128 + 384
                        p.op("act", lambda: A.activation(out=ET[sb_][:], in_=PSS[sb_][:], func=AF.Exp, scale=SCALE),
                             reads=[r_PSS[sb_]], writes=[r_ET[sb_]])
                        p.op("dve", lambda: V.tensor_tensor(out=PTL[pb][:], in0=ET[sb_][:],
                                                            in1=TS[i][:, off:off + 512], op=ALU.mult),
                             reads=[r_ET[sb_], r_TS[i]], writes=[r_PTL[pb]])
                    else:
                        bias = ck[:, kt * 10 + ridx:kt * 10 + ridx + 1]
                        if kt >= 4 * qb:
                            dg = kt - 4 * qb
                            p.op("dve", lambda: V.tensor_tensor(out=ET[sb_][:], in0=PSS[sb_][:],
                                                                in1=cneg[:, dg * 512:(dg + 1) * 512], op=ALU.add),
                                 reads=[r_PSS[sb_], r_k], writes=[r_ET[sb_]])
                            p.op("act", lambda: A.activation(out=PTL[pb][:], in_=ET[sb_][:], func=AF.Exp,
                                                             scale=SCALE, bias=bias),
                                 reads=[r_ET[sb_], r_k], writes=[r_PTL[pb]])
                        else:
                            p.op("act", lambda: A.activation(out=PTL[pb][:], in_=PSS[sb_][:], func=AF.Exp,
                                                             scale=SCALE, bias=bias),
                                 reads=[r_PSS[sb_], r_k], writes=[r_PTL[pb]])

                    def tail(i=i, kt=kt, pb=pb, po=po, n=n, nk=len(kts), qb=qb, oi=oi, hg=hg):
                        p.mm([lambda: T.matmul(PSO[po][0:65, :], VA[i][:, kt, :], PTL[pb][:],
                                               start=(n == 0), stop=(n == nk - 1))],
                             reads=[r_VA[i], r_PTL[pb]], writes=[r_PSO[po]])
                        if n != nk - 1:
                            return
                        p.op("dve", lambda: V.reciprocal(rden[64:65, :], PSO[po][64:65, :]),
                             reads=[r_PSO[po]], writes=[r_rden])
                        p.mm([lambda: T.matmul(PSB[0:64, :], bsel[64:96, 0:64], rden[64:96, :], start=True, stop=True)],
                             reads=[r_rden, r_k], writes=[r_PSB])
                        p.op("act", lambda: A.copy(osb[0:64, :], PSO[po][0:64, :]), reads=[r_PSO[po]], writes=[r_osb])
                        p.op("dve", lambda: V.tensor_tensor(out=OST[oi][0:64, qb * 512:(qb + 1) * 512],
                                                            in0=osb[0:64, :], in1=PSB[0:64, :], op=ALU.mult),
                             reads=[r_osb, r_PSB], writes=[r_OST[oi]])
                        if qb == 7:
                            p.dma("sp", OT[hg * 64:(hg + 1) * 64, :], OST[oi][0:64, :], reads=[r_OST[oi]])

                    pending.append(tail)
                    if len(pending) > LA:
                        pending.pop(0)()
        while pending:
            pending.pop(0)()
        ph.close()

    def ln_block(ph_bufs, tb, l, which_g, lng_off, lnb_off, psm_get, resid_src, dst, final):
        (rT, xc, rbf, sqb, mean, msq, var, rstd, t1, xo, PS1, PS2, PST, osbig,
         r_rT, r_xc, r_rbf, r_sqb, r_st, r_t1, r_xo, r_PS1, r_PS2, r_PST, r_osbig) = ph_bufs
        for j in range(KC):
            jj = j % 2
            p.dma("sp", xc[jj][:], resid_src[j * 128:(j + 1) * 128, tb * 512:(tb + 1) * 512], writes=[r_xc[jj]])
            psm, r_psm = psm_get(j)
            p.op("dve", lambda: V.scalar_tensor_tensor(out=rT[:, j, :], in0=psm[:], scalar=mod(l, which_g, j),
                                                       in1=xc[jj][:], op0=ALU.mult, op1=ALU.add),
                 reads=[r_psm, r_xc[jj]], writes=[r_rT[j]])
            p.op("pool", lambda: G.tensor_copy(rbf[jj][:], rT[:, j, :]), reads=[r_rT[j]], writes=[r_rbf[jj]])
            p.op("act", lambda: A.activation(out=sqb[jj][:], in_=rT[:, j, :], func=AF.Square),
                 reads=[r_rT[j]], writes=[r_sqb[jj]])
            p.mm([lambda: T.matmul(PS1[:], ones_bf[:], rbf[jj][:], start=(j == 0), stop=(j == KC - 1))],
                 reads=[r_rbf[jj], r_c], writes=[r_PS1])
            p.mm([lambda: T.matmul(PS2[:], ones_bf[:], sqb[jj][:], start=(j == 0), stop=(j == KC - 1))],
                 reads=[r_sqb[jj], r_c], writes=[r_PS2])
        p.op("act", lambda: A.activation(out=mean[:], in_=PS1[:], func=AF.Identity, scale=1.0 / D),
             reads=[r_PS1], writes=[r_st])
        p.op("dve", lambda: V.tensor_tensor(out=msq[:], in0=mean[:], in1=mean[:], op=ALU.mult),
             reads=[r_st], writes=[r_st])
        p.op("dve", lambda: V.scalar_tensor_tensor(out=var[:], in0=PS2[:], scalar=1.0 / D, in1=msq[:],
                                                   op0=ALU.mult, op1=ALU.subtract), reads=[r_PS2, r_st], writes=[r_st])
        p.op("dve", lambda: V.tensor_scalar(out=var[:], in0=var[:], scalar1=EPS / (ALPHA * ALPHA), scalar2=None,
                                            op0=ALU.add), reads=[r_st], writes=[r_st])
        p.op("pool", lambda: G.tensor_tensor(out=rstd[:], in0=var[:], in1=mhalf[:], op=ALU.pow),
             reads=[r_st, r_c], writes=[r_st])
        for j in range(KC):
            jj = j % 2
            p.op("dve", lambda: V.tensor_tensor(out=t1[jj][:], in0=rT[:, j, :], in1=mean[:], op=ALU.subtract),
                 reads=[r_rT[j], r_st], writes=[r_t1[jj]])
            p.op("pool", lambda: G.tensor_tensor(out=t1[jj][:], in0=t1[jj][:], in1=rstd[:], op=ALU.mult),
                 reads=[r_t1[jj], r_st], writes=[r_t1[jj]])
            p.op("act", lambda: A.activation(out=xo[jj][:], in_=t1[jj][:], func=AF.Identity,
                                             scale=lnp[:, lng_off + j:lng_off + j + 1],
                                             bias=lnp[:, lnb_off + j:lnb_off + j + 1]),
                 reads=[r_t1[jj], r_c], writes=[r_xo[jj]])
            if not final:
                p.dma("sp", dst[j * 128:(j + 1) * 128, tb * 512:(tb + 1) * 512], xo[jj][:], reads=[r_xo[jj]])
            else:
                pt_ = j % 2
                p.mm([(lambda t=t: T.transpose(PST[pt_][:, t * 128:(t + 1) * 128], xo[jj][:, t * 128:(t + 1) * 128],
                                               ident[:])) for t in range(4)],
                     reads=[r_xo[jj], r_c], writes=[r_PST[pt_]])
                p.op("act", lambda: A.copy(osbig[:, :, j * 128:(j + 1) * 128],
                                           PST[pt_][:].rearrange("q (t c) -> q t c", c=128)),
                     reads=[r_PST[pt_]], writes=[r_osbig])
        if final:
            p.dma("sp", out[tb * 512:(tb + 1) * 512, :].rearrange("(t q) d -> q t d", q=128), osbig[:],
                  reads=[r_osbig])

    def ln_bufs(ph, final):
        rT = ph.sb("rT", [128, KC, 512], F32)
        xc = [ph.sb("xc%d" % i, [128, 512], F32) for i in range(2)]
        rbf = [ph.sb("rbf%d" % i, [128, 512], BF16) for i in range(2)]
        sqb = [ph.sb("sqb%d" % i, [128, 512], BF16) for i in range(2)]
        mean = ph.sb("mean", [128, 512], F32)
        msq = ph.sb("msq", [128, 512], F32)
        var = ph.sb("var", [128, 512], F32)
        rstd = ph.sb("rstd", [128, 512], F32)
        t1 = [ph.sb("t1%d" % i, [128, 512], F32) for i in range(2)]
        xo = [ph.sb("xo%d" % i, [128, 512], F32) for i in range(2)]
        PS1 = ph.ps("ps1")
        PS2 = ph.ps("ps2")
        PST = [ph.ps("pst%d" % i) for i in range(2)] if final else None
        osbig = ph.sb("osbig", [128, 4, D], F32) if final else None
        return (rT, xc, rbf, sqb, mean, msq, var, rstd, t1, xo, PS1, PS2, PST, osbig,
                [R() for _ in range(KC)], [R(), R()], [R(), R()], [R(), R()], R(), [R(), R()], [R(), R()],
                R(), R(), [R(), R()], R())

    def phase_POST(l, resid_src, dst):
        ph = Phase()
        bufs = ln_bufs(ph, False)
        ob = ph.sb("ob", [128, KC, 512], F32)
        sq = [ph.sb("sq%d" % i, [128, 512], BF16) for i in range(2)]
        ysb = ph.sb("ysb", [128, KC, 512], BF16)
        rs = [ph.sb("rs%d" % i, [128, 512], F32) for i in range(3)]
        wo = [ph.sb("wo%d" % i, [128, KC, 128], BF16) for i in range(2)]
        r_ob = R()
        r_sq = [R(), R()]
        r_ysb = R()
        r_rs = [R(), R(), R()]
        r_wo = [R(), R()]
        PG = [ph.ps("pg%d" % i) for i in range(3)]
        r_PG = [R(), R(), R()]
        PM = [ph.ps("pm%d" % i) for i in range(2)]
        r_PM = [R(), R()]
        grp_of = lambda c: 0 if c < 6 else (1 if c < 11 else 2)
        gfirst = (0, 6, 11)
        glast = (5, 10, 15)
        gn = (768.0, 640.0, 640.0)
        ov = OT.rearrange("(c q) t -> q c t", q=128)
        nw = 0
        for tb in range(8):
            p.dma("sp", ob[:], ov[:, :, tb * 512:(tb + 1) * 512], writes=[r_ob])
            for c in range(KC):
                g = grp_of(c)
                p.op("act", lambda: A.activation(out=sq[c % 2][:], in_=ob[:, c, :], func=AF.Square),
                     reads=[r_ob], writes=[r_sq[c % 2]])
                p.mm([lambda: T.matmul(PG[g][:], ones_bf[:], sq[c % 2][:], start=(c == gfirst[g]),
                                       stop=(c == glast[g]))], reads=[r_sq[c % 2], r_c], writes=[r_PG[g]])
            for g in range(3):
                p.op("dve", lambda: V.tensor_scalar(out=rs[g][:], in0=PG[g][:], scalar1=1.0 / gn[g], scalar2=EPS,
                                                    op0=ALU.mult, op1=ALU.add), reads=[r_PG[g]], writes=[r_rs[g]])
                p.op("pool", lambda: G.tensor_tensor(out=rs[g][:], in0=rs[g][:], in1=mhalf[:], op=ALU.pow),
                     reads=[r_rs[g], r_c], writes=[r_rs[g]])
            for c in range(KC):
                g = grp_of(c)
                p.op("dve", lambda: V.scalar_tensor_tensor(out=ysb[:, c, :], in0=ob[:, c, :],
                                                           scalar=gmix[:, l * 16 + c:l * 16 + c + 1], in1=rs[g][:],
                                                           op0=ALU.mult, op1=ALU.mult),
                     reads=[r_ob, r_rs[g], r_c], writes=[r_ysb])

            def psm_get(j):
                nonlocal nw
                wi = nw % 2
                nw += 1
                p.dma("sp", wo[wi][:], WO16[j], writes=[r_wo[wi]])
                p.mm([(lambda kc=kc: T.matmul(PM[wi][:], wo[wi][:, kc, :], ysb[:, kc, :], start=(kc == 0),
                                              stop=(kc == KC - 1))) for kc in range(KC)],
                     reads=[r_wo[wi], r_ysb], writes=[r_PM[wi]])
                return PM[wi], r_PM[wi]

            ln_block(bufs, tb, l, "g1", l * 16, 32 + l * 16, psm_get, resid_src, dst, False)
        ph.close()

    def phase_UP(l, src):
        oph = Phase()
        hT = oph.sb("hT2", [128, KC, S], BF16)
        r_hT = [R() for _ in range(8)]
        ph = Phase()
        build_hT(ph, hT, r_hT, src, l, "sc2", "sh2")
        ph.close()
        ph = Phase()
        wa = [ph.sb("wua%d" % i, [128, KC, 256], BF16) for i in range(2)]
        wb = [ph.sb("wub%d" % i, [128, KC, 256], BF16) for i in range(2)]
        r_w = [R(), R()]
        ua = [ph.sb("ua%d" % i, [128, 514], F32) for i in range(2)]
        ub = [ph.sb("ub%d" % i, [128, 514], F32) for i in range(2)]
        r_ua = [R(), R()]
        r_ub = [R(), R()]
        ta = [ph.sb("ta%d" % i, [128, 512], F32) for i in range(2)]
        tb_ = [ph.sb("tb%d" % i, [128, 512], F32) for i in range(2)]
        r_ta = [R(), R()]
        r_tb = [R(), R()]
        gt = [ph.sb("gt%d" % i, [128, S], BF16) for i in range(2)]
        r_gt = [R(), R()]
        PA = [ph.ps("pa%d" % i) for i in range(3)]
        PB = [ph.ps("pb%d" % i) for i in range(3)]
        r_PA = [R() for _ in range(3)]
        r_PB = [R() for _ in range(3)]
        cw = lambda tap, ci: convw[:, l * 264 + tap * 88 + ci:l * 264 + tap * 88 + ci + 1]
        cbias = lambda ci: convb[:, l * 88 + ci:l * 88 + ci + 1]
        n = 0
        for g2 in range(22):
            wi = g2 % 2
            p.dma("pool", wa[wi][:], w_up[l][:, g2 * 256:(g2 + 1) * 256].rearrange("(kc q) c -> q kc c", q=128),
                  writes=[r_w[wi]])
            p.dma("pool", wb[wi][:],
                  w_up[l][:, DFF + g2 * 256:DFF + (g2 + 1) * 256].rearrange("(kc q) c -> q kc c", q=128),
                  writes=[r_w[wi]])
            for jj in range(2):
                j = g2 * 2 + jj
                gi = j % 2
                for tb in range(8):
                    b = n % 3
                    u = n % 2
                    n += 1
                    p.mm([(lambda kc=kc: T.matmul(PA[b][:], wa[wi][:, kc, jj * 128:(jj + 1) * 128],
                                                  hT[:, kc, tb * 512:(tb + 1) * 512], start=(kc == 0),
                                                  stop=(kc == KC - 1))) for kc in range(KC)],
                         reads=[r_w[wi], r_hT[tb]], writes=[r_PA[b]])
                    p.mm([(lambda kc=kc: T.matmul(PB[b][:], wb[wi][:, kc, jj * 128:(jj + 1) * 128],
                                                  hT[:, kc, tb * 512:(tb + 1) * 512], start=(kc == 0),
                                                  stop=(kc == KC - 1))) for kc in range(KC)],
                         reads=[r_w[wi], r_hT[tb]], writes=[r_PB[b]])
                    for (uu, r_uu, PP, r_PP, tt_, r_tt, ci, e1) in (
                            (ua, r_ua, PA, r_PA, ta, r_ta, j, "act"), (ub, r_ub, PB, r_PB, tb_, r_tb, NFC + j, "dve")):
                        if e1 == "act":
                            p.op("act", lambda: A.copy(uu[u][:, 2:514], PP[b][:]), reads=[r_PP[b]], writes=[r_uu[u]])
                        else:
                            p.op("dve", lambda: V.tensor_copy(uu[u][:, 2:514], PP[b][:]), reads=[r_PP[b]],
                                 writes=[r_uu[u]])
                        if tb == 0:
                            p.op("pool", lambda: G.memset(uu[u][:, 0:2], 0.0), writes=[r_uu[u]])
                        else:
                            p.op("pool", lambda: G.tensor_copy(uu[u][:, 0:2], uu[1 - u][:, 512:514]),
                                 reads=[r_uu[1 - u]], writes=[r_uu[u]])
                        p.op("act", lambda: A.activation(out=tt_[u][:], in_=uu[u][:, 2:514], func=AF.Identity,
                                                         scale=cw(2, ci), bias=cbias(ci)),
                             reads=[r_uu[u], r_c], writes=[r_tt[u]])
                        p.op("dve", lambda: V.scalar_tensor_tensor(out=tt_[u][:], in0=uu[u][:, 1:513], scalar=cw(1, ci),
                                                                   in1=tt_[u][:], op0=ALU.mult, op1=ALU.add),
                             reads=[r_uu[u], r_tt[u], r_c], writes=[r_tt[u]])
                        p.op("dve", lambda: V.scalar_tensor_tensor(out=tt_[u][:], in0=uu[u][:, 0:512], scalar=cw(0, ci),
                                                                   in1=tt_[u][:], op0=ALU.mult, op1=ALU.add),
                             reads=[r_uu[u], r_tt[u], r_c], writes=[r_tt[u]])
                    p.op("act", lambda: A.activation(out=ta[u][:], in_=ta[u][:], func=AF.Silu),
                         reads=[r_ta[u]], writes=[r_ta[u]])
                    p.op("dve", lambda: V.tensor_tensor(out=gt[gi][:, tb * 512:(tb + 1) * 512], in0=ta[u][:],
                                                        in1=tb_[u][:], op=ALU.mult),
                         reads=[r_ta[u], r_tb[u]], writes=[r_gt[gi]])
                p.dma("sp", GT[j * 128:(j + 1) * 128, :], gt[gi][:], reads=[r_gt[gi]])
        ph.close()
        oph.close()

    def phase_DOWN(l, resid_src, dst, final):
        ph = Phase()
        bufs = ln_bufs(ph, final)
        gin = ph.sb("gin", [128, NFC, 512], BF16)
        r_gin = R()
        wd = [ph.sb("wd%d" % i, [128, NFC, 128], BF16) for i in range(2)]
        r_wd = [R(), R()]
        PM = [ph.ps("pmd%d" % i) for i in range(2)]
        r_PM = [R(), R()]
        gv = GT.rearrange("(kc q) t -> q kc t", q=128)
        nw = 0
        for tb in range(8):
            for k4 in range(4):
                p.dma("sp", gin[:, k4 * 11:(k4 + 1) * 11, :], gv[:, k4 * 11:(k4 + 1) * 11, tb * 512:(tb + 1) * 512],
                      writes=[r_gin])

            def psm_get(j):
                nonlocal nw
                wi = nw % 2
                nw += 1
                p.dma("sp", wd[wi][:], WD16[j], writes=[r_wd[wi]])
                p.mm([(lambda kc=kc: T.matmul(PM[wi][:], wd[wi][:, kc, :], gin[:, kc, :], start=(kc == 0),
                                              stop=(kc == NFC - 1))) for kc in range(NFC)],
                     reads=[r_wd[wi], r_gin], writes=[r_PM[wi]])
                return PM[wi], r_PM[wi]

            ln_block(bufs, tb, l, "g2", 64 + l * 16, 96 + l * 16, psm_get, resid_src, dst, final)
        ph.close()

    stop = None
    for d_ in dbg:
        if d_.startswith("stop:"):
            stop = d_[5:]
    def done(tag):
        return stop == tag

    def run():
        if "skip:M" not in dbg:
            phase_M()
        if done("M"): return
        if "skip:T" not in dbg:
            phase_T()
        if done("T"): return
        if "skip:B" not in dbg:
            phase_B()
        if done("B"): return
        cur, oth = XT, X1T
        for l in range(2):
            if "skip:W" in dbg and False:
                phase_W(l)
            phase_QKV(l, XT)
            if done("QKV%d" % l): return
            hs = range(32)
            for d_ in dbg:
                if d_.startswith("heads:"):
                    hs = [int(v) for v in d_[6:].split(".")]
            phase_ATT(l, hs)
            if done("ATT%d" % l): return
            phase_POST(l, XT, X1T)
            if done("POST%d" % l): return
            phase_UP(l, X1T)
            if done("UP%d" % l): return
            phase_DOWN(l, X1T, XT, final=(l == 1))
            if done("DOWN%d" % l): return
    run()
    p.barrier()
    return nc, p


def make_inputs(inputs, b, consts):
    f = lambda a: np.ascontiguousarray(a, dtype=np.float32)
    pl = lambda v: f(np.asarray(v).reshape(-1, 128).T)
    m = {}
    m["x"] = f(inputs["x"][b])
    m["cT"] = pl(inputs["c"][b])
    for l in range(2):
        m["w_ada%d" % l] = f(inputs["w_ada"][l])
        m["bada%d" % l] = pl(inputs["b_ada"][l])
        m["w_in%d" % l] = f(inputs["w_in"][l])
        m["w_out%d" % l] = f(inputs["w_out"][l])
        m["w_up%d" % l] = f(inputs["w_up"][l])
        m["w_down%d" % l] = f(inputs["w_down"][l])
    m["bf"] = f(np.asarray(inputs["b_f"]).T)
    gm = [np.concatenate([inputs["g_mix_a"][l], inputs["g_mix_b"][l], inputs["g_mix_c"][l]]) for l in range(2)]
    m["gmix"] = f(np.concatenate([pl(g) for g in gm], axis=1))
    m["lnp"] = f(np.concatenate([pl(inputs[k][l]) for k in ("ln1_g", "ln1_b", "ln2_g", "ln2_b") for l in range(2)],
                                axis=1))
    cw = []
    for l in range(2):
        for tap in range(3):
            cw.append(pl(inputs["conv_w"][l][tap]))
    m["convw"] = f(np.concatenate(cw, axis=1))
    m["convb"] = f(np.concatenate([pl(inputs["conv_b"][l]) for l in range(2)], axis=1))
    rb = np.asarray(inputs["rel_bias"], dtype=np.float32)
    m["relrep"] = f(np.repeat(rb[:, :, None], 128, axis=2).reshape(32, 22 * 128))
    m.update(consts)
    return m


def kernel(**inputs):
    inputs = {k: np.asarray(v) for k, v in inputs.items()}
    consts = host_constants()
    nc, _ = build()
    per_b = [make_inputs(inputs, b, consts) for b in range(4)]
    idle = {k: np.zeros_like(v) for k, v in per_b[0].items()}
    in_maps = [per_b[c // 2] if c % 2 == 0 else idle for c in range(NCORES)]
    res = run_bass_kernel_spmd(nc, in_maps, core_ids=list(range(NCORES)))
    outs = [np.asarray(res.results[2 * b]["out"], dtype=np.float32) for b in range(4)]
    return np.stack(outs, axis=0)
```

```python
from contextlib import ExitStack
import math
import numpy as np
import concourse.bass as bass
import concourse.mybir as mybir
from concourse.bass_utils import run_bass_kernel_spmd

F32 = mybir.dt.float32
BF16 = mybir.dt.bfloat16
AF = mybir.ActivationFunctionType
ALU = mybir.AluOpType
AX = mybir.AxisListType

S = 4096
D = 2048
KC = 16
DFF = 5632
NFC = 44
INC = 6154
LB = 4608
TSW = 4480
ALPHA = 4.0 ** 0.25
EPS = 1e-5
SCALE = 0.125
NCORES = 8


class R:
    __slots__ = ("name", "w", "rd")

    def __init__(self, name=""):
        self.name = name
        self.w = None
        self.rd = {}


class Prog:
    SAME = True
    NDS = 20

    def __init__(self, nc):
        self.nc = nc
        self.E = {"pe": nc.tensor, "act": nc.scalar, "dve": nc.vector,
                  "pool": nc.gpsimd, "sp": nc.sync}
        self.sem = {}
        self.cnt = {}
        for e in self.E:
            self.sem[e] = nc.alloc_semaphore("s_" + e)
            self.cnt[e] = 0
        self.waited = {e: {} for e in self.E}
        self.dsem = {}
        self.dcnt = {}
        self.dnext = {}
        for e in ("sp", "pool"):
            self.dsem[e] = [nc.alloc_semaphore("d_%s%d" % (e, i)) for i in range(self.NDS)]
            self.dcnt[e] = [0] * self.NDS
            self.dnext[e] = 0
        self.n_ins = 0

    def _wait(self, eng, toks):
        best = {}
        wd = self.waited[eng]
        for (k, h, v) in toks:
            if k == eng and (eng == "pe" or not self.SAME):
                continue
            if wd.get(k, 0) >= v:
                continue
            if k not in best or best[k][1] < v:
                best[k] = (h, v)
        for k, (h, v) in best.items():
            self.E[eng].wait_ge(h, v)
            wd[k] = v
            self.n_ins += 1

    def _deps(self, eng, reads, writes, extra=()):
        toks = list(extra)
        for r in reads:
            if r.w is not None:
                toks.append(r.w)
        for w in writes:
            if w.w is not None:
                toks.append(w.w)
            toks.extend(w.rd.values())
        self._wait(eng, toks)

    def _reg(self, tok, reads, writes):
        for r in reads:
            r.rd[tok[0]] = tok
        for w in writes:
            w.w = tok
            w.rd = {}

    def op(self, eng, fn, reads=(), writes=()):
        self._deps(eng, reads, writes)
        ins = fn()
        self.n_ins += 1
        self.cnt[eng] += 1
        ins.then_inc(self.sem[eng], 1)
        self._reg((eng, self.sem[eng], self.cnt[eng]), reads, writes)

    def mm(self, fns, reads=(), writes=()):
        self._deps("pe", reads, writes)
        ins = None
        for f in fns:
            ins = f()
            self.n_ins += 1
        self.cnt["pe"] += 1
        ins.then_inc(self.sem["pe"], 1)
        self._reg(("pe", self.sem["pe"], self.cnt["pe"]), reads, writes)

    def dma(self, eng, out, in_, reads=(), writes=()):
        i = self.dnext[eng]
        self.dnext[eng] = (i + 1) % self.NDS
        key = "d_%s%d" % (eng, i)
        extra = []
        if self.dcnt[eng][i]:
            extra.append((key, self.dsem[eng][i], self.dcnt[eng][i]))
        self._deps(eng, reads, writes, extra)
        ins = self.E[eng].dma_start(out=out, in_=in_)
        self.n_ins += 1
        self.dcnt[eng][i] += 16
        ins.then_inc(self.dsem[eng][i], 16)
        self._reg((key, self.dsem[eng][i], self.dcnt[eng][i]), reads, writes)

    def barrier(self):
        toks = []
        for e in self.E:
            if self.cnt[e]:
                toks.append((e, self.sem[e], self.cnt[e]))
        for e in self.dsem:
            for i in range(self.NDS):
                if self.dcnt[e][i]:
                    toks.append(("d_%s%d" % (e, i), self.dsem[e][i], self.dcnt[e][i]))
        same = self.SAME
        self.SAME = False
        for e in self.E:
            self._wait(e, toks)
        self.SAME = same


def t5_bucket_np(d):
    n = np.maximum(d, 0)
    nf = np.maximum(n, 1).astype(np.float32)
    large = 16 + (np.log(nf / np.float32(16)) / np.float32(math.log(2048 / 16)) * np.float32(16)).astype(np.int32)
    large = np.minimum(large, 31)
    return np.where(n < 16, n, large)


def host_constants():
    c = {}
    c["ident"] = np.eye(128, dtype=np.float32)
    i = np.arange(LB)
    d = i - 512
    bk = t5_bucket_np(d)
    oh = np.zeros((32, LB), np.float32)
    oh[bk, i] = 1.0
    oh[:, d < 0] = 0.0
    c["oh"] = oh
    cb = (d >= 0).astype(np.float32)
    ca = (((d >= 0) & (d <= 128)).astype(np.float32)
          + ((d >= 0) & (d <= 512) & (d % 4 == 0)).astype(np.float32)
          + ((d >= 0) & (d <= 2048) & (d % 16 == 0)).astype(np.float32))
    c["cmA"] = np.tile(ca[None, :], (128, 1)).astype(np.float32)
    c["cmB"] = np.tile(cb[None, :], (128, 1)).astype(np.float32)
    b1 = np.zeros((16, S), np.float32)
    for n in range(16):
        b1[n, n * 256:(n + 1) * 256] = 1.0
    c["blk1h"] = b1
    pn = np.zeros((128, 32, 16), np.float32)
    of = np.zeros((128, 32, 16), np.float32)
    for tt in range(32):
        qblk = tt // 2
        pn[:, tt, qblk:] = -1e30
        of[:, tt, qblk] = 1.0
    c["pastneg"] = pn.reshape(128, 512)
    c["ownfix"] = of.reshape(128, 512)
    cn = np.zeros((128, 4, 512), np.float32)
    p_ = np.arange(128)[:, None]
    j_ = np.arange(512)[None, :]
    for ii in range(4):
        cn[:, ii, :] = np.where(ii * 128 + p_ <= j_, 0.0, -1e9)
    c["cneg"] = cn.reshape(128, 2048)
    return c


def build(dbg=()):
    nc = bass.Bass("TRN2", target_bir_lowering=False)
    p = Prog(nc)

    def din(name, shape):
        return nc.dram_tensor(name, list(shape), F32, kind="ExternalInput").ap()

    def scratch(name, shape, dt):
        kind = "ExternalOutput" if name in dbg else "Internal"
        return nc.dram_tensor(name, list(shape), dt, kind=kind).ap()

    x_in = din("x", [S, D])
    cT = din("cT", [128, KC])
    w_ada = [din("w_ada%d" % l, [D, 6 * D]) for l in range(2)]
    bada = [din("bada%d" % l, [128, 96]) for l in range(2)]
    w_in = [din("w_in%d" % l, [D, INC]) for l in range(2)]
    bf_in = din("bf", [10, 2])
    gmix_in = din("gmix", [128, 32])
    w_out = [din("w_out%d" % l, [D, D]) for l in range(2)]
    lnp_in = din("lnp", [128, 128])
    w_up = [din("w_up%d" % l, [D, 2 * DFF]) for l in range(2)]
    convw_in = din("convw", [128, 2 * 3 * 88])
    convb_in = din("convb", [128, 2 * 88])
    w_down = [din("w_down%d" % l, [DFF, D]) for l in range(2)]
    relrep_in = din("relrep", [32, 22 * 128])
    ident_in = din("ident", [128, 128])
    oh_in = din("oh", [32, LB])
    cmA_in = din("cmA", [128, LB])
    cmB_in = din("cmB", [128, LB])
    blk1h_in = din("blk1h", [16, S])
    pastneg_in = din("pastneg", [128, 512])
    ownfix_in = din("ownfix", [128, 512])
    cneg_in = din("cneg", [128, 2048])
    out = nc.dram_tensor("out", [S, D], F32, kind="ExternalOutput").ap()

    XT = scratch("XT", [D, S], F32)
    X1T = scratch("X1T", [D, S], F32)
    PT = scratch("PT", [6144, S], BF16)
    VTM = scratch("VTM", [S, D], BF16)
    OT = scratch("OT", [D, S], F32)
    GT = scratch("GT", [DFF, S], BF16)
    FB = scratch("FB", [22, 128 * LB], F32)
    CUMQ = scratch("CUMQ", [10, S], BF16)
    CUMK = scratch("CUMK", [128, 320], F32)
    WD16 = scratch("WD16", [16, 128, NFC, 128], BF16)
    WO16 = scratch("WO16", [16, 128, KC, 128], BF16)
    MODD = scratch("MODD", [2, 128, 96], F32)

    uid = [0]

    class Phase:
        def __init__(self):
            self.st = ExitStack()

        def sb(self, name, shape, dt):
            uid[0] += 1
            return self.st.enter_context(nc.sbuf_tensor("s%d_%s" % (uid[0], name), list(shape), dt))

        def ps(self, name):
            uid[0] += 1
            return self.st.enter_context(nc.psum_tensor("p%d_%s" % (uid[0], name), [128, 512], F32))

        def close(self):
            p.barrier()
            self.st.close()

    V = nc.vector
    A = nc.scalar
    G = nc.gpsimd
    T = nc.tensor

    def evac(i, out_, in_, reads, writes):
        if i % 2 == 0:
            p.op("act", lambda: A.copy(out_, in_), reads, writes)
        else:
            p.op("dve", lambda: V.tensor_copy(out_, in_), reads, writes)

    gph = Phase()
    ident = gph.sb("ident", [128, 128], F32)
    r_c = R("consts")
    ones_bf = gph.sb("ones_bf", [128, 128], BF16)
    ones_f = gph.sb("ones_f", [128, 64], F32)
    mhalf = gph.sb("mhalf", [128, 512], F32)
    modT = gph.sb("modT", [128, 192], F32)
    gmix = gph.sb("gmix", [128, 32], F32)
    lnp = gph.sb("lnp", [128, 128], F32)
    convw = gph.sb("convw", [128, 528], F32)
    convb = gph.sb("convb", [128, 176], F32)
    bfs = gph.sb("bfs", [10, 2], F32)
    p.dma("sp", ident[:], ident_in, writes=[r_c])
    p.dma("sp", gmix[:], gmix_in, writes=[r_c])
    p.dma("sp", lnp[:], lnp_in, writes=[r_c])
    p.dma("sp", convw[:], convw_in, writes=[r_c])
    p.dma("sp", convb[:], convb_in, writes=[r_c])
    p.dma("sp", bfs[:], bf_in, writes=[r_c])
    p.op("dve", lambda: V.memset(ones_bf[:], 1.0), writes=[r_c])
    p.op("dve", lambda: V.memset(ones_f[:], 1.0), writes=[r_c])
    p.op("dve", lambda: V.memset(mhalf[:], -0.5), writes=[r_c])
    p.barrier()

    def phase_T():
        ph = Phase()
        xs = [ph.sb("xs%d" % i, [128, 4, D], F32) for i in range(2)]
        xT = [ph.sb("xTs%d" % i, [128, KC, 512], F32) for i in range(2)]
        r_xs = [R(), R()]
        r_xT = [R(), R()]
        ps = [ph.ps("psT%d" % i) for i in range(4)]
        r_ps = [R() for _ in range(4)]
        for tb in range(8):
            i = tb % 2
            p.dma("sp", xs[i][:], x_in[tb * 512:(tb + 1) * 512, :].rearrange("(t q) d -> q t d", q=128),
                  writes=[r_xs[i]])
            for kc in range(KC):
                b = kc % 4
                p.mm([(lambda t=t: T.transpose(ps[b][:, t * 128:(t + 1) * 128],
                                              xs[i][:, t, kc * 128:(kc + 1) * 128], ident[:]))
                      for t in range(4)], reads=[r_xs[i], r_c], writes=[r_ps[b]])
                evac(kc, xT[i][:, kc, :], ps[b][:], [r_ps[b]], [r_xT[i]])
            p.dma("sp", XT.rearrange("(kc q) t -> q kc t", q=128)[:, :, tb * 512:(tb + 1) * 512], xT[i][:],
                  reads=[r_xT[i]])
        ph.close()

    def phase_M():
        ph = Phase()
        cond = ph.sb("cond", [128, KC], F32)
        bad = ph.sb("bad", [128, 192], F32)
        wa = [ph.sb("wa%d" % i, [128, KC, 512], F32) for i in range(2)]
        r_wa = [R(), R()]
        r_cond = R()
        r_mod = R()
        mps = ph.ps("mps")
        r_mps = R()
        p.dma("sp", cond[:], cT, writes=[r_cond])
        p.dma("sp", bad[:, 0:96], bada[0], writes=[r_cond])
        p.dma("sp", bad[:, 96:192], bada[1], writes=[r_cond])
        p.op("act", lambda: A.activation(out=cond[:], in_=cond[:], func=AF.Silu), reads=[r_cond], writes=[r_cond])
        n = 0
        for l in range(2):
            for jb in range(24):
                i = n % 2
                n += 1
                p.dma("sp", wa[i][:], w_ada[l][:, jb * 512:(jb + 1) * 512].rearrange("(kc q) c -> q kc c", q=128),
                      writes=[r_wa[i]])
                for j4 in range(4):
                    col = jb * 4 + j4
                    p.mm([(lambda kc=kc: T.matmul(mps[:, col:col + 1], wa[i][:, kc, j4 * 128:(j4 + 1) * 128],
                                                  cond[:, kc:kc + 1], start=(kc == 0), stop=(kc == KC - 1)))
                          for kc in range(KC)], reads=[r_wa[i], r_cond], writes=[r_mps])
            o = l * 96
            p.op("dve", lambda: V.tensor_tensor(out=modT[:, o:o + 96], in0=mps[:, 0:96], in1=bad[:, o:o + 96],
                                                op=ALU.add), reads=[r_mps, r_cond], writes=[r_mod])
            for (a, sc) in ((16, 1.0), (32, 1.0 / ALPHA), (64, 1.0), (80, 1.0 / ALPHA)):
                p.op("dve", lambda: V.tensor_scalar(out=modT[:, o + a:o + a + 16], in0=modT[:, o + a:o + a + 16],
                                                    scalar1=1.0, scalar2=sc, op0=ALU.add, op1=ALU.mult),
                     reads=[r_mod], writes=[r_mod])
        if "MODD" in dbg:
            p.dma("sp", MODD.rearrange("l q c -> q l c"), modT[:].rearrange("q (l c) -> q l c", l=2), reads=[r_mod])
        ph.close()

    def mod(l, which, c):
        base = {"sh1": 0, "sc1": 16, "g1": 32, "sh2": 48, "sc2": 64, "g2": 80}[which]
        return modT[:, l * 96 + base + c: l * 96 + base + c + 1]

    def phase_B():
        ph = Phase()
        oh = ph.sb("oh", [32, LB], F32)
        cmA = ph.sb("cmA", [128, LB], F32)
        cmB = ph.sb("cmB", [128, LB], F32)
        rel = ph.sb("rel", [32, 22 * 128], F32)
        gs = [ph.sb("gs%d" % i, [128, LB], F32) for i in range(2)]
        r_g = [R(), R()]
        r_k = R()
        ps = [ph.ps("psB%d" % i) for i in range(4)]
        r_ps = [R() for _ in range(4)]
        p.dma("sp", oh[:], oh_in, writes=[r_k])
        p.dma("sp", cmA[:], cmA_in, writes=[r_k])
        p.dma("sp", cmB[:], cmB_in, writes=[r_k])
        p.dma("sp", rel[:], relrep_in, writes=[r_k])
        n = 0
        for h in range(22):
            i = h % 2
            for blk in range(9):
                b = n % 4
                n += 1
                p.mm([lambda: T.matmul(ps[b][:], rel[:, h * 128:(h + 1) * 128], oh[:, blk * 512:(blk + 1) * 512],
                                       start=True, stop=True)], reads=[r_k], writes=[r_ps[b]])
                p.op("act", lambda: A.activation(out=gs[i][:, blk * 512:(blk + 1) * 512], in_=ps[b][:], func=AF.Exp),
                     reads=[r_ps[b]], writes=[r_g[i]])
            cm = cmA if h < 12 else cmB
            p.op("dve", lambda: V.tensor_tensor(out=gs[i][:], in0=gs[i][:], in1=cm[:], op=ALU.mult),
                 reads=[r_g[i], r_k], writes=[r_g[i]])
            p.dma("sp", FB[h].rearrange("(q c) -> q c", q=128), gs[i][:], reads=[r_g[i]])
        ph.close()

    def gen_W(l, ph):
        wt = [ph.sb("wt%d" % i, [128, 4, D], BF16) for i in range(2)]
        r_wt = [R(), R()]
        n = 0
        for (src, dst, ng) in ((w_out[l], WO16, 4), (w_down[l], WD16, 11)):
            dv = dst.rearrange("j q kc c -> q kc j c")
            for kg in range(ng):
                i = n % 2
                n += 1
                p.dma("pool", wt[i][:], src[kg * 512:(kg + 1) * 512, :].rearrange("(kc q) n -> q kc n", q=128),
                      writes=[r_wt[i]])
                for k4 in range(4):
                    p.dma("sp", dv[:, kg * 4 + k4, :, :], wt[i][:, k4, :].rearrange("q (j c) -> q j c", c=128),
                          reads=[r_wt[i]])
                yield

    def phase_W(l):
        ph = Phase()
        for _ in gen_W(l, ph):
            pass
        ph.close()

    def build_hT(ph, hT, r_hT, src, l, sc, sh):
        xin = [ph.sb("xin%d" % i, [128, KC, 512], F32) for i in range(2)]
        r_xin = [R(), R()]
        sv = src.rearrange("(kc q) t -> q kc t", q=128)
        for tb in range(8):
            i = tb % 2
            p.dma("sp", xin[i][:], sv[:, :, tb * 512:(tb + 1) * 512], writes=[r_xin[i]])
            for kc in range(KC):
                o_ = hT[:, kc, tb * 512:(tb + 1) * 512]
                if kc % 2 == 0:
                    p.op("act", lambda: A.activation(out=o_, in_=xin[i][:, kc, :], func=AF.Identity,
                                                     scale=mod(l, sc, kc), bias=mod(l, sh, kc)),
                         reads=[r_xin[i]], writes=[r_hT[tb]])
                else:
                    p.op("dve", lambda: V.tensor_scalar(out=o_, in0=xin[i][:, kc, :], scalar1=mod(l, sc, kc),
                                                        scalar2=mod(l, sh, kc), op0=ALU.mult, op1=ALU.add),
                         reads=[r_xin[i]], writes=[r_hT[tb]])

    VRANGES = ((1536, 2304, 0), (3584, 4224, 768), (5504, 6144, 1408))

    def vcol_of(col0):
        for (a, b, base) in VRANGES:
            if a <= col0 < b:
                return base + (col0 - a)
        return None

    def phase_QKV(l, src):
        oph = Phase()
        hT = oph.sb("hT", [128, KC, S], BF16)
        r_hT = [R() for _ in range(8)]
        ph = Phase()
        build_hT(ph, hT, r_hT, src, l, "sc1", "sh1")
        ph.close()
        ph = Phase()
        wb = [ph.sb("wb%d" % i, [128, KC, 512], BF16) for i in range(2)]
        r_wb = [R(), R()]
        fst = [ph.sb("fst%d" % i, [128, S], BF16) for i in range(2)]
        r_fst = [R(), R()]
        vst = [ph.sb("vst%d" % i, [128, 8, 512], BF16) for i in range(2)]
        r_vst = [R(), R()]
        ps = [ph.ps("psQ%d" % i) for i in range(6)]
        r_ps = [R() for _ in range(6)]
        npz = 0
        nf = 0
        nv = 0
        for g in range(12):
            i = g % 2
            p.dma("pool", wb[i][:], w_in[l][:, g * 512:(g + 1) * 512].rearrange("(kc q) c -> q kc c", q=128),
                  writes=[r_wb[i]])
            for jj in range(4):
                col0 = (g * 4 + jj) * 128
                vcol = vcol_of(col0)
                wsl = lambda kc: wb[i][:, kc, jj * 128:(jj + 1) * 128]
                if vcol is None:
                    fi = nf % 2
                    nf += 1
                    for tb in range(8):
                        b = npz % 6
                        npz += 1
                        p.mm([(lambda kc=kc: T.matmul(ps[b][:], wsl(kc), hT[:, kc, tb * 512:(tb + 1) * 512],
                                                      start=(kc == 0), stop=(kc == KC - 1))) for kc in range(KC)],
                             reads=[r_wb[i], r_hT[tb]], writes=[r_ps[b]])
                        evac(npz, fst[fi][:, tb * 512:(tb + 1) * 512], ps[b][:], [r_ps[b]], [r_fst[fi]])
                    p.dma("sp", PT[col0:col0 + 128, :], fst[fi][:], reads=[r_fst[fi]])
                else:
                    full = g in (3, 7, 11)
                    if full and jj > 0:
                        continue
                    nw_ = 512 if full else 128
                    vv = VTM.rearrange("(tt q) c -> q tt c", q=128)
                    for t8 in range(4):
                        vi = nv % 2
                        nv += 1
                        for t_ in range(8):
                            tt = t8 * 8 + t_
                            b = npz % 6
                            npz += 1
                            p.mm([(lambda kc=kc: T.matmul(ps[b][:, 0:nw_], hT[:, kc, tt * 128:(tt + 1) * 128],
                                                          wb[i][:, kc, jj * 128:jj * 128 + nw_],
                                                          start=(kc == 0), stop=(kc == KC - 1))) for kc in range(KC)],
                                 reads=[r_wb[i], r_hT[tt // 4]], writes=[r_ps[b]])
                            evac(npz, vst[vi][:, t_, 0:nw_], ps[b][:, 0:nw_], [r_ps[b]], [r_vst[vi]])
                        p.dma("sp", vv[:, t8 * 8:(t8 + 1) * 8, vcol:vcol + nw_], vst[vi][:, :, 0:nw_],
                              reads=[r_vst[vi]])
        ph.close()
        ph = Phase()
        wtail = ph.sb("wtail", [128, KC, 10], BF16)
        r_wtail = R()
        fT = ph.sb("fT", [32, S], F32)
        e2 = ph.sb("e2", [10, S], F32)
        cq = ph.sb("cq", [10, S], BF16)
        ck = ph.sb("ck", [128, 320], F32)
        r_f = R()
        r_e2 = R()
        r_cq = R()
        r_ck = R()
        ps = [ph.ps("psF%d" % i) for i in range(6)]
        r_ps = [R() for _ in range(6)]
        p.dma("pool", wtail[:], w_in[l][:, 6144:6154].rearrange("(kc q) c -> q kc c", q=128), writes=[r_wtail])
        p.op("pool", lambda: G.memset(fT[:], 0.0), writes=[r_f])
        for tb in range(8):
            b = npz % 6
            npz += 1
            p.mm([(lambda kc=kc: T.matmul(ps[b][0:10, :], wtail[:, kc, :], hT[:, kc, tb * 512:(tb + 1) * 512],
                                          start=(kc == 0), stop=(kc == KC - 1))) for kc in range(KC)],
                 reads=[r_wtail, r_hT[tb]], writes=[r_ps[b]])
            p.op("act", lambda: A.activation(out=fT[0:10, tb * 512:(tb + 1) * 512], in_=ps[b][0:10, :],
                                             func=AF.Identity, bias=bfs[0:10, l:l + 1]),
                 reads=[r_ps[b], r_c], writes=[r_f])
        p.op("act", lambda: A.activation(out=e2[0:10, :], in_=fT[0:10, :], func=AF.Exp, scale=-1.0),
             reads=[r_f], writes=[r_e2])
        p.op("act", lambda: A.activation(out=e2[0:10, :], in_=e2[0:10, :], func=AF.Ln, bias=1.0),
             reads=[r_e2], writes=[r_e2])
        p.op("dve", lambda: V.tensor_tensor_scan(out=fT[0:10, :], data0=e2[0:10, :], data1=e2[0:10, :], initial=0.0,
                                                 op0=ALU.add, op1=ALU.bypass), reads=[r_e2, r_f], writes=[r_f])
        p.op("act", lambda: A.activation(out=cq[0:10, :], in_=fT[0:10, :], func=AF.Identity, scale=-1.0 / SCALE),
             reads=[r_f], writes=[r_cq])
        p.dma("sp", CUMQ, cq[0:10, :], reads=[r_cq])
        b = npz % 6
        p.mm([(lambda tt=tt: T.matmul(ps[b][:, tt * 10:(tt + 1) * 10], fT[0:32, tt * 128:(tt + 1) * 128],
                                      ident[0:32, 0:10], start=True, stop=True)) for tt in range(32)],
             reads=[r_f, r_c], writes=[r_ps[b]])
        p.op("dve", lambda: V.tensor_copy(ck[:], ps[b][:, 0:320]), reads=[r_ps[b]], writes=[r_ck])
        p.dma("sp", CUMK, ck[:], reads=[r_ck])
        ph.close()
        oph.close()

    def phase_ATT(l, heads=range(32)):
        ph = Phase()
        QA = [ph.sb("QA%d" % i, [128, S], BF16) for i in range(2)]
        KA = [ph.sb("KA%d" % i, [128, S], BF16) for i in range(2)]
        VA = [ph.sb("VA%d" % i, [128, 32, 65], BF16) for i in range(2)]
        TS = [ph.sb("TS%d" % i, [128, TSW], F32) for i in range(2)]
        OST = [ph.sb("OST%d" % i, [64, S], F32) for i in range(2)]
        LA = 4
        NPS = 5
        NPT = 7
        ET = [ph.sb("ET%d" % i, [128, 512], F32) for i in range(NPS)]
        PTL = [ph.sb("PTL%d" % i, [128, 512], BF16) for i in range(NPT)]
        cneg = ph.sb("cneg", [128, 2048], F32)
        pastneg = ph.sb("pastneg", [128, 512], F32)
        ownfix = ph.sb("ownfix", [128, 512], F32)
        ck = ph.sb("ck", [128, 320], F32)
        gate = ph.sb("gate", [128, 512], F32)
        top8 = ph.sb("top8", [128, 256], F32)
        sel = ph.sb("sel", [128, 512], F32)
        selw = ph.sb("selw", [128, 32 * 80], F32)
        km = ph.sb("km", [128, 16], F32)
        kmh = ph.sb("kmh", [128, 16], BF16)
        kml = ph.sb("kml", [128, 16], BF16)
        rden = ph.sb("rden", [128, 512], F32)
        osb = ph.sb("osb", [64, 512], F32)
        r_QA = [R(), R()]
        r_KA = [R(), R()]
        r_VA = [R(), R()]
        r_TS = [R(), R()]
        r_OST = [R(), R()]
        r_ET = [R() for _ in range(NPS)]
        r_PTL = [R() for _ in range(NPT)]
        r_k = R()
        r_gate = R()
        r_top8 = R()
        r_sel = R()
        r_km = R()
        r_rden = R()
        r_osb = R()
        PSS = [ph.ps("pss%d" % i) for i in range(NPS)]
        r_PSS = [R() for _ in range(NPS)]
        PSO = [ph.ps("pso%d" % i) for i in range(2)]
        r_PSO = [R(), R()]
        PSB = ph.ps("psb")
        r_PSB = R()
        PSG = PSB
        r_PSG = r_PSB
        PSX = PSB
        r_PSX = r_PSB
        p.dma("sp", cneg[:], cneg_in, writes=[r_k])
        p.dma("sp", pastneg[:], pastneg_in, writes=[r_k])
        p.dma("sp", ownfix[:], ownfix_in, writes=[r_k])
        p.dma("sp", ck[:], CUMK, writes=[r_k])
        bsel = ph.sb("bsel", [128, 64], F32)
        p.op("pool", lambda: G.memset(bsel[:], 0.0), writes=[r_k])
        p.op("pool", lambda: G.memset(bsel[64:65, :], 1.0), writes=[r_k])
        p.op("pool", lambda: G.memset(rden[:], 0.0), writes=[r_rden])
        p.op("pool", lambda: G.memset(selw[:], 0.0), writes=[r_sel])
        for i in range(2):
            p.op("pool", lambda: G.memset(VA[i][:, :, 64:65], 1.0), writes=[r_VA[i]])
            p.op("pool", lambda: G.memset(QA[i][:], 0.0), writes=[r_QA[i]])
            p.op("pool", lambda: G.memset(KA[i][:], 0.0), writes=[r_KA[i]])

        def hinfo(hg):
            if hg < 12:
                return ("A", hg * 64, 768 + hg * 64, hg * 64, hg)
            if hg < 22:
                hb = hg - 12
                return ("B", 2304 + hb * 64, 2944 + hb * 64, 768 + hb * 64, hg)
            hc = hg - 22
            return ("C", 4224 + hc * 64, 4864 + hc * 64, 1408 + hc * 64, hc)

        def load(hg, i):
            typ, q0, k0, v0, ridx = hinfo(hg)
            if True:
                p.dma("sp", QA[i][0:64, :], PT[q0:q0 + 64, :], writes=[r_QA[i]])
                p.dma("sp", KA[i][0:64, :], PT[k0:k0 + 64, :], writes=[r_KA[i]])
                if typ == "B":
                    p.op("pool", lambda: G.memset(KA[i][64:96, :], 0.0), writes=[r_KA[i]])
                    p.dma("pool", KA[i][64:80, :], blk1h_in, writes=[r_KA[i]])
                if typ == "C":
                    p.dma("sp", QA[i][64:65, :], CUMQ[ridx:ridx + 1, :], writes=[r_QA[i]])
                    p.op("pool", lambda: G.memset(KA[i][64:96, :], 0.0), writes=[r_KA[i]])
                    p.op("pool", lambda: G.memset(KA[i][64:65, :], 1.0), writes=[r_KA[i]])
            vv = VTM.rearrange("(tt q) c -> q tt c", q=128)
            for q4 in range(4):
                p.dma("sp", VA[i][:, q4 * 8:(q4 + 1) * 8, 0:64], vv[:, q4 * 8:(q4 + 1) * 8, v0:v0 + 64],
                      writes=[r_VA[i]])
            if typ != "C":
                src = bass.AP(tensor=FB.tensor, offset=ridx * 128 * LB + 128, ap=[[LB - 1, 128], [1, TSW]])
                p.dma("sp", TS[i][:], src, writes=[r_TS[i]])

        hl = list(heads)
        wgen = gen_W(l, ph) if len(hl) >= 15 else None
        load(hl[0], 0)
        cnt = 0
        nqb = 0
        pending = []
        for n_h, hg in enumerate(hl):
            i = n_h % 2
            typ, q0, k0, v0, ridx = hinfo(hg)
            while pending:
                pending.pop(0)()
            if n_h + 1 < len(hl):
                load(hl[n_h + 1], (n_h + 1) % 2)
            if wgen is not None and n_h % 2 == 0:
                next(wgen, None)
            K = {"A": 64, "B": 96, "C": 96}[typ]
            if typ == "B":
                p.op("dve", lambda: V.tensor_reduce(out=km[0:64, :],
                                                    in_=KA[i][0:64, :].rearrange("q (n s) -> q n s", s=256),
                                                    axis=AX.X, op=ALU.add), reads=[r_KA[i]], writes=[r_km])
                p.op("dve", lambda: V.tensor_copy(kmh[0:64, :], km[0:64, :]), reads=[r_km], writes=[r_km])
                p.op("dve", lambda: V.tensor_tensor(out=kml[0:64, :], in0=km[0:64, :], in1=kmh[0:64, :],
                                                    op=ALU.subtract), reads=[r_km], writes=[r_km])
                fns = []
                for tt in range(32):
                    fns.append(lambda tt=tt: T.matmul(PSG[:, tt * 16:(tt + 1) * 16],
                                                      QA[i][0:64, tt * 128:(tt + 1) * 128], kmh[0:64, :],
                                                      start=True, stop=False))
                    fns.append(lambda tt=tt: T.matmul(PSG[:, tt * 16:(tt + 1) * 16],
                                                      QA[i][0:64, tt * 128:(tt + 1) * 128], kml[0:64, :],
                                                      start=False, stop=True))
                p.mm(fns, reads=[r_QA[i], r_km], writes=[r_PSG])
                p.op("dve", lambda: V.tensor_tensor(out=gate[:], in0=PSG[:], in1=pastneg[:], op=ALU.add),
                     reads=[r_PSG, r_k], writes=[r_gate])
                for tt in range(32):
                    p.op("dve", lambda: V.max(out=top8[:, tt * 8:(tt + 1) * 8], in_=gate[:, tt * 16:(tt + 1) * 16]),
                         reads=[r_gate], writes=[r_top8])
                thr = top8[:].rearrange("q (t e) -> q t e", e=8)[:, :, 2:3].to_broadcast([128, 32, 16])
                g3 = gate[:].rearrange("q (t e) -> q t e", e=16)
                s3 = sel[:].rearrange("q (t e) -> q t e", e=16)
                p.op("dve", lambda: V.tensor_tensor(out=s3, in0=g3, in1=thr, op=ALU.is_ge),
                     reads=[r_gate, r_top8], writes=[r_sel])
                p.op("dve", lambda: V.tensor_tensor(out=sel[:], in0=sel[:], in1=ownfix[:], op=ALU.max),
                     reads=[r_sel, r_k], writes=[r_sel])
                p.op("dve", lambda: V.tensor_scalar(out=selw[:].rearrange("q (t e) -> q t e", e=80)[:, :, 64:80],
                                                    in0=s3, scalar1=-1.0, scalar2=30000.0,
                                                    op0=ALU.add, op1=ALU.mult), reads=[r_sel], writes=[r_sel])
                for g8 in range(8):
                    p.mm([(lambda t=t: T.transpose(PSX[0:80, t * 128:(t + 1) * 128],
                                                   selw[:, (g8 * 4 + t) * 80:(g8 * 4 + t + 1) * 80], ident[:]))
                          for t in range(4)], reads=[r_sel, r_c], writes=[r_PSX])
                    p.op("act", lambda: A.copy(QA[i][64:80, g8 * 512:(g8 + 1) * 512], PSX[64:80, :]),
                         reads=[r_PSX], writes=[r_QA[i]])
            oi = n_h % 2
            for qb in range(8):
                kt_lo = max(0, 4 * qb - 16) if typ == "A" else 0
                kts = list(range(kt_lo, 4 * qb + 4))
                po = nqb % 2
                nqb += 1
                for n, kt in enumerate(kts):
                    sb_ = cnt % NPS
                    pb = cnt % NPT
                    cnt += 1
                    p.mm([lambda: T.matmul(PSS[sb_][:], KA[i][0:K, kt * 128:(kt + 1) * 128],
                                           QA[i][0:K, qb * 512:(qb + 1) * 512], start=True, stop=True)],
                         reads=[r_KA[i], r_QA[i]], writes=[r_PSS[sb_]])
                    if typ != "C":
                        off = qb * 512 - kt * 128 + 384
                        p.op("act", lambda: A.activation(out=ET[sb_][:], in_=PSS[sb_][:], func=AF.Exp, scale=SCALE),
                             reads=[r_PSS[sb_]], writes=[r_ET[sb_]])
                        p.op("dve", lambda: V.tensor_tensor(out=PTL[pb][:], in0=ET[sb_][:],
                                                            in1=TS[i][:, off:off + 512], op=ALU.mult),
                             reads=[r_ET[sb_], r_TS[i]], writes=[r_PTL[pb]])
                    else:
                        bias = ck[:, kt * 10 + ridx:kt * 10 + ridx + 1]
                        if kt >= 4 * qb:
                            dg = kt - 4 * qb
                            p.op("dve", lambda: V.tensor_tensor(out=ET[sb_][:], in0=PSS[sb_][:],
                                                                in1=cneg[:, dg * 512:(dg + 1) * 512], op=ALU.add),
                                 reads=[r_PSS[sb_], r_k], writes=[r_ET[sb_]])
                            p.op("act", lambda: A.activation(out=PTL[pb][:], in_=ET[sb_][:], func=AF.Exp,
                                                             scale=SCALE, bias=bias),
                                 reads=[r_ET[sb_], r_k], writes=[r_PTL[pb]])
                        else:
                            p.op("act", lambda: A.activation(out=PTL[pb][:], in_=PSS[sb_][:], func=AF.Exp,
                                                             scale=SCALE, bias=bias),
                                 reads=[r_PSS[sb_], r_k], writes=[r_PTL[pb]])

                    def tail(i=i, kt=kt, pb=pb, po=po, n=n, nk=len(kts), qb=qb, oi=oi, hg=hg):
                        p.mm([lambda: T.matmul(PSO[po][0:65, :], VA[i][:, kt, :], PTL[pb][:],
                                               start=(n == 0), stop=(n == nk - 1))],
                             reads=[r_VA[i], r_PTL[pb]], writes=[r_PSO[po]])
                        if n != nk - 1:
                            return
                        p.op("dve", lambda: V.reciprocal(rden[64:65, :], PSO[po][64:65, :]),
                             reads=[r_PSO[po]], writes=[r_rden])
                        p.mm([lambda: T.matmul(PSB[0:64, :], bsel[64:96, 0:64], rden[64:96, :], start=True, stop=True)],
                             reads=[r_rden, r_k], writes=[r_PSB])
                        p.op("act", lambda: A.copy(osb[0:64, :], PSO[po][0:64, :]), reads=[r_PSO[po]], writes=[r_osb])
                        p.op("dve", lambda: V.tensor_tensor(out=OST[oi][0:64, qb * 512:(qb + 1) * 512],
                                                            in0=osb[0:64, :], in1=PSB[0:64, :], op=ALU.mult),
                             reads=[r_osb, r_PSB], writes=[r_OST[oi]])
                        if qb == 7:
                            p.dma("sp", OT[hg * 64:(hg + 1) * 64, :], OST[oi][0:64, :], reads=[r_OST[oi]])

                    pending.append(tail)
                    if len(pending) > LA:
                        pending.pop(0)()
        while pending:
            pending.pop(0)()
        ph.close()

    def ln_block(ph_bufs, tb, l, which_g, lng_off, lnb_off, psm_get, resid_src, dst, final):
        (rT, xc, rbf, sqb, mean, msq, var, rstd, t1, xo, PS1, PS2, PST, osbig,
         r_rT, r_xc, r_rbf, r_sqb, r_st, r_t1, r_xo, r_PS1, r_PS2, r_PST, r_osbig) = ph_bufs
        spend = []
        for j in range(KC):
            jj = j % 2
            p.dma("sp", xc[jj][:], resid_src[j * 128:(j + 1) * 128, tb * 512:(tb + 1) * 512], writes=[r_xc[jj]])
            psm, r_psm = psm_get(j)
            p.op("dve", lambda: V.scalar_tensor_tensor(out=rT[:, j, :], in0=psm[:], scalar=mod(l, which_g, j),
                                                       in1=xc[jj][:], op0=ALU.mult, op1=ALU.add),
                 reads=[r_psm, r_xc[jj]], writes=[r_rT[j]])
            j3 = j % 3
            p.op("pool", lambda: G.tensor_copy(rbf[j3][:], rT[:, j, :]), reads=[r_rT[j]], writes=[r_rbf[j3]])
            p.op("act", lambda: A.activation(out=sqb[j3][:], in_=rT[:, j, :], func=AF.Square),
                 reads=[r_rT[j]], writes=[r_sqb[j3]])

            def stats(j=j, j3=j3):
                p.mm([lambda: T.matmul(PS1[:], ones_bf[:], rbf[j3][:], start=(j == 0), stop=(j == KC - 1))],
                     reads=[r_rbf[j3], r_c], writes=[r_PS1])
                p.mm([lambda: T.matmul(PS2[:], ones_bf[:], sqb[j3][:], start=(j == 0), stop=(j == KC - 1))],
                     reads=[r_sqb[j3], r_c], writes=[r_PS2])

            if spend:
                spend.pop(0)()
            spend.append(stats)
        while spend:
            spend.pop(0)()
        p.op("act", lambda: A.activation(out=mean[:], in_=PS1[:], func=AF.Identity, scale=1.0 / D),
             reads=[r_PS1], writes=[r_st])
        p.op("dve", lambda: V.tensor_tensor(out=msq[:], in0=mean[:], in1=mean[:], op=ALU.mult),
             reads=[r_st], writes=[r_st])
        p.op("dve", lambda: V.scalar_tensor_tensor(out=var[:], in0=PS2[:], scalar=1.0 / D, in1=msq[:],
                                                   op0=ALU.mult, op1=ALU.subtract), reads=[r_PS2, r_st], writes=[r_st])
        p.op("dve", lambda: V.tensor_scalar(out=var[:], in0=var[:], scalar1=EPS / (ALPHA * ALPHA), scalar2=None,
                                            op0=ALU.add), reads=[r_st], writes=[r_st])
        p.op("pool", lambda: G.tensor_tensor(out=rstd[:], in0=var[:], in1=mhalf[:], op=ALU.pow),
             reads=[r_st, r_c], writes=[r_st])
        for j in range(KC):
            jj = j % 2
            p.op("dve", lambda: V.tensor_tensor(out=t1[jj][:], in0=rT[:, j, :], in1=mean[:], op=ALU.subtract),
                 reads=[r_rT[j], r_st], writes=[r_t1[jj]])
            p.op("pool", lambda: G.tensor_tensor(out=t1[jj][:], in0=t1[jj][:], in1=rstd[:], op=ALU.mult),
                 reads=[r_t1[jj], r_st], writes=[r_t1[jj]])
            p.op("act", lambda: A.activation(out=xo[jj][:], in_=t1[jj][:], func=AF.Identity,
                                             scale=lnp[:, lng_off + j:lng_off + j + 1],
                                             bias=lnp[:, lnb_off + j:lnb_off + j + 1]),
                 reads=[r_t1[jj], r_c], writes=[r_xo[jj]])
            if not final:
                p.dma("sp", dst[j * 128:(j + 1) * 128, tb * 512:(tb + 1) * 512], xo[jj][:], reads=[r_xo[jj]])
            else:
                pt_ = j % 2
                p.mm([(lambda t=t: T.transpose(PST[pt_][:, t * 128:(t + 1) * 128], xo[jj][:, t * 128:(t + 1) * 128],
                                               ident[:])) for t in range(4)],
                     reads=[r_xo[jj], r_c], writes=[r_PST[pt_]])
                p.op("act", lambda: A.copy(osbig[:, :, j * 128:(j + 1) * 128],
                                           PST[pt_][:].rearrange("q (t c) -> q t c", c=128)),
                     reads=[r_PST[pt_]], writes=[r_osbig])
        if final:
            p.dma("sp", out[tb * 512:(tb + 1) * 512, :].rearrange("(t q) d -> q t d", q=128), osbig[:],
                  reads=[r_osbig])

    def ln_bufs(ph, final):
        rT = ph.sb("rT", [128, KC, 512], F32)
        xc = [ph.sb("xc%d" % i, [128, 512], F32) for i in range(2)]
        rbf = [ph.sb("rbf%d" % i, [128, 512], BF16) for i in range(3)]
        sqb = [ph.sb("sqb%d" % i, [128, 512], BF16) for i in range(3)]
        mean = ph.sb("mean", [128, 512], F32)
        msq = ph.sb("msq", [128, 512], F32)
        var = ph.sb("var", [128, 512], F32)
        rstd = ph.sb("rstd", [128, 512], F32)
        t1 = [ph.sb("t1%d" % i, [128, 512], F32) for i in range(2)]
        xo = [ph.sb("xo%d" % i, [128, 512], F32) for i in range(2)]
        PS1 = ph.ps("ps1")
        PS2 = ph.ps("ps2")
        PST = [ph.ps("pst%d" % i) for i in range(2)] if final else None
        osbig = ph.sb("osbig", [128, 4, D], F32) if final else None
        return (rT, xc, rbf, sqb, mean, msq, var, rstd, t1, xo, PS1, PS2, PST, osbig,
                [R() for _ in range(KC)], [R(), R()], [R(), R(), R()], [R(), R(), R()], R(), [R(), R()], [R(), R()],
                R(), R(), [R(), R()], R())

    def phase_POST(l, resid_src, dst):
        ph = Phase()
        bufs = ln_bufs(ph, False)
        ob = ph.sb("ob", [128, KC, 512], F32)
        sq = [ph.sb("sq%d" % i, [128, 512], BF16) for i in range(2)]
        ysb = ph.sb("ysb", [128, KC, 512], BF16)
        rs = [ph.sb("rs%d" % i, [128, 512], F32) for i in range(3)]
        wo = [ph.sb("wo%d" % i, [128, KC, 128], BF16) for i in range(3)]
        r_ob = R()
        r_sq = [R(), R()]
        r_ysb = R()
        r_rs = [R(), R(), R()]
        r_wo = [R(), R(), R()]
        PG = [ph.ps("pg%d" % i) for i in range(3)]
        r_PG = [R(), R(), R()]
        PM = [ph.ps("pm%d" % i) for i in range(2)]
        r_PM = [R(), R()]
        grp_of = lambda c: 0 if c < 6 else (1 if c < 11 else 2)
        gfirst = (0, 6, 11)
        glast = (5, 10, 15)
        gn = (768.0, 640.0, 640.0)
        ov = OT.rearrange("(c q) t -> q c t", q=128)
        nw = 0
        for tb in range(8):
            p.dma("sp", ob[:], ov[:, :, tb * 512:(tb + 1) * 512], writes=[r_ob])
            for c in range(KC):
                g = grp_of(c)
                p.op("act", lambda: A.activation(out=sq[c % 2][:], in_=ob[:, c, :], func=AF.Square),
                     reads=[r_ob], writes=[r_sq[c % 2]])
                p.mm([lambda: T.matmul(PG[g][:], ones_bf[:], sq[c % 2][:], start=(c == gfirst[g]),
                                       stop=(c == glast[g]))], reads=[r_sq[c % 2], r_c], writes=[r_PG[g]])
            for g in range(3):
                p.op("dve", lambda: V.tensor_scalar(out=rs[g][:], in0=PG[g][:], scalar1=1.0 / gn[g], scalar2=EPS,
                                                    op0=ALU.mult, op1=ALU.add), reads=[r_PG[g]], writes=[r_rs[g]])
                p.op("pool", lambda: G.tensor_tensor(out=rs[g][:], in0=rs[g][:], in1=mhalf[:], op=ALU.pow),
                     reads=[r_rs[g], r_c], writes=[r_rs[g]])
            for c in range(KC):
                g = grp_of(c)
                p.op("dve", lambda: V.scalar_tensor_tensor(out=ysb[:, c, :], in0=ob[:, c, :],
                                                           scalar=gmix[:, l * 16 + c:l * 16 + c + 1], in1=rs[g][:],
                                                           op0=ALU.mult, op1=ALU.mult),
                     reads=[r_ob, r_rs[g], r_c], writes=[r_ysb])

            def psm_get(j):
                nonlocal nw
                wi = nw % 3
                pm = nw % 2
                nw += 1
                p.dma("sp", wo[wi][:], WO16[j], writes=[r_wo[wi]])
                p.mm([(lambda kc=kc: T.matmul(PM[pm][:], wo[wi][:, kc, :], ysb[:, kc, :], start=(kc == 0),
                                              stop=(kc == KC - 1))) for kc in range(KC)],
                     reads=[r_wo[wi], r_ysb], writes=[r_PM[pm]])
                return PM[pm], r_PM[pm]

            ln_block(bufs, tb, l, "g1", l * 16, 32 + l * 16, psm_get, resid_src, dst, False)
        ph.close()

    def phase_UP(l, src):
        oph = Phase()
        hT = oph.sb("hT2", [128, KC, S], BF16)
        r_hT = [R() for _ in range(8)]
        ph = Phase()
        build_hT(ph, hT, r_hT, src, l, "sc2", "sh2")
        ph.close()
        ph = Phase()
        wa = [ph.sb("wua%d" % i, [128, KC, 256], BF16) for i in range(2)]
        wb = [ph.sb("wub%d" % i, [128, KC, 256], BF16) for i in range(2)]
        r_w = [R(), R()]
        ua = [ph.sb("ua%d" % i, [128, 514], F32) for i in range(3)]
        ub = [ph.sb("ub%d" % i, [128, 514], F32) for i in range(3)]
        r_ua = [R(), R(), R()]
        r_ub = [R(), R(), R()]
        ta = [ph.sb("ta%d" % i, [128, 512], F32) for i in range(3)]
        tb_ = [ph.sb("tb%d" % i, [128, 512], F32) for i in range(3)]
        r_ta = [R(), R(), R()]
        r_tb = [R(), R(), R()]
        upend = []
        gt = [ph.sb("gt%d" % i, [128, S], BF16) for i in range(2)]
        r_gt = [R(), R()]
        PA = [ph.ps("pa%d" % i) for i in range(3)]
        PB = [ph.ps("pb%d" % i) for i in range(3)]
        r_PA = [R() for _ in range(3)]
        r_PB = [R() for _ in range(3)]
        cw = lambda tap, ci: convw[:, l * 264 + tap * 88 + ci:l * 264 + tap * 88 + ci + 1]
        cbias = lambda ci: convb[:, l * 88 + ci:l * 88 + ci + 1]
        n = 0
        for g2 in range(22):
            wi = g2 % 2
            p.dma("pool", wa[wi][:], w_up[l][:, g2 * 256:(g2 + 1) * 256].rearrange("(kc q) c -> q kc c", q=128),
                  writes=[r_w[wi]])
            p.dma("pool", wb[wi][:],
                  w_up[l][:, DFF + g2 * 256:DFF + (g2 + 1) * 256].rearrange("(kc q) c -> q kc c", q=128),
                  writes=[r_w[wi]])
            for jj in range(2):
                j = g2 * 2 + jj
                gi = j % 2
                for tb in range(8):
                    b = n % 3
                    u = n % 3
                    up_ = (n - 1) % 3
                    n += 1
                    p.mm([(lambda kc=kc: T.matmul(PA[b][:], wa[wi][:, kc, jj * 128:(jj + 1) * 128],
                                                  hT[:, kc, tb * 512:(tb + 1) * 512], start=(kc == 0),
                                                  stop=(kc == KC - 1))) for kc in range(KC)],
                         reads=[r_w[wi], r_hT[tb]], writes=[r_PA[b]])
                    p.mm([(lambda kc=kc: T.matmul(PB[b][:], wb[wi][:, kc, jj * 128:(jj + 1) * 128],
                                                  hT[:, kc, tb * 512:(tb + 1) * 512], start=(kc == 0),
                                                  stop=(kc == KC - 1))) for kc in range(KC)],
                         reads=[r_w[wi], r_hT[tb]], writes=[r_PB[b]])
                    p.op("act", lambda: A.copy(ua[u][:, 2:514], PA[b][:]), reads=[r_PA[b]], writes=[r_ua[u]])
                    p.op("dve", lambda: V.tensor_copy(ub[u][:, 2:514], PB[b][:]), reads=[r_PB[b]], writes=[r_ub[u]])
                    for (uu, r_uu) in ((ua, r_ua), (ub, r_ub)):
                        if tb == 0:
                            p.op("pool", lambda: G.memset(uu[u][:, 0:2], 0.0), writes=[r_uu[u]])
                        else:
                            p.op("pool", lambda: G.tensor_copy(uu[u][:, 0:2], uu[up_][:, 512:514]),
                                 reads=[r_uu[up_]], writes=[r_uu[u]])

                    def post(u=u, tb=tb, j=j, gi=gi):
                        for (uu, r_uu, tt_, r_tt, ci) in ((ua, r_ua, ta, r_ta, j), (ub, r_ub, tb_, r_tb, NFC + j)):
                            p.op("act", lambda: A.activation(out=tt_[u][:], in_=uu[u][:, 2:514], func=AF.Identity,
                                                             scale=cw(2, ci), bias=cbias(ci)),
                                 reads=[r_uu[u], r_c], writes=[r_tt[u]])
                            p.op("dve", lambda: V.scalar_tensor_tensor(out=tt_[u][:], in0=uu[u][:, 1:513],
                                                                       scalar=cw(1, ci), in1=tt_[u][:],
                                                                       op0=ALU.mult, op1=ALU.add),
                                 reads=[r_uu[u], r_tt[u], r_c], writes=[r_tt[u]])
                            p.op("dve", lambda: V.scalar_tensor_tensor(out=tt_[u][:], in0=uu[u][:, 0:512],
                                                                       scalar=cw(0, ci), in1=tt_[u][:],
                                                                       op0=ALU.mult, op1=ALU.add),
                                 reads=[r_uu[u], r_tt[u], r_c], writes=[r_tt[u]])
                        p.op("act", lambda: A.activation(out=ta[u][:], in_=ta[u][:], func=AF.Silu),
                             reads=[r_ta[u]], writes=[r_ta[u]])
                        p.op("dve", lambda: V.tensor_tensor(out=gt[gi][:, tb * 512:(tb + 1) * 512], in0=ta[u][:],
                                                            in1=tb_[u][:], op=ALU.mult),
                             reads=[r_ta[u], r_tb[u]], writes=[r_gt[gi]])
                        if tb == 7:
                            p.dma("sp", GT[j * 128:(j + 1) * 128, :], gt[gi][:], reads=[r_gt[gi]])

                    if upend:
                        upend.pop(0)()
                    upend.append(post)
        while upend:
            upend.pop(0)()
        ph.close()
        oph.close()

    def phase_DOWN(l, resid_src, dst, final):
        ph = Phase()
        bufs = ln_bufs(ph, final)
        gin = ph.sb("gin", [128, NFC, 512], BF16)
        r_gin = R()
        wd = [ph.sb("wd%d" % i, [128, NFC, 128], BF16) for i in range(3)]
        r_wd = [R(), R(), R()]
        PM = [ph.ps("pmd%d" % i) for i in range(2)]
        r_PM = [R(), R()]
        gv = GT.rearrange("(kc q) t -> q kc t", q=128)
        nw = 0
        for tb in range(8):
            for k4 in range(4):
                p.dma("sp", gin[:, k4 * 11:(k4 + 1) * 11, :], gv[:, k4 * 11:(k4 + 1) * 11, tb * 512:(tb + 1) * 512],
                      writes=[r_gin])

            def psm_get(j):
                nonlocal nw
                wi = nw % 3
                pm = nw % 2
                nw += 1
                p.dma("sp", wd[wi][:], WD16[j], writes=[r_wd[wi]])
                p.mm([(lambda kc=kc: T.matmul(PM[pm][:], wd[wi][:, kc, :], gin[:, kc, :], start=(kc == 0),
                                              stop=(kc == NFC - 1))) for kc in range(NFC)],
                     reads=[r_wd[wi], r_gin], writes=[r_PM[pm]])
                return PM[pm], r_PM[pm]

            ln_block(bufs, tb, l, "g2", 64 + l * 16, 96 + l * 16, psm_get, resid_src, dst, final)
        ph.close()

    stop = None
    for d_ in dbg:
        if d_.startswith("stop:"):
            stop = d_[5:]
    def done(tag):
        return stop == tag

    def run():
        if "skip:M" not in dbg:
            phase_M()
        if done("M"): return
        if "skip:T" not in dbg:
            phase_T()
        if done("T"): return
        if "skip:B" not in dbg:
            phase_B()
        if done("B"): return
        cur, oth = XT, X1T
        for l in range(2):
            if "skip:W" in dbg and False:
                phase_W(l)
            phase_QKV(l, XT)
            if done("QKV%d" % l): return
            hs = range(32)
            for d_ in dbg:
                if d_.startswith("heads:"):
                    hs = [int(v) for v in d_[6:].split(".")]
            phase_ATT(l, hs)
            if done("ATT%d" % l): return
            phase_POST(l, XT, X1T)
            if done("POST%d" % l): return
            phase_UP(l, X1T)
            if done("UP%d" % l): return
            phase_DOWN(l, X1T, XT, final=(l == 1))
            if done("DOWN%d" % l): return
    run()
    p.barrier()
    return nc, p


def make_inputs(inputs, b, consts):
    f = lambda a: np.ascontiguousarray(a, dtype=np.float32)
    pl = lambda v: f(np.asarray(v).reshape(-1, 128).T)
    m = {}
    m["x"] = f(inputs["x"][b])
    m["cT"] = pl(inputs["c"][b])
    for l in range(2):
        m["w_ada%d" % l] = f(inputs["w_ada"][l])
        m["bada%d" % l] = pl(inputs["b_ada"][l])
        m["w_in%d" % l] = f(inputs["w_in"][l])
        m["w_out%d" % l] = f(inputs["w_out"][l])
        m["w_up%d" % l] = f(inputs["w_up"][l])
        m["w_down%d" % l] = f(inputs["w_down"][l])
    m["bf"] = f(np.asarray(inputs["b_f"]).T)
    gm = [np.concatenate([inputs["g_mix_a"][l], inputs["g_mix_b"][l], inputs["g_mix_c"][l]]) for l in range(2)]
    m["gmix"] = f(np.concatenate([pl(g) for g in gm], axis=1))
    m["lnp"] = f(np.concatenate([pl(inputs[k][l]) for k in ("ln1_g", "ln1_b", "ln2_g", "ln2_b") for l in range(2)],
                                axis=1))
    cw = []
    for l in range(2):
        for tap in range(3):
            cw.append(pl(inputs["conv_w"][l][tap]))
    m["convw"] = f(np.concatenate(cw, axis=1))
    m["convb"] = f(np.concatenate([pl(inputs["conv_b"][l]) for l in range(2)], axis=1))
    rb = np.asarray(inputs["rel_bias"], dtype=np.float32)
    m["relrep"] = f(np.repeat(rb[:, :, None], 128, axis=2).reshape(32, 22 * 128))
    m.update(consts)
    return m


def kernel(**inputs):
    inputs = {k: np.asarray(v) for k, v in inputs.items()}
    consts = host_constants()
    nc, _ = build()
    per_b = [make_inputs(inputs, b, consts) for b in range(4)]
    idle = {k: np.zeros_like(v) for k, v in per_b[0].items()}
    in_maps = [per_b[c // 2] if c % 2 == 0 else idle for c in range(NCORES)]
    res = run_bass_kernel_spmd(nc, in_maps, core_ids=list(range(NCORES)))
    outs = [np.asarray(res.results[2 * b]["out"], dtype=np.float32) for b in range(4)]
    return np.stack(outs, axis=0)
```
